# Optimizing a Trainium2 kernel written in Bass

```python
import math
import jax, jax.numpy as jnp
from jax import lax
import numpy as np

D_MODEL = 1024
BATCH = 4
SEQ = 8192
DEPTH = 1

MEM_LEN = 256
XA_HEADS = 4
XA_HEAD_DIM = 128
DIFF_HEADS = 4
DIFF_QK_DIM = 64
DIFF_V_DIM = 2 * DIFF_QK_DIM
DSA_HEADS = 8
DSA_HEAD_DIM = 64
IDX_HEADS = 8
IDX_DIM = 64
TOPK_MAX = 256
ROPE_THETA = 500000.0
ROPE_FRACTION = 4
Q_BLOCK = 128
N_BRANCH = 3
N_GROUPS = 4
EXPERTS_PER_GROUP = 4
N_EXPERTS = N_GROUPS * EXPERTS_PER_GROUP
TOP_K_EXPERTS = 2
D_EXPERT = 512
EPS = 1e-6

W_DIFF_QK = DIFF_HEADS * 2 * DIFF_QK_DIM
W_DIFF_V = DIFF_HEADS * DIFF_V_DIM
W_DSA = DSA_HEADS * DSA_HEAD_DIM
W_IDX_Q = IDX_HEADS * IDX_DIM
W_IDX_K = IDX_DIM
W_IDX_W = IDX_HEADS
W_MEM_Q = XA_HEADS * XA_HEAD_DIM
W_GATES = N_BRANCH * D_MODEL
WIDTHS = (W_DIFF_QK, W_DIFF_QK, W_DIFF_V, W_DSA, W_DSA, W_DSA,
          W_IDX_Q, W_IDX_K, W_IDX_W, W_MEM_Q, W_GATES)
D_IN_PROJ = sum(WIDTHS)
SPLIT_POINTS = tuple(sum(WIDTHS[:i + 1]) for i in range(len(WIDTHS) - 1))

kernel_name = "hybrid_gated_diffattn_dsa_memxattn_hiermoe"


def rmsnorm(x, g):
    xf = x.astype(jnp.float32)
    y = xf * lax.rsqrt(jnp.mean(xf * xf, axis=-1, keepdims=True) + EPS)
    return (y * g.astype(jnp.float32)).astype(x.dtype)


def partial_rope(x, pos):
    d = x.shape[-1]
    rot = d // ROPE_FRACTION
    half = rot // 2
    inv_freq = ROPE_THETA ** (-jnp.arange(0, rot, 2, dtype=jnp.float32) / rot)
    ang = pos.astype(jnp.float32)[..., None] * inv_freq
    shape = ang.shape[:2] + (1,) * (x.ndim - 3) + (half,)
    cos = jnp.cos(ang).reshape(shape)
    sin = jnp.sin(ang).reshape(shape)
    xf = x.astype(jnp.float32)
    x1, x2, rest = xf[..., :half], xf[..., half:rot], xf[..., rot:]
    out = jnp.concatenate([x1 * cos - x2 * sin, x2 * cos + x1 * sin, rest], axis=-1)
    return out.astype(x.dtype)


def to_blocks(a):
    b, s = a.shape[:2]
    return a.reshape((b, s // Q_BLOCK, Q_BLOCK) + a.shape[2:]).swapaxes(0, 1)


def from_blocks(a):
    a = a.swapaxes(0, 1)
    return a.reshape((a.shape[0], a.shape[1] * a.shape[2]) + a.shape[3:])


def diff_attention(q1, q2, k1, k2, v, lam, g_sub, lam_init):
    b, s, h, dq = q1.shape
    scale = dq ** -0.5
    key_pos = jnp.arange(s)

    def block(args):
        i, q1b, q2b = args
        q_pos = i * Q_BLOCK + jnp.arange(Q_BLOCK)
        causal = (key_pos[None, :] <= q_pos[:, None])[None, None]
        s1 = jnp.einsum('bqhd,bkhd->bhqk', q1b, k1).astype(jnp.float32) * scale
        s2 = jnp.einsum('bqhd,bkhd->bhqk', q2b, k2).astype(jnp.float32) * scale
        p1 = jax.nn.softmax(jnp.where(causal, s1, -jnp.inf), axis=-1)
        p2 = jax.nn.softmax(jnp.where(causal, s2, -jnp.inf), axis=-1)
        a = p1 - lam * p2
        return jnp.einsum('bhqk,bkhd->bqhd', a.astype(v.dtype), v)

    nb = s // Q_BLOCK
    out = from_blocks(lax.map(block, (jnp.arange(nb), to_blocks(q1), to_blocks(q2))))
    out = rmsnorm(out, g_sub) * (1.0 - lam_init)
    return out.reshape(b, s, h * v.shape[-1])


def dsa_attention(q, k, v, q_idx, k_idx, w_idx, top_k):
    b, s, h, dh = q.shape
    key_pos = jnp.arange(s)
    idx_scale = IDX_DIM ** -0.5
    head_scale = IDX_HEADS ** -0.5
    att_scale = dh ** -0.5
    gather_rows = jax.vmap(lambda arr, sel: arr[sel])

    def block(args):
        i, qb, qib, wib = args
        q_pos = i * Q_BLOCK + jnp.arange(Q_BLOCK)
        causal = key_pos[None, :] <= q_pos[:, None]
        logits = jnp.einsum('bqhd,bkd->bqhk', qib, k_idx).astype(jnp.float32) * idx_scale
        score = jnp.einsum('bqh,bqhk->bqk', wib.astype(jnp.float32) * head_scale,
                           jax.nn.relu(logits))
        score = jnp.where(causal[None], score, -jnp.inf)
        _, sel = lax.top_k(score, top_k)
        valid = sel <= q_pos[None, :, None]
        ks = gather_rows(k, sel)
        vs = gather_rows(v, sel)
        sc = jnp.einsum('bqhd,bqkhd->bhqk', qb, ks).astype(jnp.float32) * att_scale
        sc = jnp.where(valid[:, None], sc, -jnp.inf)
        p = jax.nn.softmax(sc, axis=-1)
        return jnp.einsum('bhqk,bqkhd->bqhd', p.astype(v.dtype), vs)

    nb = s // Q_BLOCK
    out = from_blocks(lax.map(block, (jnp.arange(nb), to_blocks(q), to_blocks(q_idx), to_blocks(w_idx))))
    return out.reshape(b, s, h * dh)


def memory_attention(q, mk, mv):
    b, s, h, d = q.shape
    sc = jnp.einsum('bqhd,bmhd->bhqm', q, mk).astype(jnp.float32) * (d ** -0.5)
    p = jax.nn.softmax(sc, axis=-1)
    out = jnp.einsum('bhqm,bmhd->bqhd', p.astype(mv.dtype), mv)
    return out.reshape(b, s, h * d)


def hier_moe(xn, w_rg, b_rg, w_re, b_re, w_e_in, w_e_out):
    b, s, d = xn.shape
    t = xn.reshape(b * s, d)
    n_tok = t.shape[0]
    g_logits = (t @ w_rg).astype(jnp.float32) + b_rg.astype(jnp.float32)
    g_prob = jax.nn.softmax(g_logits, axis=-1)
    grp = jnp.argmax(g_logits, axis=-1)
    p_grp = jnp.take_along_axis(g_prob, grp[:, None], axis=-1)
    e_logits = ((t @ w_re).astype(jnp.float32) + b_re.astype(jnp.float32)).reshape(
        n_tok, N_GROUPS, EXPERTS_PER_GROUP)
    e_logits = jnp.take_along_axis(e_logits, grp[:, None, None], axis=1)[:, 0]
    e_prob = jax.nn.softmax(e_logits, axis=-1)
    top_w, top_i = lax.top_k(e_prob, TOP_K_EXPERTS)
    top_w = top_w / jnp.sum(top_w, axis=-1, keepdims=True)
    expert_id = grp[:, None] * EXPERTS_PER_GROUP + top_i
    combine = p_grp * top_w
    cw = jnp.sum(jax.nn.one_hot(expert_id, N_EXPERTS, dtype=jnp.float32) * combine[..., None], axis=1)
    y = jnp.zeros((n_tok, d), jnp.float32)
    for e in range(N_EXPERTS):
        gu = t @ w_e_in[e]
        hid = jax.nn.silu(gu[:, :D_EXPERT]) * gu[:, D_EXPERT:]
        y = y + cw[:, e:e + 1] * (hid @ w_e_out[e]).astype(jnp.float32)
    return y.astype(xn.dtype).reshape(b, s, d)


def setup_inputs(seed: int = 0) -> dict:
    key = jax.random.key(seed)
    ks = jax.random.split(key, 32)
    f32 = jnp.float32
    L, D = DEPTH, D_MODEL

    def nrm(k, shape, scale):
        return jax.random.normal(k, shape, f32) * scale

    x = jax.random.normal(ks[0], (BATCH, SEQ, D), f32)
    offsets = jax.random.randint(ks[1], (BATCH, 1), 0, 4096, dtype=jnp.int32)
    positions = offsets + jnp.arange(SEQ, dtype=jnp.int32)[None, :]
    mem = jax.random.normal(ks[2], (BATCH, MEM_LEN, D), f32)
    return {
        "x": x,
        "positions": positions,
        "mem": mem,
        "g_mix": 1.0 + nrm(ks[3], (L, D), 0.02),
        "w_in": nrm(ks[4], (L, D, D_IN_PROJ), D ** -0.5),
        "b_gate": nrm(ks[5], (L, W_GATES), 0.1),
        "lambda_q1": nrm(ks[6], (L, DIFF_QK_DIM), 0.1),
        "lambda_k1": nrm(ks[7], (L, DIFF_QK_DIM), 0.1),
        "lambda_q2": nrm(ks[8], (L, DIFF_QK_DIM), 0.1),
        "lambda_k2": nrm(ks[9], (L, DIFF_QK_DIM), 0.1),
        "g_diff_sub": 1.0 + nrm(ks[10], (L, DIFF_V_DIM), 0.02),
        "g_mem": 1.0 + nrm(ks[11], (L, D), 0.02),
        "w_mem_kv": nrm(ks[12], (L, D, 2 * W_MEM_Q), D ** -0.5),
        "w_br_diff": nrm(ks[13], (L, W_DIFF_V, D), W_DIFF_V ** -0.5),
        "w_br_dsa": nrm(ks[14], (L, W_DSA, D), W_DSA ** -0.5),
        "w_br_mem": nrm(ks[15], (L, W_MEM_Q, D), W_MEM_Q ** -0.5),
        "w_out": nrm(ks[16], (L, D, D), D ** -0.5),
        "g_ffn": 1.0 + nrm(ks[17], (L, D), 0.02),
        "w_route_group": nrm(ks[18], (L, D, N_GROUPS), D ** -0.5),
        "b_route_group": nrm(ks[19], (L, N_GROUPS), 0.01),
        "w_route_expert": nrm(ks[20], (L, D, N_EXPERTS), D ** -0.5),
        "b_route_expert": nrm(ks[21], (L, N_EXPERTS), 0.01),
        "w_exp_in": nrm(ks[22], (L, N_EXPERTS, D, 2 * D_EXPERT), D ** -0.5),
        "w_exp_out": nrm(ks[23], (L, N_EXPERTS, D_EXPERT, D), D_EXPERT ** -0.5),
        "g_final": 1.0 + nrm(ks[24], (D,), 0.02),
    }


def reference(x, positions, mem, g_mix, w_in, b_gate, lambda_q1, lambda_k1, lambda_q2, lambda_k2,
              g_diff_sub, g_mem, w_mem_kv, w_br_diff, w_br_dsa, w_br_mem, w_out, g_ffn,
              w_route_group, b_route_group, w_route_expert, b_route_expert, w_exp_in, w_exp_out,
              g_final):
    b, s, d = x.shape
    m = mem.shape[1]
    top_k = min(TOPK_MAX, s // 4)
    h = x
    for l in range(DEPTH):
        lam_init = 0.8 - 0.6 * math.exp(-0.3 * l)
        n = rmsnorm(h, g_mix[l])
        proj = n @ w_in[l]
        dq, dk, dv, sq, sk, sv, iq, ik, iw, mq, gl = jnp.split(proj, SPLIT_POINTS, axis=-1)

        dq = partial_rope(dq.reshape(b, s, DIFF_HEADS, 2, DIFF_QK_DIM), positions)
        dk = partial_rope(dk.reshape(b, s, DIFF_HEADS, 2, DIFF_QK_DIM), positions)
        dv = dv.reshape(b, s, DIFF_HEADS, DIFF_V_DIM)
        lam = (jnp.exp(jnp.sum(lambda_q1[l].astype(jnp.float32) * lambda_k1[l].astype(jnp.float32)))
               - jnp.exp(jnp.sum(lambda_q2[l].astype(jnp.float32) * lambda_k2[l].astype(jnp.float32)))
               + lam_init)
        y_diff = diff_attention(dq[..., 0, :], dq[..., 1, :], dk[..., 0, :], dk[..., 1, :], dv,
                                lam, g_diff_sub[l], lam_init)

        sq = partial_rope(sq.reshape(b, s, DSA_HEADS, DSA_HEAD_DIM), positions)
        sk = partial_rope(sk.reshape(b, s, DSA_HEADS, DSA_HEAD_DIM), positions)
        sv = sv.reshape(b, s, DSA_HEADS, DSA_HEAD_DIM)
        iq = partial_rope(iq.reshape(b, s, IDX_HEADS, IDX_DIM), positions)
        ik = partial_rope(ik, positions)
        y_dsa = dsa_attention(sq, sk, sv, iq, ik, iw, top_k)

        mkv = rmsnorm(mem, g_mem[l]) @ w_mem_kv[l]
        mk = mkv[..., :W_MEM_Q].reshape(b, m, XA_HEADS, XA_HEAD_DIM)
        mv = mkv[..., W_MEM_Q:].reshape(b, m, XA_HEADS, XA_HEAD_DIM)
        y_mem = memory_attention(mq.reshape(b, s, XA_HEADS, XA_HEAD_DIM), mk, mv)

        gates = jax.nn.sigmoid((gl + b_gate[l]).astype(jnp.float32)).astype(h.dtype).reshape(b, s, N_BRANCH, d)
        merged = (gates[:, :, 0] * (y_diff @ w_br_diff[l])
                  + gates[:, :, 1] * (y_dsa @ w_br_dsa[l])
                  + gates[:, :, 2] * (y_mem @ w_br_mem[l]))
        h = h + merged @ w_out[l]

        h = h + hier_moe(rmsnorm(h, g_ffn[l]), w_route_group[l], b_route_group[l],
                         w_route_expert[l], b_route_expert[l], w_exp_in[l], w_exp_out[l])
    return rmsnorm(h, g_final)
```

```python
import numpy as np
from contextlib import ExitStack
import concourse.bass as bass
import concourse.mybir as mybir
from concourse.bass_utils import run_bass_kernel_spmd

F32 = mybir.dt.float32
BF16 = mybir.dt.bfloat16
I32 = mybir.dt.int32
ALU = mybir.AluOpType
AF = mybir.ActivationFunctionType
AX = mybir.AxisListType

SAME_ENGINE_SYNC = True


class Tok:
    __slots__ = ("w", "r", "psum")

    def __init__(self, psum=False):
        self.w = None
        self.r = {}
        self.psum = psum


def toks(n):
    return [Tok() for _ in range(n)]


class Sched:
    ENGS = ("pe", "act", "dve", "pool", "sp")
    CH = 16000
    NDMA = 48

    def __init__(self, nc, es, est_counts):
        self.nc = nc
        self.q = {e: [] for e in self.ENGS}
        self.cnt = {e: 0 for e in self.ENGS}
        self.waited = {e: {} for e in self.ENGS}
        self.sems = {}
        for e in ("pe", "act", "dve", "pool"):
            n = est_counts.get(e, self.CH) // self.CH + 2
            self.sems[e] = [es.enter_context(nc.semaphore(f"s_{e}{i}")) for i in range(n)]
        self.NSW = 16
        self.dsem = [es.enter_context(nc.semaphore(f"s_dma{i}")) for i in range(self.NDMA + self.NSW)]
        self.dk = 0
        self.dk_sw = 0
        self.dlast = {}
        self.nwait = 0
        self.pending_unsig = {e: False for e in self.ENGS}
        self.log = {e: [] for e in self.ENGS}
        self.semname = {}

    def _semval(self, tok):
        if tok[0] == "c":
            _, e, k = tok
            return self.sems[e][(k - 1) // self.CH], (k - 1) % self.CH + 1, ("c", e, (k - 1) // self.CH)
        _, i, v = tok
        return self.dsem[i], v, ("d", i)

    def wait(self, eng, tok):
        if tok is None:
            return
        if tok[0] == "c" and tok[1] == eng:
            if eng == "pe" or not SAME_ENGINE_SYNC:
                return
        sem, val, key = self._semval(tok)
        if self.waited[eng].get(key, 0) >= val:
            return
        self.waited[eng][key] = val
        self.nwait += 1
        self.q[eng].append(lambda E, sem=sem, val=val: E.wait_ge(sem, val))
        self.log[eng].append(('wait', key, val))

    def _deps(self, eng, reads, writes):
        for t in reads:
            self.wait(eng, t.w)
            if t.psum:
                for k, r in t.r.items():
                    if k != eng and k != "pe":
                        self.wait(eng, r)
        for t in writes:
            self.wait(eng, t.w)
            for r in t.r.values():
                self.wait(eng, r)

    def _mark(self, tok, reads, writes):
        for t in reads:
            t.r[tok[1] if tok[0] == "c" else ("d", tok[1])] = tok
        for t in writes:
            t.w = tok
            t.r = {}

    def op(self, eng, fn, reads=(), writes=(), signal=True):
        self._deps(eng, reads, writes)
        if signal:
            self.cnt[eng] += 1
            k = self.cnt[eng]
            tok = ("c", eng, k)
            sem = self.sems[eng][(k - 1) // self.CH]
            self.q[eng].append(lambda E, fn=fn, sem=sem: fn(E).then_inc(sem, 1))
            self.log[eng].append(('op', k))
        else:
            tok = ("c", eng, self.cnt[eng] + 1)
            self.q[eng].append(lambda E, fn=fn: fn(E))
            self.log[eng].append(('op-unsig', self.cnt[eng] + 1))
        self._mark(tok, reads, writes)
        return tok

    def dma(self, eng, out, in_, reads=(), writes=(), **kw):
        self._deps(eng, reads, writes)
        if eng == "pool":
            i = self.NDMA + self.dk_sw % self.NSW
            v = 16 * (self.dk_sw // self.NSW + 1)
            self.dk_sw += 1
        else:
            i = self.dk % self.NDMA
            v = 16 * (self.dk // self.NDMA + 1)
            self.dk += 1
        self.dlast[i] = v
        if v > 16:
            self.wait(eng, ("d", i, v - 16))
        sem = self.dsem[i]
        self.q[eng].append(lambda E, out=out, in_=in_, sem=sem, kw=kw:
                           E.dma_start(out=out, in_=in_, **kw).then_inc(sem, 16))
        tok = ("d", i, v)
        self.log[eng].append(('dma', i, v))
        self._mark(tok, reads, writes)
        return tok

    def finish(self, out_toks):
        for t in out_toks:
            self.wait("sp", t.w)
        for e in ("pe", "act", "dve", "pool"):
            if self.cnt[e]:
                self.wait("sp", ("c", e, self.cnt[e]))

    def emit(self):
        nc = self.nc
        with nc.Block() as block:
            @block.tensor
            def _(E):
                for f in self.q["pe"]:
                    f(E)

            @block.scalar
            def _(E):
                for f in self.q["act"]:
                    f(E)

            @block.vector
            def _(E):
                for f in self.q["dve"]:
                    f(E)

            @block.gpsimd
            def _(E):
                for f in self.q["pool"]:
                    f(E)

            @block.sync
            def _(E):
                for f in self.q["sp"]:
                    f(E)

import math

PI = math.pi
FP8 = mybir.dt.float8e5
NCH_LIMIT = None
ROPE_ADD_ENG = 'pool'
ROPE_LEVEL = 5
EXP = 0
FM_LIMIT = 99
SIG_ALL = False
SKIP_NORM = False
DVE_VAR = 0
EPS = 1e-6
TF = 8192
TO = 4096
CW1 = 6.28125
CW2 = 2.0 * math.pi - 6.28125
C_DQ, C_DK, C_DV, C_SQ, C_SK, C_SV, C_IQ, C_IK, C_IW, C_MQ, C_GL = (
    0, 512, 1024, 1536, 2048, 2560, 3072, 3584, 3648, 3656, 4168)


class Buf:
    __slots__ = ("t", "k")

    def __init__(self, t):
        self.t = t
        self.k = Tok()


def build(debug=False, stop_after=99, norope=False, notm=False, nofm=False, notab=False, tabonly=False):
    nc = bass.Bass("TRN2", target_bir_lowering=False)

    def din(name, shape, dt=F32):
        return nc.dram_tensor(name, list(shape), dt, kind="ExternalInput").ap()

    def dscr(name, shape, dt):
        return nc.dram_tensor(name, list(shape), dt,
                              kind=("ExternalOutput" if debug else "Internal")).ap()

    xT_full = din("xT_full", [1024, TF])
    xT_own = din("xT_own", [1024, TO])
    x_own = din("x_own", [TO, 1024])
    pos_full = din("pos_full", [1, TF], I32)
    pos_own = din("pos_own", [1, TO], I32)
    memT = din("memT", [1024, 256])
    w_in = din("w_in", [1024, 7240])
    b_gate24 = din("b_gate24", [128, 24])
    lam_in = din("lam_in", [1, 256])
    gsub_in = din("gsub_in", [128, 1])
    gmix8 = din("gmix8", [128, 8])
    gmem8 = din("gmem8", [128, 8])
    gffn8 = din("gffn8", [128, 8])
    w_mem_kv = din("w_mem_kv", [1024, 1024])
    w_br = din("w_br", [3, 512, 1024])
    w_out = din("w_out", [1024, 1024])
    w_r = din("w_r", [1024, 20])
    b_r = din("b_r", [1, 20])
    w_e_in = din("w_e_in", [16, 1024, 1024])
    w_e_out = din("w_e_out", [16, 512, 1024])
    gfin = din("gfin", [1, 1024])
    ident_in = din("ident_in", [128, 128])
    rmat_in = din("rmat_in", [128, 128])
    invf_in = din("invf_in", [128, 1])
    cm_diff_in = din("cm_diff_in", [128, 8, 512])
    cm_idx_in = din("cm_idx_in", [128, 256])
    out_own = nc.dram_tensor("out_own", [TO, 1024], F32, kind="ExternalOutput").ap()

    NTF = dscr("NTF", [8, 128, TF], BF16)
    NTO = dscr("NTO", [8, 128, TO], BF16)
    KTD = dscr("KTD", [4, 128, TF], BF16)
    IKT = dscr("IKT", [128, TF], BF16)
    VD = dscr("VD", [TF, 512], BF16)
    KTS = dscr("KTS", [4, 128, TF], BF16)
    VAUG = dscr("VAUG", [TF, 1024], BF16)
    QTD = dscr("QTD", [4, 128, TO], BF16)
    QTS = dscr("QTS", [4, 128, TO], BF16)
    IQT = dscr("IQT", [4, 128, TO], BF16)
    MQT = dscr("MQT", [4, 128, TO], BF16)
    IW = dscr("IW", [TO, 8], F32)
    YD = dscr("YD", [4, 128, TO], BF16)
    YS = dscr("YS", [4, 128, TO], BF16)
    XNT = dscr("XNT", [8, 128, TO], BF16)
    H2 = dscr("H2", [TO, 1024], F32)

    with ExitStack() as es:
        S = Sched(nc, es, {"pe": 120000, "act": 40000, "dve": 60000, "pool": 20000})

        def sb(st, name, shape, dt):
            return Buf(st.enter_context(nc.sbuf_tensor(name, list(shape), dt)))

        pb = [Buf(es.enter_context(nc.psum_tensor(f"pb{i}", [128, 512], F32))) for i in range(8)]
        for b_ in pb:
            b_.k.psum = True

        ident = sb(es, "ident", [128, 128], BF16)
        rmat = sb(es, "rmat", [128, 128], BF16)
        ones = sb(es, "ones", [128, 128], BF16)
        zeros = sb(es, "zeros", [128, 512], BF16)
        invf = sb(es, "invf", [128, 1], F32)
        S.dma("pool", ident.t[:], ident_in, writes=[ident.k])
        S.dma("pool", rmat.t[:], rmat_in, writes=[rmat.k])
        S.dma("sp", invf.t[:], invf_in, writes=[invf.k])
        S.op("pool", lambda E: E.memset(ones.t[:], 1.0), writes=[ones.k])
        S.op("pool", lambda E: E.memset(zeros.t[:], 0.0), writes=[zeros.k])

        def barrier():
            for e in S.ENGS:
                for p in ("pe", "act", "dve", "pool"):
                    if p != e and S.cnt[p]:
                        S.wait(e, ("c", p, S.cnt[p]))
                for i, v in S.dlast.items():
                    S.wait(e, ("d", i, v))

        def mmgroup(out_ap, pbuf, pairs, rtoks, start=True, stop=True, sig_last=True, **kw):
            n = len(pairs)
            for i, (l, r) in enumerate(pairs):
                S.op("pe", lambda E, l=l, r=r, i=i: E.matmul(
                    out_ap, l, r, start=(start and i == 0), stop=(stop and i == n - 1), **kw),
                    reads=rtoks, writes=[pbuf.k], signal=((sig_last and i == n - 1) or SIG_ALL))

        def wload(dst_ap, src_ap, tok):
            S.dma("pool", dst_ap, src_ap, writes=[tok])

        def norm_chunk(xs, sq, nt, sd, rs, g8, w):
            S.op("act", lambda E: E.activation(out=sq.t[:, :, :w], in_=xs.t[:, :, :w], func=AF.Square),
                 reads=[xs.k], writes=[sq.k])
            mmgroup(pb[7].t[:, :w], pb[7], [(ones.t[:], sq.t[:, kc, :w]) for kc in range(8)], [sq.k, ones.k])
            S.op("act", lambda E: E.activation(out=sd.t[:, :w], in_=pb[7].t[:, :w], func=AF.Sqrt,
                                               scale=1.0 / 1024, bias=EPS),
                 reads=[pb[7].k], writes=[sd.k])
            S.op("dve", lambda E: E.reciprocal(out=rs.t[:, :w], in_=sd.t[:, :w]), reads=[sd.k], writes=[rs.k])
            for kc in range(8):
                S.op("dve", lambda E, kc=kc: E.scalar_tensor_tensor(
                    out=nt.t[:, kc, :w], in0=xs.t[:, kc, :w], scalar=g8.t[:, kc:kc + 1], in1=rs.t[:, :w],
                    op0=ALU.mult, op1=ALU.mult), reads=[xs.k, rs.k, g8.k], writes=[nt.k])

        def phase_norm(xT, T, g_in, dest, tag):
            with ExitStack() as ph:
                g8 = sb(ph, "g8" + tag, [128, 8], F32)
                S.dma("sp", g8.t[:], g_in, writes=[g8.k])
                xs = [sb(ph, f"xs{tag}{i}", [128, 8, 512], F32) for i in range(2)]
                sq = [sb(ph, f"sq{tag}{i}", [128, 8, 512], BF16) for i in range(2)]
                nt = [sb(ph, f"nt{tag}{i}", [128, 8, 512], BF16) for i in range(2)]
                sd = sb(ph, "sd" + tag, [128, 512], F32)
                rs = sb(ph, "rs" + tag, [128, 512], F32)
                nch = T // 512

                def ld(c):
                    S.dma("sp", xs[c % 2].t[:], xT[:, c * 512:(c + 1) * 512].rearrange("(k p) t -> p k t", p=128),
                          writes=[xs[c % 2].k])
                ld(0)
                for c in range(nch):
                    if c + 1 < nch:
                        ld(c + 1)
                    i = c % 2
                    norm_chunk(xs[i], sq[i], nt[i], sd, rs, g8, 512)
                    S.dma("sp", dest[:, :, c * 512:(c + 1) * 512].rearrange("k p t -> p k t"), nt[i].t[:],
                          reads=[nt[i].k])
                barrier()

        if not SKIP_NORM:
            phase_norm(xT_full, TF, gmix8, NTF, "f")
        if stop_after >= 1 and not SKIP_NORM:
            phase_norm(xT_own, TO, gmix8, NTO, "o")

        def phase_proj(nT_dram, T, pos_dram, wcols, W, fm_specs, tm_specs, tag):
            with ExitStack() as ph:
                wt = sb(ph, "wt" + tag, [128, 8, W], BF16)
                for (dst0, src0, n) in wcols:
                    wload(wt.t[:, :, dst0:dst0 + n],
                          w_in[:, src0:src0 + n].rearrange("(k p) c -> p k c", p=128), wt.k)
                ntc = [sb(ph, f"ntc{tag}{i}", [128, 8, 512], BF16) for i in range(2)]
                posi = sb(ph, "posi" + tag, [128, T], I32)
                S.dma("sp", posi.t[:], pos_dram[0, :].partition_broadcast(128), writes=[posi.k])
                pf = sb(ph, "pf" + tag, [128, 512], F32)
                ang = sb(ph, "ang" + tag, [128, 512], F32)
                ki = sb(ph, "ki" + tag, [128, 512], I32)
                kf = sb(ph, "kf" + tag, [128, 512], F32)
                rr = sb(ph, "rr" + tag, [128, 512], F32)
                tt = sb(ph, "tt" + tag, [128, 512], F32)
                Ccs = [sb(ph, f"Cc{tag}{i}", [128, 512], F32) for i in range(2)]
                Scs = [sb(ph, f"Sc{tag}{i}", [128, 512], F32) for i in range(2)]
                qbb = [sb(ph, f"qbb{tag}{i}", [128, 512], BF16) for i in range(2)]
                t1b = [sb(ph, f"t1b{tag}{i}", [128, 512], F32) for i in range(2)]
                t2b = [sb(ph, f"t2b{tag}{i}", [128, 512], F32) for i in range(2)]
                otb = [sb(ph, f"otb{tag}{i}", [128, 512], BF16) for i in range(3)]
                vst = [sb(ph, f"vst{tag}{i}", [128, 512], BF16) for i in range(2)]
                vau = [sb(ph, f"vau{tag}{i}", [128, 8, 128], BF16) for i in range(2)]
                iwst = [sb(ph, f"iwst{tag}{i}", [128, 8], F32) for i in range(2)]
                for i in range(2):
                    S.op("pool", lambda E, i=i: E.memset(vau[i].t[:], 1.0), writes=[vau[i].k])
                nch = T // 512 if NCH_LIMIT is None else (NCH_LIMIT * 2 if T == TF else NCH_LIMIT)

                def ld(c):
                    S.dma("sp", ntc[c % 2].t[:], nT_dram[:, :, c * 512:(c + 1) * 512].rearrange("k p t -> p k t"),
                          writes=[ntc[c % 2].k])

                def tables_dve(c):
                    S.op("dve", lambda E, c=c: E.tensor_copy(out=pf.t[:], in_=posi.t[:, c * 512:(c + 1) * 512]),
                         reads=[posi.k], writes=[pf.k])
                    S.op("dve", lambda E: E.tensor_scalar(out=ang.t[:], in0=pf.t[:], scalar1=invf.t[:, 0:1],
                                                          scalar2=None, op0=ALU.mult),
                         reads=[pf.k, invf.k], writes=[ang.k])
                    S.op("dve", lambda E: E.tensor_scalar(out=ki.t[:], in0=ang.t[:], scalar1=1.0 / (2 * PI),
                                                          scalar2=None, op0=ALU.mult),
                         reads=[ang.k], writes=[ki.k])
                    S.op("dve", lambda E: E.tensor_copy(out=kf.t[:], in_=ki.t[:]), reads=[ki.k], writes=[kf.k])
                    S.op("dve", lambda E: E.scalar_tensor_tensor(out=rr.t[:], in0=kf.t[:], scalar=-CW1, in1=ang.t[:],
                                                                 op0=ALU.mult, op1=ALU.add),
                         reads=[kf.k, ang.k], writes=[rr.k])
                    S.op("dve", lambda E: E.scalar_tensor_tensor(out=rr.t[:], in0=kf.t[:], scalar=-CW2, in1=rr.t[:],
                                                                 op0=ALU.mult, op1=ALU.add),
                         reads=[kf.k, rr.k], writes=[rr.k])
                    S.op("dve", lambda E: E.tensor_scalar(out=tt.t[:], in0=rr.t[:], scalar1=PI, scalar2=-2 * PI,
                                                          op0=ALU.is_gt, op1=ALU.mult), reads=[rr.k], writes=[tt.k])
                    S.op("dve", lambda E: E.tensor_tensor(out=rr.t[:], in0=rr.t[:], in1=tt.t[:], op=ALU.add),
                         reads=[rr.k, tt.k], writes=[rr.k])
                    S.op("dve", lambda E: E.tensor_scalar(out=tt.t[:], in0=rr.t[:], scalar1=-PI, scalar2=2 * PI,
                                                          op0=ALU.is_lt, op1=ALU.mult), reads=[rr.k], writes=[tt.k])
                    S.op("dve", lambda E: E.tensor_tensor(out=rr.t[:], in0=rr.t[:], in1=tt.t[:], op=ALU.add),
                         reads=[rr.k, tt.k], writes=[rr.k])
                    S.op("dve", lambda E: E.tensor_scalar(out=rr.t[:], in0=rr.t[:], scalar1=-3.141592, scalar2=3.141592,
                                                          op0=ALU.max, op1=ALU.min), reads=[rr.k], writes=[rr.k])

                def tables_act(c):
                    Cc, Sc = Ccs[c % 2], Scs[c % 2]
                    S.op("act", lambda E: E.activation(out=Sc.t[:], in_=rr.t[:], func=AF.Sin),
                         reads=[rr.k], writes=[Sc.k])
                    S.op("act", lambda E: E.activation(out=tt.t[:], in_=rr.t[:], func=AF.Abs),
                         reads=[rr.k], writes=[tt.k])
                    S.op("act", lambda E: E.activation(out=Cc.t[:], in_=tt.t[:], func=AF.Sin, scale=-1.0, bias=PI / 2),
                         reads=[tt.k], writes=[Cc.k])

                ld(0)
                nst = 0
                for c in range(nch):
                    if c + 1 < nch:
                        ld(c + 1)
                    nb = ntc[c % 2]
                    use_tab = any(sp[2] for sp in fm_specs) and not norope and not notab
                    if use_tab and c == 0:
                        tables_dve(0)
                        tables_act(0)
                    if use_tab and c + 1 < nch:
                        tables_dve(c + 1)
                    Cc, Sc = Ccs[c % 2], Scs[c % 2]
                    pabanks = (pb[0], pb[1], pb[6])
                    pending = None

                    def rope_tail(p):
                        ti_, pa_, qb_, ot_, dst_, Cc_, Sc_ = p
                        pr = pb[2 + ti_ % 2]
                        t1, t2 = t1b[ti_ % 2], t2b[ti_ % 2]
                        mmgroup(pr.t[:, :], pr, [(rmat.t[:], qb_.t[:])], [rmat.k, qb_.k])
                        S.op("dve", lambda E: E.tensor_tensor(out=t1.t[:], in0=pa_.t[:], in1=Cc_.t[:], op=ALU.mult),
                             reads=[pa_.k, Cc_.k], writes=[t1.k])
                        S.op("dve", lambda E: E.tensor_tensor(out=t2.t[:], in0=pr.t[:], in1=Sc_.t[:], op=ALU.mult),
                             reads=[pr.k, Sc_.k], writes=[t2.k])
                        S.op(ROPE_ADD_ENG, lambda E: E.tensor_tensor(out=ot_.t[:], in0=t1.t[:], in1=t2.t[:],
                                                                     op=ALU.add),
                             reads=[t1.k, t2.k], writes=[ot_.k])
                        S.dma("sp", dst_, ot_.t[:], reads=[ot_.k])

                    for ti, (wcol, dest, rope) in enumerate([] if nofm else fm_specs[:FM_LIMIT]):
                        rope = rope and not norope and not tabonly
                        pa = pabanks[ti % 3]
                        mmgroup(pa.t[:, :], pa, [(wt.t[:, kc, wcol:wcol + 128], nb.t[:, kc, :]) for kc in range(8)],
                                [wt.k, nb.k])
                        ot = otb[nst % 3]
                        nst += 1
                        if rope:
                            qb = qbb[ti % 2]
                            S.op("act", lambda E, qb=qb, pa=pa: E.activation(out=qb.t[:], in_=pa.t[:], func=AF.Copy),
                                 reads=[pa.k], writes=[qb.k])
                        else:
                            S.op("act", lambda E, ot=ot, pa=pa: E.activation(out=ot.t[:], in_=pa.t[:], func=AF.Copy),
                                 reads=[pa.k], writes=[ot.k])
                            S.dma("sp", dest(c), ot.t[:], reads=[ot.k])
                        if pending is not None:
                            rope_tail(pending)
                        pending = (ti, pa, qb, ot, dest(c), Cc, Sc) if rope else None
                    if pending is not None:
                        rope_tail(pending)
                    if use_tab and c + 1 < nch:
                        tables_act(c + 1)
                    for (wcol, ncol, kind) in ([] if notm else tm_specs):
                        for tb in range(4):
                            pv = pb[4 + tb % 2]
                            mmgroup(pv.t[:, :ncol], pv,
                                    [(nb.t[:, kc, tb * 128:(tb + 1) * 128], wt.t[:, kc, wcol:wcol + ncol])
                                     for kc in range(8)], [wt.k, nb.k])
                            r0 = (c * 4 + tb) * 128
                            if kind == "vd":
                                st_ = vst[tb % 2]
                                S.op("act", lambda E, st_=st_, pv=pv: E.activation(out=st_.t[:], in_=pv.t[:],
                                                                                  func=AF.Copy),
                                     reads=[pv.k], writes=[st_.k])
                                S.dma("sp", VD[r0:r0 + 128, :], st_.t[:], reads=[st_.k])
                            elif kind == "vaug":
                                st_ = vau[tb % 2]
                                S.op("act", lambda E, st_=st_, pv=pv: E.activation(
                                    out=st_.t[:, :, 0:64], in_=pv.t[:, :].rearrange("p (h d) -> p h d", h=8),
                                    func=AF.Copy), reads=[pv.k], writes=[st_.k])
                                S.dma("sp", VAUG[r0:r0 + 128, :].rearrange("p (h d) -> p h d", h=8), st_.t[:],
                                      reads=[st_.k])
                            else:
                                st_ = iwst[tb % 2]
                                S.op("act", lambda E, st_=st_, pv=pv: E.activation(out=st_.t[:], in_=pv.t[:, 0:8],
                                                                                  func=AF.Copy),
                                     reads=[pv.k], writes=[st_.k])
                                S.dma("sp", IW[r0:r0 + 128, :], st_.t[:], reads=[st_.k])
                barrier()

        def fm_dest(T4, h):
            return lambda c: T4[h, :, c * 512:(c + 1) * 512]

        if stop_after >= 2:
          phase_proj(NTF, TF, pos_full,
                   [(0, C_DK, 512), (512, C_IK, 64), (576, C_IK, 64), (640, C_DV, 512)], 1152,
                   [(h * 128, fm_dest(KTD, h), True) for h in range(4)]
                   + [(512, (lambda c: IKT[:, c * 512:(c + 1) * 512]), True)],
                   [(640, 512, "vd")], "p1")
        if stop_after >= 3:
          phase_proj(NTF, TF, pos_full,
                   [(0, C_SK, 512), (512, C_SV, 512)], 1024,
                   [(h * 128, fm_dest(KTS, h), True) for h in range(4)],
                   [(512, 512, "vaug")], "p2")
        if stop_after >= 4:
          phase_proj(NTO, TO, pos_own,
                   [(0, C_DQ, 512), (512, C_SQ, 512), (1024, C_IQ, 512), (1536, C_MQ, 512), (2048, C_IW, 8)], 2056,
                   [(h * 128, fm_dest(QTD, h), True) for h in range(4)]
                   + [(512 + h * 128, fm_dest(QTS, h), True) for h in range(4)]
                   + [(1024 + h * 128, fm_dest(IQT, h), True) for h in range(4)]
                   + [(1536 + h * 128, fm_dest(MQT, h), False) for h in range(4)],
                   [(2048, 8, "iw")], "p3")
        def _phaseA():
            if stop_after >= 5:
                with ExitStack() as ph:
                    KT = sb(ph, "KTa", [128, 4, TF], BF16)
                    V = sb(ph, "Va", [128, 64, 512], BF16)
                    cmd = sb(ph, "cmd", [128, 8, 512], BF16)
                    lamb = sb(ph, "lamb", [128, 4, 64], F32)
                    lp = sb(ph, "lp", [128, 2, 64], F32)
                    ls = sb(ph, "ls", [128, 2], F32)
                    le = sb(ph, "le", [128, 2], F32)
                    neglam = sb(ph, "neglam", [128, 1], F32)
                    gsub = sb(ph, "gsub", [128, 1], F32)
                    gs08 = sb(ph, "gs08", [128, 1], F32)
                    for h in range(4):
                        S.dma("sp", KT.t[:, h, :], KTD[h], writes=[KT.k])
                    for q4 in range(4):
                        S.dma("sp", V.t[:, q4 * 16:(q4 + 1) * 16, :],
                              VD[q4 * 2048:(q4 + 1) * 2048, :].rearrange("(kb p) c -> p kb c", p=128), writes=[V.k])
                    wload(cmd.t[:], cm_diff_in, cmd.k)
                    S.dma("sp", lamb.t[:].rearrange("p a d -> p (a d)"), lam_in[0, :].partition_broadcast(128), writes=[lamb.k])
                    S.dma("sp", gsub.t[:], gsub_in, writes=[gsub.k])
                    for a in range(2):
                        S.op("dve", lambda E, a=a: E.tensor_tensor(out=lp.t[:, a, :], in0=lamb.t[:, 2 * a, :],
                                                                  in1=lamb.t[:, 2 * a + 1, :], op=ALU.mult),
                             reads=[lamb.k], writes=[lp.k])
                        S.op("dve", lambda E, a=a: E.reduce_sum(out=ls.t[:, a:a + 1], in_=lp.t[:, a, :], axis=AX.X),
                             reads=[lp.k], writes=[ls.k])
                    S.op("act", lambda E: E.activation(out=le.t[:], in_=ls.t[:], func=AF.Exp), reads=[ls.k], writes=[le.k])
                    S.op("dve", lambda E: E.tensor_tensor(out=neglam.t[:], in0=le.t[:, 1:2], in1=le.t[:, 0:1], op=ALU.subtract),
                         reads=[le.k], writes=[neglam.k])
                    S.op("dve", lambda E: E.tensor_scalar(out=neglam.t[:], in0=neglam.t[:], scalar1=-0.2, scalar2=None,
                                                          op0=ALU.add), reads=[neglam.k], writes=[neglam.k])
                    S.op("dve", lambda E: E.tensor_scalar(out=gs08.t[:], in0=gsub.t[:], scalar1=0.8, scalar2=None,
                                                          op0=ALU.mult), reads=[gsub.k], writes=[gs08.k])
                    QP = [[sb(ph, f"QPa{i}_{mp}", [128, 4, 512], BF16) for mp in range(2)] for i in range(2)]
                    for i in range(2):
                        for mp in range(2):
                            S.op("pool", lambda E, i=i, mp=mp: E.memset(QP[i][mp].t[:], 0.0), writes=[QP[i][mp].k])
                    pT = [sb(ph, f"pTa{i}", [128, 512], BF16) for i in range(4)]
                    rdb = sb(ph, "rdba", [128, 512], F32)
                    o1b = sb(ph, "o1b", [128, 512], F32)
                    tb_ = sb(ph, "tba", [128, 512], F32)
                    yb = sb(ph, "yba", [128, 512], F32)
                    ysq = sb(ph, "ysqa", [128, 512], BF16)
                    sdn = sb(ph, "sdna", [128, 512], F32)
                    rn = sb(ph, "rna", [128, 512], F32)
                    yob = [sb(ph, f"yoba{i}", [128, 512], BF16) for i in range(2)]
                    NG = 8 if NCH_LIMIT is None else NCH_LIMIT

                    def ldq(g):
                        for mp in range(2):
                            r0 = mp * 64
                            S.dma("sp", QP[g % 2][mp].t[r0:r0 + 64, :, :],
                                  QTD[:, r0:r0 + 64, g * 512:(g + 1) * 512].rearrange("h p t -> p h t"),
                                  writes=[QP[g % 2][mp].k])
                    ldq(0)
                    for g in range(NG):
                        if g + 1 < NG:
                            ldq(g + 1)
                        nkb = 8 * g + 8
                        for h in range(4):
                            for mp in range(2):
                                po, pd = pb[2 + mp], pb[4 + mp]
                                Q = QP[g % 2][mp]
                                lo_, hi_ = mp * 64, (mp + 1) * 64
                                psb = (pb[0], pb[1], pb[7])
                                for step in range(nkb + 2):
                                    if step < nkb:
                                        kb = step
                                        ps, pt = psb[kb % 3], pT[kb % 4]
                                        c0 = ((kb - 8 * g) // 2) * 128 if kb >= 8 * g else 0
                                        S.op("pe", lambda E, ps=ps, kb=kb, h=h, Q=Q, c0=c0: E.matmul(
                                            ps.t[:, c0:], KT.t[:, h, kb * 128:(kb + 1) * 128], Q.t[:, h, c0:],
                                            start=True, stop=True), reads=[KT.k, Q.k], writes=[ps.k])
                                        S.op("act", lambda E, ps=ps, pt=pt, c0=c0: E.activation(out=pt.t[:, c0:], in_=ps.t[:, c0:],
                                                                                               func=AF.Exp, scale=0.125),
                                             reads=[ps.k], writes=[pt.k])
                                        if kb >= 8 * g:
                                            z = kb - 8 * g
                                            S.op("dve", lambda E, pt=pt, z=z, c0=c0: E.tensor_tensor(
                                                out=pt.t[:, c0:], in0=pt.t[:, c0:], in1=cmd.t[:, z, c0:], op=ALU.mult),
                                                 reads=[pt.k, cmd.k], writes=[pt.k])
                                    if step >= 2:
                                        kb = step - 2
                                        pt = pT[kb % 4]
                                        c0 = ((kb - 8 * g) // 2) * 128 if kb >= 8 * g else 0
                                        S.op("pe", lambda E, po=po, pt=pt, kb=kb, h=h, nkb=nkb, c0=c0: E.matmul(
                                            po.t[:, c0:], V.t[:, kb, h * 128:(h + 1) * 128], pt.t[:, c0:],
                                            start=(kb == 0), stop=(kb == nkb - 1)), reads=[V.k, pt.k], writes=[po.k],
                                            signal=False)
                                        S.op("pe", lambda E, pd=pd, pt=pt, kb=kb, nkb=nkb, c0=c0: E.matmul(
                                            pd.t[:, c0:], ones.t[:], pt.t[:, c0:], start=(kb == 0), stop=(kb == nkb - 1)),
                                            reads=[ones.k, pt.k], writes=[pd.k])
                                S.op("dve", lambda E, pd=pd: E.reciprocal(out=rdb.t[:], in_=pd.t[:]), reads=[pd.k], writes=[rdb.k])
                                if mp == 0:
                                    S.op("dve", lambda E, po=po: E.tensor_tensor(out=o1b.t[:], in0=po.t[:], in1=rdb.t[:],
                                                                                op=ALU.mult),
                                         reads=[po.k, rdb.k], writes=[o1b.k])
                                else:
                                    S.op("dve", lambda E, po=po: E.tensor_tensor(out=tb_.t[:], in0=po.t[:], in1=rdb.t[:],
                                                                                op=ALU.mult),
                                         reads=[po.k, rdb.k], writes=[tb_.k])
                            yo = yob[h % 2]
                            S.op("dve", lambda E: E.scalar_tensor_tensor(out=yb.t[:], in0=tb_.t[:], scalar=neglam.t[:, 0:1],
                                                                         in1=o1b.t[:], op0=ALU.mult, op1=ALU.add),
                                 reads=[tb_.k, neglam.k, o1b.k], writes=[yb.k])
                            S.op("act", lambda E: E.activation(out=ysq.t[:], in_=yb.t[:], func=AF.Square),
                                 reads=[yb.k], writes=[ysq.k])
                            mmgroup(pb[6].t[:, :], pb[6], [(ones.t[:], ysq.t[:])], [ones.k, ysq.k])
                            S.op("act", lambda E: E.activation(out=sdn.t[:], in_=pb[6].t[:], func=AF.Sqrt, scale=1.0 / 128,
                                                               bias=EPS), reads=[pb[6].k], writes=[sdn.k])
                            S.op("dve", lambda E: E.reciprocal(out=rn.t[:], in_=sdn.t[:]), reads=[sdn.k], writes=[rn.k])
                            S.op("dve", lambda E, yo=yo: E.scalar_tensor_tensor(out=yo.t[:], in0=yb.t[:], scalar=gs08.t[:, 0:1],
                                                                                in1=rn.t[:], op0=ALU.mult, op1=ALU.mult),
                                 reads=[yb.k, gs08.k, rn.k], writes=[yo.k])
                            S.dma("sp", YD[h, :, g * 512:(g + 1) * 512], yo.t[:], reads=[yo.k])
                    barrier()

        _phaseA()
        def _phaseB():
            if stop_after >= 6:
                with ExitStack() as ph:
                    KT = sb(ph, "KTb", [128, 4, TF], BF16)
                    IK = sb(ph, "IKb", [128, TF], BF16)
                    scores = [sb(ph, f"score{i}", [128, TF], F32) for i in range(2)]
                    maskbs = [sb(ph, f"maskb{i}", [128, TF], FP8) for i in range(2)]
                    NV = 8
                    Vr = [sb(ph, f"Vr{i}", [128, 1024], BF16) for i in range(NV)]
                    cmi = sb(ph, "cmi", [128, 256], F32)
                    for h in range(4):
                        S.dma("sp", KT.t[:, h, :], KTS[h], writes=[KT.k])
                    S.dma("sp", IK.t[:], IKT, writes=[IK.k])
                    S.dma("sp", cmi.t[:], cm_idx_in, writes=[cmi.k])
                    QS = [[sb(ph, f"QSb{i}_{par}", [128, 4, 512], BF16) for par in range(2)] for i in range(1)]
                    QI = [[sb(ph, f"QIb{i}_{par}", [128, 4, 512], BF16) for par in range(2)] for i in range(1)]
                    for i in range(1):
                        for par in range(2):
                            S.op("pool", lambda E, i=i, par=par: E.memset(QS[i][par].t[:], 0.0), writes=[QS[i][par].k])
                            S.op("pool", lambda E, i=i, par=par: E.memset(QI[i][par].t[:], 0.0), writes=[QI[i][par].k])
                    IWs = [sb(ph, f"IWb{i}", [128, 4, 8], F32) for i in range(1)]
                    Dh = sb(ph, "Dh", [128, 8, 128], BF16)
                    Rb = [sb(ph, f"Rb{i}", [128, 512], BF16) for i in range(3)]
                    pT = [sb(ph, f"pTb{i}", [128, 512], BF16) for i in range(4)]
                    rmax = sb(ph, "rmax", [128, 1], F32)
                    rmin = sb(ph, "rmin", [128, 1], F32)
                    span = sb(ph, "span", [128, 1], F32)
                    lo = sb(ph, "lo", [128, 1], F32)
                    mid = sb(ph, "mid", [128, 1], F32)
                    cnt = sb(ph, "cnt", [128, 1], F32)
                    gs = sb(ph, "gs", [128, 1], F32)
                    rdb = sb(ph, "rdbb", [128, 128], F32)
                    ysb = [sb(ph, f"ysb{i}", [128, 4, 128], BF16) for i in range(2)]
                    NG = 8 if NCH_LIMIT is None else NCH_LIMIT
                    NIT = 14
                    CSC = (8.0 ** -0.5) * (64.0 ** -0.5)

                    def ldq_i(g):
                        for par in range(2):
                            r0 = par * 64
                            S.dma("sp", QI[0][par].t[r0:r0 + 64, :, :],
                                  IQT[:, r0:r0 + 64, g * 512:(g + 1) * 512].rearrange("h p t -> p h t"), writes=[QI[0][par].k])
                        S.dma("sp", IWs[0].t[:], IW[g * 512:(g + 1) * 512, :].rearrange("(b p) h -> p b h", p=128),
                              writes=[IWs[0].k])

                    def ldq_s(g):
                        for par in range(2):
                            r0 = par * 64
                            S.dma("sp", QS[0][par].t[r0:r0 + 64, :, :],
                                  QTS[:, r0:r0 + 64, g * 512:(g + 1) * 512].rearrange("h p t -> p h t"), writes=[QS[0][par].k])
                    vcount = 0
                    blocks = [(g, blk) for g in range(NG) for blk in range(4)]

                    def stage1a(t):
                        g, blk = blocks[t]
                        if blk == 0:
                            ldq_i(g)
                        Qi, Iw = QI[0], IWs[0]
                        score = scores[t % 2]
                        m = 4 * g + blk
                        nkb = 2 * m + 2
                        n = nkb * 128
                        qs = slice(blk * 128, (blk + 1) * 128)
                        for h in range(8):
                            S.op("dve", lambda E, h=h, Iw=Iw, blk=blk: E.tensor_scalar(
                                out=Dh.t[:, h, :], in0=ident.t[:], scalar1=Iw.t[:, blk, h:h + 1], scalar2=CSC,
                                op0=ALU.mult, op1=ALU.mult), reads=[ident.k, Iw.k], writes=[Dh.k])
                        yield
                        nch = (nkb + 3) // 4
                        psc = pb[2]
                        items = [(c, h) for c in range(nch) for h in range(8)]
                        for idx in range(len(items) + 1):
                            if idx < len(items):
                                c, h = items[idx]
                                w = min(512, n - c * 512)
                                pa, rb = pb[h % 2], Rb[h % 3]
                                Qih = Qi[h % 2]
                                S.op("pe", lambda E, pa=pa, h=h, c=c, w=w, Qih=Qih, qs=qs: E.matmul(
                                    pa.t[:, :w], Qih.t[:, h // 2, qs], IK.t[:, c * 512:c * 512 + w],
                                    start=True, stop=True), reads=[Qih.k, IK.k], writes=[pa.k])
                                S.op("act", lambda E, pa=pa, rb=rb, w=w: E.activation(out=rb.t[:, :w], in_=pa.t[:, :w],
                                                                                     func=AF.Relu),
                                     reads=[pa.k], writes=[rb.k])
                            if idx >= 1:
                                c, h = items[idx - 1]
                                w = min(512, n - c * 512)
                                rb = Rb[h % 3]
                                S.op("pe", lambda E, psc=psc, h=h, rb=rb, w=w: E.matmul(
                                    psc.t[:, :w], Dh.t[:, h, :], rb.t[:, :w], start=(h == 0), stop=(h == 7)),
                                    reads=[Dh.k, rb.k], writes=[psc.k], signal=(h == 7))
                                if h == 7:
                                    S.op("act", lambda E, psc=psc, c=c, w=w, score=score: E.activation(
                                        out=score.t[:, c * 512:c * 512 + w], in_=psc.t[:, :w], func=AF.Copy),
                                        reads=[psc.k], writes=[score.k])
                            if idx % 8 == 7:
                                yield

                    def stage1b(t):
                        g, blk = blocks[t]
                        score = scores[t % 2]
                        maskb = maskbs[t % 2]
                        m = 4 * g + blk
                        nkb = 2 * m + 2
                        n = nkb * 128
                        S.op("dve", lambda E, n=n: E.tensor_reduce(out=rmax.t[:], in_=score.t[:, :n], axis=AX.X, op=ALU.max),
                             reads=[score.k], writes=[rmax.k])
                        S.op("dve", lambda E, n=n: E.tensor_reduce(out=rmin.t[:], in_=score.t[:, :n], axis=AX.X, op=ALU.min),
                             reads=[score.k], writes=[rmin.k])
                        S.op("dve", lambda E, n=n: E.tensor_tensor(out=score.t[:, n - 256:n], in0=score.t[:, n - 256:n],
                                                                  in1=cmi.t[:], op=ALU.add),
                             reads=[score.k, cmi.k], writes=[score.k])
                        S.op("dve", lambda E: E.tensor_tensor(out=span.t[:], in0=rmax.t[:], in1=rmin.t[:], op=ALU.subtract),
                             reads=[rmax.k, rmin.k], writes=[span.k])
                        S.op("dve", lambda E: E.tensor_scalar(out=span.t[:], in0=span.t[:], scalar1=2e-4, scalar2=None,
                                                              op0=ALU.add), reads=[span.k], writes=[span.k])
                        S.op("dve", lambda E: E.tensor_scalar(out=lo.t[:], in0=rmin.t[:], scalar1=-1e-4, scalar2=None,
                                                              op0=ALU.add), reads=[rmin.k], writes=[lo.k])
                        yield
                        for it in range(NIT):
                            f = 2.0 ** -(it + 1)
                            S.op("dve", lambda E, f=f: E.scalar_tensor_tensor(out=mid.t[:], in0=span.t[:], scalar=f,
                                                                              in1=lo.t[:], op0=ALU.mult, op1=ALU.add),
                                 reads=[span.k, lo.k], writes=[mid.k])
                            S.op("dve", lambda E, n=n, maskb=maskb: E.tensor_scalar(
                                out=maskb.t[:, :n], in0=score.t[:, :n], scalar1=mid.t[:, 0:1], scalar2=0.0, op0=ALU.is_ge,
                                op1=ALU.add, accum_out=cnt.t[:], saturate=False), reads=[score.k, mid.k], writes=[maskb.k, cnt.k])
                            S.op("dve", lambda E, f=f: E.tensor_scalar(out=gs.t[:], in0=cnt.t[:], scalar1=255.5, scalar2=f,
                                                                      op0=ALU.is_ge, op1=ALU.mult),
                                 reads=[cnt.k], writes=[gs.k])
                            S.op("dve", lambda E: E.scalar_tensor_tensor(out=lo.t[:], in0=gs.t[:], scalar=span.t[:, 0:1],
                                                                         in1=lo.t[:], op0=ALU.mult, op1=ALU.add),
                                 reads=[gs.k, span.k, lo.k], writes=[lo.k])
                            yield
                        S.op("dve", lambda E, n=n, maskb=maskb: E.tensor_scalar(
                            out=maskb.t[:, :n], in0=score.t[:, :n], scalar1=lo.t[:, 0:1], scalar2=-30000.0, op0=ALU.is_lt,
                            op1=ALU.mult, saturate=False), reads=[score.k, lo.k], writes=[maskb.k])

                    def stage2(t):
                        nonlocal vcount
                        g, blk = blocks[t]
                        if blk == 0:
                            ldq_s(g)
                        Qs = QS[0]
                        maskb = maskbs[t % 2]
                        m = 4 * g + blk
                        nkb = 2 * m + 2
                        qs = slice(blk * 128, (blk + 1) * 128)
                        for bnk in (4, 5):
                            mmgroup(pb[bnk].t[:, :], pb[bnk], [(zeros.t[:, 0:128], zeros.t[:])], [zeros.k])
                        ngr = (nkb + 3) // 4
                        slots = {}

                        def ldv(kg):
                            nonlocal vcount
                            for kb in range(kg * 4, min(nkb, kg * 4 + 4)):
                                sl = vcount % NV
                                vcount += 1
                                slots[kb] = sl
                                S.dma("sp", Vr[sl].t[:], VAUG[kb * 128:(kb + 1) * 128, :], writes=[Vr[sl].k])
                        ldv(0)
                        yield
                        psb = (pb[6], pb[7], pb[3])
                        items = [(kg, h) for kg in range(ngr) for h in range(8)]
                        for idx in range(len(items) + 2):
                            if idx < len(items):
                                kg, h = items[idx]
                                if h == 2 and kg + 1 < ngr:
                                    ldv(kg + 1)
                                kbs = list(range(kg * 4, min(nkb, kg * 4 + 4)))
                                wg = len(kbs) * 128
                                ps, pt = psb[idx % 3], pT[idx % 4]
                                Qsh = Qs[h % 2]
                                for i, kb in enumerate(kbs):
                                    S.op("pe", lambda E, ps=ps, i=i, kb=kb, h=h, Qsh=Qsh, qs=qs: E.matmul(
                                        ps.t[:, i * 128:(i + 1) * 128], KT.t[:, h // 2, kb * 128:(kb + 1) * 128],
                                        Qsh.t[:, h // 2, qs], start=True, stop=False, skip_group_check=True),
                                        reads=[KT.k, Qsh.k], writes=[ps.k], signal=False)
                                    S.op("pe", lambda E, ps=ps, i=i, kb=kb, maskb=maskb: E.matmul(
                                        ps.t[:, i * 128:(i + 1) * 128], maskb.t[:, kb * 128:(kb + 1) * 128], ident.t[:],
                                        start=False, stop=True, skip_group_check=True),
                                        reads=[maskb.k, ident.k], writes=[ps.k], signal=(i == len(kbs) - 1))
                                S.op("act", lambda E, ps=ps, pt=pt, wg=wg: E.activation(
                                    out=pt.t[:, :wg], in_=ps.t[:, :wg], func=AF.Exp, scale=0.125),
                                    reads=[ps.k], writes=[pt.k])
                            if idx >= 2:
                                kg, h = items[idx - 2]
                                kbs = list(range(kg * 4, min(nkb, kg * 4 + 4)))
                                pt = pT[(idx - 2) % 4]
                                acc = pb[4 + h // 4]
                                a0 = (h % 4) * 128
                                for i, kb in enumerate(kbs):
                                    vr = Vr[slots[kb]]
                                    S.op("pe", lambda E, acc=acc, a0=a0, vr=vr, h=h, pt=pt, i=i: E.matmul(
                                        acc.t[:, a0:a0 + 128], vr.t[:, h * 128:(h + 1) * 128],
                                        pt.t[:, i * 128:(i + 1) * 128], start=False, stop=False, skip_group_check=True),
                                        reads=[vr.k, pt.k], writes=[acc.k], signal=(i == len(kbs) - 1))
                            if idx % 8 == 7:
                                yield
                        yt = ysb[m % 2]
                        for h in range(8):
                            acc = pb[4 + h // 4]
                            a0 = (h % 4) * 128
                            S.op("dve", lambda E, acc=acc, a0=a0: E.reciprocal(out=rdb.t[0:64, :],
                                                                              in_=acc.t[64:128, a0:a0 + 128]),
                                 reads=[acc.k], writes=[rdb.k])
                            p0 = (h % 2) * 64
                            S.op("dve", lambda E, acc=acc, a0=a0, p0=p0, h=h, yt=yt: E.tensor_tensor(
                                out=yt.t[p0:p0 + 64, h // 2, :], in0=acc.t[0:64, a0:a0 + 128], in1=rdb.t[0:64, :],
                                op=ALU.mult), reads=[acc.k, rdb.k], writes=[yt.k])
                        S.dma("sp", YS[:, :, m * 128:(m + 1) * 128].rearrange("t p q -> p t q"), yt.t[:], reads=[yt.k])

                    nb_ = len(blocks)

                    def run_all(gen):
                        for _ in gen:
                            pass

                    def interleave(fast, slow, ratio):
                        fast = [g for g in fast if g is not None]
                        k = 0
                        while fast or slow is not None:
                            for g in list(fast):
                                try:
                                    next(g)
                                except StopIteration:
                                    fast.remove(g)
                            k += 1
                            if slow is not None and (k % ratio == 0 or not fast):
                                try:
                                    next(slow)
                                except StopIteration:
                                    slow = None
                    run_all(stage1a(0))
                    if nb_ > 1:
                        run_all(stage1a(1))
                    run_all(stage1b(0))
                    for t in range(nb_):
                        if t + 2 < nb_:
                            run_all(stage1a(t + 2))
                        if t + 1 < nb_:
                            run_all(stage1b(t + 1))
                        run_all(stage2(t))
                    barrier()

        _phaseB()
        def _phaseC():
            if stop_after >= 7:
                with ExitStack() as ph:
                    wg = sb(ph, "wgC", [128, 8, 3072], BF16)
                    wb = sb(ph, "wbC", [128, 12, 1024], BF16)
                    wo = sb(ph, "woC", [128, 8, 1024], BF16)
                    bg = sb(ph, "bgC", [128, 24], F32)
                    gf8 = sb(ph, "gf8C", [128, 8], F32)
                    mkT = sb(ph, "mkT", [128, 4, 256], BF16)
                    mv = sb(ph, "mvC", [128, 2, 512], BF16)
                    wload(wg.t[:], w_in[:, C_GL:C_GL + 3072].rearrange("(k p) c -> p k c", p=128), wg.k)
                    for br in range(3):
                        wload(wb.t[:, br * 4:(br + 1) * 4, :], w_br[br].rearrange("(k p) c -> p k c", p=128), wb.k)
                    wload(wo.t[:], w_out.rearrange("(k p) c -> p k c", p=128), wo.k)
                    S.dma("sp", bg.t[:], b_gate24, writes=[bg.k])
                    S.dma("sp", gf8.t[:], gffn8, writes=[gf8.k])
                    xs = [sb(ph, f"xsC{i}", [128, 8, 512], F32) for i in range(1)]
                    sq = sb(ph, "sqC", [128, 8, 512], BF16)
                    ntc = [sb(ph, f"ntC{i}", [128, 8, 512], BF16) for i in range(1)]
                    sd = sb(ph, "sdC", [128, 512], F32)
                    rs = sb(ph, "rsC", [128, 512], F32)
                    with ExitStack() as ph2:
                        wm = sb(ph2, "wmC", [128, 8, 1024], BF16)
                        gm8 = sb(ph2, "gm8C", [128, 8], F32)
                        wload(wm.t[:], w_mem_kv.rearrange("(k p) c -> p k c", p=128), wm.k)
                        S.dma("sp", gm8.t[:], gmem8, writes=[gm8.k])
                        S.dma("sp", xs[0].t[:, :, 0:256], memT.rearrange("(k p) t -> p k t", p=128), writes=[xs[0].k])
                        norm_chunk(xs[0], sq, ntc[0], sd, rs, gm8, 256)
                        for hm in range(4):
                            mmgroup(pb[0].t[:, :256], pb[0],
                                    [(wm.t[:, kc, hm * 128:(hm + 1) * 128], ntc[0].t[:, kc, 0:256]) for kc in range(8)],
                                    [wm.k, ntc[0].k])
                            S.op("act", lambda E, hm=hm: E.activation(out=mkT.t[:, hm, :], in_=pb[0].t[:, :256], func=AF.Copy),
                                 reads=[pb[0].k], writes=[mkT.k])
                        for mb in range(2):
                            mmgroup(pb[1].t[:, :], pb[1],
                                    [(ntc[0].t[:, kc, mb * 128:(mb + 1) * 128], wm.t[:, kc, 512:1024]) for kc in range(8)],
                                    [wm.k, ntc[0].k])
                            S.op("act", lambda E, mb=mb: E.activation(out=mv.t[:, mb, :], in_=pb[1].t[:, :], func=AF.Copy),
                                 reads=[pb[1].k], writes=[mv.k])
                        barrier()
                    mq = [sb(ph, f"mqC{i}", [128, 4, 512], BF16) for i in range(1)]
                    yd = [sb(ph, f"ydC{i}", [128, 4, 512], BF16) for i in range(1)]
                    ys = [sb(ph, f"ysC{i}", [128, 4, 512], BF16) for i in range(1)]
                    ym = sb(ph, "ymC", [128, 4, 512], BF16)
                    xt = [sb(ph, f"xtC{i}", [128, 4, 1024], F32) for i in range(1)]
                    pT = [sb(ph, f"pTC{i}", [128, 512], BF16) for i in range(2)]
                    rdb = sb(ph, "rdbC", [128, 512], F32)
                    gate = [sb(ph, f"gateC{i}", [128, 512], F32) for i in range(2)]
                    macc = sb(ph, "maccC", [128, 512], F32)
                    mtmp = sb(ph, "mtmpC", [128, 512], F32)
                    mg = sb(ph, "mgC", [128, 8, 512], BF16)
                    xn = sb(ph, "xnC", [128, 8, 512], BF16)
                    NCC = 8 if NCH_LIMIT is None else NCH_LIMIT

                    def ld_mq(c):
                        S.dma("sp", mq[0].t[:], MQT[:, :, c * 512:(c + 1) * 512].rearrange("h p t -> p h t"), writes=[mq[0].k])

                    def ld_merge(c):
                        cs = slice(c * 512, (c + 1) * 512)
                        S.dma("sp", ntc[0].t[:], NTO[:, :, cs].rearrange("k p t -> p k t"), writes=[ntc[0].k])
                        S.dma("sp", yd[0].t[:], YD[:, :, cs].rearrange("h p t -> p h t"), writes=[yd[0].k])
                        S.dma("sp", ys[0].t[:], YS[:, :, cs].rearrange("h p t -> p h t"), writes=[ys[0].k])

                    def ld_xs(c):
                        S.dma("sp", xs[0].t[:], xT_own[:, c * 512:(c + 1) * 512].rearrange("(k p) t -> p k t", p=128),
                              writes=[xs[0].k])

                    def ld_xt(c):
                        S.dma("sp", xt[0].t[:], x_own[c * 512:(c + 1) * 512, :].rearrange("(b p) d -> p b d", p=128),
                              writes=[xt[0].k])
                    ld_mq(0)
                    ld_merge(0)
                    ld_xs(0)
                    ld_xt(0)
                    for c in range(NCC):
                        i = 0
                        nxt = c + 1 < NCC
                        nb, mqc, ydc, ysc, xsc, xtc = ntc[i], mq[i], yd[i], ys[i], xs[i], xt[i]
                        for hm in range(4):
                            for mb in range(2):
                                ps, pt = pb[mb], pT[mb]
                                mmgroup(ps.t[:, :], ps, [(mkT.t[:, hm, mb * 128:(mb + 1) * 128], mqc.t[:, hm, :])],
                                        [mkT.k, mqc.k])
                                S.op("act", lambda E, ps=ps, pt=pt: E.activation(out=pt.t[:], in_=ps.t[:], func=AF.Exp,
                                                                                scale=128.0 ** -0.5),
                                     reads=[ps.k], writes=[pt.k])
                            mmgroup(pb[2].t[:, :], pb[2],
                                    [(mv.t[:, mb, hm * 128:(hm + 1) * 128], pT[mb].t[:]) for mb in range(2)],
                                    [mv.k, pT[0].k, pT[1].k])
                            mmgroup(pb[3].t[:, :], pb[3], [(ones.t[:], pT[mb].t[:]) for mb in range(2)],
                                    [ones.k, pT[0].k, pT[1].k])
                            S.op("dve", lambda E: E.reciprocal(out=rdb.t[:], in_=pb[3].t[:]), reads=[pb[3].k], writes=[rdb.k])
                            S.op("dve", lambda E, hm=hm: E.tensor_tensor(out=ym.t[:, hm, :], in0=pb[2].t[:], in1=rdb.t[:],
                                                                        op=ALU.mult),
                                 reads=[pb[2].k, rdb.k], writes=[ym.k])
                        if nxt:
                            ld_mq(c + 1)
                        ysrc = (ydc, ysc, ym)
                        for ct in range(8):
                            for br in range(3):
                                pg, pbr, gt = pb[4 + br % 2], pb[6 + br % 2], gate[br % 2]
                                col = br * 1024 + ct * 128
                                mmgroup(pg.t[:, :], pg, [(wg.t[:, kc, col:col + 128], nb.t[:, kc, :]) for kc in range(8)],
                                        [wg.k, nb.k])
                                S.op("act", lambda E, pg=pg, gt=gt, br=br, ct=ct: E.activation(
                                    out=gt.t[:], in_=pg.t[:], func=AF.Sigmoid, bias=bg.t[:, br * 8 + ct:br * 8 + ct + 1]),
                                    reads=[pg.k, bg.k], writes=[gt.k])
                                ysb_ = ysrc[br]
                                mmgroup(pbr.t[:, :], pbr,
                                        [(wb.t[:, br * 4 + kc, ct * 128:(ct + 1) * 128], ysb_.t[:, kc, :]) for kc in range(4)],
                                        [wb.k, ysb_.k])
                                if br == 0:
                                    S.op("dve", lambda E, pbr=pbr, gt=gt: E.tensor_tensor(out=macc.t[:], in0=pbr.t[:],
                                                                                         in1=gt.t[:], op=ALU.mult),
                                         reads=[pbr.k, gt.k], writes=[macc.k])
                                else:
                                    S.op("dve", lambda E, pbr=pbr, gt=gt: E.tensor_tensor(out=mtmp.t[:], in0=pbr.t[:],
                                                                                         in1=gt.t[:], op=ALU.mult),
                                         reads=[pbr.k, gt.k], writes=[mtmp.k])
                                    if br == 1:
                                        S.op("pool", lambda E: E.tensor_tensor(out=macc.t[:], in0=macc.t[:], in1=mtmp.t[:],
                                                                               op=ALU.add),
                                             reads=[macc.k, mtmp.k], writes=[macc.k])
                                    else:
                                        S.op("pool", lambda E, ct=ct: E.tensor_tensor(out=mg.t[:, ct, :], in0=macc.t[:],
                                                                                      in1=mtmp.t[:], op=ALU.add),
                                             reads=[macc.k, mtmp.k], writes=[mg.k])
                        if nxt:
                            ld_merge(c + 1)
                        for ct in range(8):
                            po = pb[ct % 2]
                            mmgroup(po.t[:, :], po, [(wo.t[:, kc, ct * 128:(ct + 1) * 128], mg.t[:, kc, :]) for kc in range(8)],
                                    [wo.k, mg.k])
                            S.op("dve", lambda E, po=po, ct=ct, xsc=xsc: E.tensor_tensor(out=xsc.t[:, ct, :], in0=po.t[:],
                                                                                        in1=xsc.t[:, ct, :], op=ALU.add),
                                 reads=[po.k, xsc.k], writes=[xsc.k])
                        norm_chunk(xsc, sq, xn, sd, rs, gf8, 512)
                        S.dma("sp", XNT[:, :, c * 512:(c + 1) * 512].rearrange("k p t -> p k t"), xn.t[:], reads=[xn.k])
                        if nxt:
                            ld_xs(c + 1)
                        for tb in range(4):
                            for hf in range(2):
                                po = pb[2 + (tb * 2 + hf) % 2]
                                mmgroup(po.t[:, :], po,
                                        [(mg.t[:, kc, tb * 128:(tb + 1) * 128], wo.t[:, kc, hf * 512:(hf + 1) * 512])
                                         for kc in range(8)], [wo.k, mg.k])
                                S.op("dve", lambda E, po=po, tb=tb, hf=hf, xtc=xtc: E.tensor_tensor(
                                    out=xtc.t[:, tb, hf * 512:(hf + 1) * 512], in0=po.t[:],
                                    in1=xtc.t[:, tb, hf * 512:(hf + 1) * 512], op=ALU.add),
                                    reads=[po.k, xtc.k], writes=[xtc.k])
                        S.dma("sp", H2[c * 512:(c + 1) * 512, :].rearrange("(b p) d -> p b d", p=128), xtc.t[:], reads=[xtc.k])
                        if nxt:
                            ld_xt(c + 1)
                    barrier()

        _phaseC()
        def _phaseD():
            if stop_after >= 8:
                with ExitStack() as ph:
                    acc = sb(ph, "accD", [128, 16, 1024], F32)
                    xnhs = [sb(ph, f"xnhD{i}", [128, 8, 2048], BF16) for i in range(2)]
                    w1 = [sb(ph, f"w1D{i}", [128, 8, 1024], BF16) for i in range(2)]
                    w2 = [sb(ph, f"w2D{i}", [128, 4, 1024], BF16) for i in range(2)]
                    wr = sb(ph, "wrD", [128, 8, 20], BF16)
                    brb = sb(ph, "brD", [128, 20], F32)
                    gfb = sb(ph, "gfbD", [128, 1024], F32)
                    cw = sb(ph, "cwD", [128, 16, 16], F32)
                    hid = [sb(ph, f"hidD{i}", [128, 4, 512], BF16) for i in range(2)]
                    sg = [sb(ph, f"sgD{i}", [128, 512], F32) for i in range(2)]
                    lg = sb(ph, "lgD", [128, 20], F32)
                    elm = sb(ph, "elmD", [128, 16], F32)
                    elm2 = sb(ph, "elm2D", [128, 16], F32)
                    oh1 = sb(ph, "oh1D", [128, 16], F32)
                    oh2 = sb(ph, "oh2D", [128, 16], F32)
                    ohg = sb(ph, "ohgD", [128, 4], F32)
                    egj = sb(ph, "egjD", [128, 4], F32)
                    sm = {nm: sb(ph, nm + "D", [128, 1], F32) for nm in
                          ("gmax", "ngmax", "sumg", "pgrp", "m1", "m2", "dd", "ed", "den", "wa", "wb2", "c1", "c2",
                           "ssum", "sdv", "rsv")}
                    junk = sb(ph, "junkD", [128, 1024], BF16)
                    ot = [sb(ph, f"otD{i}", [128, 1024], F32) for i in range(2)]
                    wload(wr.t[:], w_r.rearrange("(k p) c -> p k c", p=128), wr.k)
                    S.dma("sp", brb.t[:], b_r[0, :].partition_broadcast(128), writes=[brb.k])
                    S.dma("sp", gfb.t[:], gfin[0, :].partition_broadcast(128), writes=[gfb.k])
                    NHALF = 2 if NCH_LIMIT is None else 1
                    NE = 16
                    BIGN = 1e30

                    def small(eng, fn, reads, writes):
                        S.op(eng, fn, reads=[r.k for r in reads], writes=[w.k for w in writes])

                    ecount = 0
                    for hf in range(NHALF):
                        S.dma("sp", xnhs[hf].t[:], XNT[:, :, hf * 2048:(hf + 1) * 2048].rearrange("k p t -> p k t"),
                              writes=[xnhs[hf].k])
                    for hf in range(NHALF):
                        t0 = hf * 2048
                        xnh = xnhs[hf]
                        S.dma("sp", acc.t[:], H2[t0:t0 + 2048, :].rearrange("(b p) d -> p b d", p=128), writes=[acc.k])
                        def router(tb):
                            mmgroup(pb[6].t[:, 0:20], pb[6],
                                    [(xnh.t[:, kc, tb * 128:(tb + 1) * 128], wr.t[:, kc, :]) for kc in range(8)], [xnh.k, wr.k])
                            small("dve", lambda E: E.tensor_tensor(out=lg.t[:], in0=pb[6].t[:, 0:20], in1=brb.t[:], op=ALU.add),
                                  [pb[6], brb], [lg])
                            small("dve", lambda E: E.tensor_reduce(out=sm["gmax"].t[:], in_=lg.t[:, 0:4], axis=AX.X, op=ALU.max),
                                  [lg], [sm["gmax"]])
                            small("dve", lambda E: E.tensor_scalar(out=ohg.t[:], in0=lg.t[:, 0:4], scalar1=sm["gmax"].t[:, 0:1],
                                                                   scalar2=BIGN, op0=ALU.is_lt, op1=ALU.mult),
                                  [lg, sm["gmax"]], [ohg])
                            small("dve", lambda E: E.tensor_scalar(out=sm["ngmax"].t[:], in0=sm["gmax"].t[:], scalar1=-1.0,
                                                                   scalar2=None, op0=ALU.mult), [sm["gmax"]], [sm["ngmax"]])
                            small("act", lambda E: E.activation(out=egj.t[:], in_=lg.t[:, 0:4], func=AF.Exp,
                                                                bias=sm["ngmax"].t[:, 0:1], accum_out=sm["sumg"].t[:]),
                                  [lg, sm["ngmax"]], [egj, sm["sumg"]])
                            small("dve", lambda E: E.reciprocal(out=sm["pgrp"].t[:], in_=sm["sumg"].t[:]), [sm["sumg"]],
                                  [sm["pgrp"]])
                            for gi in range(4):
                                small("dve", lambda E, gi=gi: E.tensor_scalar(
                                    out=elm.t[:, gi * 4:(gi + 1) * 4], in0=lg.t[:, 4 + gi * 4:8 + gi * 4],
                                    scalar1=ohg.t[:, gi:gi + 1], scalar2=None, op0=ALU.subtract), [lg, ohg], [elm])
                            small("dve", lambda E: E.tensor_reduce(out=sm["m1"].t[:], in_=elm.t[:], axis=AX.X, op=ALU.max),
                                  [elm], [sm["m1"]])
                            small("dve", lambda E: E.tensor_scalar(out=oh1.t[:], in0=elm.t[:], scalar1=sm["m1"].t[:, 0:1],
                                                                   scalar2=None, op0=ALU.is_ge), [elm, sm["m1"]], [oh1])
                            small("dve", lambda E: E.scalar_tensor_tensor(out=elm2.t[:], in0=oh1.t[:], scalar=-BIGN, in1=elm.t[:],
                                                                          op0=ALU.mult, op1=ALU.add), [oh1, elm], [elm2])
                            small("dve", lambda E: E.tensor_reduce(out=sm["m2"].t[:], in_=elm2.t[:], axis=AX.X, op=ALU.max),
                                  [elm2], [sm["m2"]])
                            small("dve", lambda E: E.tensor_scalar(out=oh2.t[:], in0=elm2.t[:], scalar1=sm["m2"].t[:, 0:1],
                                                                   scalar2=None, op0=ALU.is_ge), [elm2, sm["m2"]], [oh2])
                            small("dve", lambda E: E.tensor_tensor(out=sm["dd"].t[:], in0=sm["m2"].t[:], in1=sm["m1"].t[:],
                                                                   op=ALU.subtract), [sm["m1"], sm["m2"]], [sm["dd"]])
                            small("act", lambda E: E.activation(out=sm["ed"].t[:], in_=sm["dd"].t[:], func=AF.Exp),
                                  [sm["dd"]], [sm["ed"]])
                            small("dve", lambda E: E.tensor_scalar(out=sm["den"].t[:], in0=sm["ed"].t[:], scalar1=1.0,
                                                                   scalar2=None, op0=ALU.add), [sm["ed"]], [sm["den"]])
                            small("dve", lambda E: E.reciprocal(out=sm["wa"].t[:], in_=sm["den"].t[:]), [sm["den"]], [sm["wa"]])
                            small("dve", lambda E: E.tensor_tensor(out=sm["wb2"].t[:], in0=sm["ed"].t[:], in1=sm["wa"].t[:],
                                                                   op=ALU.mult), [sm["ed"], sm["wa"]], [sm["wb2"]])
                            small("dve", lambda E: E.tensor_tensor(out=sm["c1"].t[:], in0=sm["wa"].t[:], in1=sm["pgrp"].t[:],
                                                                   op=ALU.mult), [sm["wa"], sm["pgrp"]], [sm["c1"]])
                            small("dve", lambda E: E.tensor_tensor(out=sm["c2"].t[:], in0=sm["wb2"].t[:], in1=sm["pgrp"].t[:],
                                                                   op=ALU.mult), [sm["wb2"], sm["pgrp"]], [sm["c2"]])
                            small("dve", lambda E: E.tensor_scalar(out=oh1.t[:], in0=oh1.t[:], scalar1=sm["c1"].t[:, 0:1],
                                                                   scalar2=None, op0=ALU.mult), [oh1, sm["c1"]], [oh1])
                            small("dve", lambda E, tb=tb: E.scalar_tensor_tensor(out=cw.t[:, tb, :], in0=oh2.t[:],
                                                                                 scalar=sm["c2"].t[:, 0:1], in1=oh1.t[:],
                                                                                 op0=ALU.mult, op1=ALU.add),
                                  [oh2, sm["c2"], oh1], [cw])
                        for e in range(NE):
                            wi, wo_ = w1[ecount % 2], w2[ecount % 2]
                            ecount += 1
                            wload(wi.t[:], w_e_in[e].rearrange("(k p) c -> p k c", p=128), wi.k)
                            wload(wo_.t[:], w_e_out[e].rearrange("(k p) c -> p k c", p=128), wo_.k)
                            for ch in range(4):
                                hd = hid[ch % 2]
                                cs = slice(ch * 512, (ch + 1) * 512)
                                for ct in range(4):
                                    pg, pu, sgt = pb[ct % 2], pb[2 + ct % 2], sg[ct % 2]
                                    mmgroup(pg.t[:, :], pg,
                                            [(wi.t[:, kc, ct * 128:(ct + 1) * 128], xnh.t[:, kc, cs]) for kc in range(8)],
                                            [wi.k, xnh.k])
                                    mmgroup(pu.t[:, :], pu,
                                            [(wi.t[:, kc, 512 + ct * 128:512 + (ct + 1) * 128], xnh.t[:, kc, cs])
                                             for kc in range(8)], [wi.k, xnh.k])
                                    S.op("act", lambda E, pg=pg, sgt=sgt: E.activation(out=sgt.t[:], in_=pg.t[:], func=AF.Silu),
                                         reads=[pg.k], writes=[sgt.k])
                                    S.op("dve", lambda E, pu=pu, sgt=sgt, hd=hd, ct=ct: E.tensor_tensor(
                                        out=hd.t[:, ct, :], in0=pu.t[:], in1=sgt.t[:], op=ALU.mult),
                                        reads=[pu.k, sgt.k], writes=[hd.k])
                                for tbl in range(4):
                                    tb = ch * 4 + tbl
                                    if e == 0:
                                        router(tb)
                                    for dh in range(2):
                                        po = pb[4 + (tbl * 2 + dh) % 2]
                                        mmgroup(po.t[:, :], po,
                                                [(hd.t[:, kc, tbl * 128:(tbl + 1) * 128], wo_.t[:, kc, dh * 512:(dh + 1) * 512])
                                                 for kc in range(4)], [hd.k, wo_.k])
                                        S.op("dve", lambda E, po=po, tb=tb, dh=dh, e=e: E.scalar_tensor_tensor(
                                            out=acc.t[:, tb, dh * 512:(dh + 1) * 512], in0=po.t[:], scalar=cw.t[:, tb, e:e + 1],
                                            in1=acc.t[:, tb, dh * 512:(dh + 1) * 512], op0=ALU.mult, op1=ALU.add),
                                            reads=[po.k, cw.k, acc.k], writes=[acc.k])
                        for tb in range(16):
                            o = ot[tb % 2]
                            small("act", lambda E, tb=tb: E.activation(out=junk.t[:], in_=acc.t[:, tb, :], func=AF.Square,
                                                                       accum_out=sm["ssum"].t[:]), [acc], [junk, sm["ssum"]])
                            small("act", lambda E: E.activation(out=sm["sdv"].t[:], in_=sm["ssum"].t[:], func=AF.Sqrt,
                                                                scale=1.0 / 1024, bias=EPS), [sm["ssum"]], [sm["sdv"]])
                            small("dve", lambda E: E.reciprocal(out=sm["rsv"].t[:], in_=sm["sdv"].t[:]), [sm["sdv"]], [sm["rsv"]])
                            small("dve", lambda E, tb=tb, o=o: E.scalar_tensor_tensor(out=o.t[:], in0=acc.t[:, tb, :],
                                                                                      scalar=sm["rsv"].t[:, 0:1], in1=gfb.t[:],
                                                                                      op0=ALU.mult, op1=ALU.mult),
                                  [acc, sm["rsv"], gfb], [o])
                            r0 = t0 + tb * 128
                            S.dma("sp", out_own[r0:r0 + 128, :], o.t[:], reads=[o.k])
                    barrier()

        _phaseD()
        S.finish([])
        globals()['_LAST_S'] = S
        S.emit()
    return nc


def _consts(j):
    ident = np.eye(128, dtype=np.float32)
    rmat = np.zeros((128, 128), np.float32)
    invf = np.zeros((128, 1), np.float32)
    for p in range(128):
        d = p % 64
        if d < 8:
            rmat[p + 8, p] = -1.0
        elif d < 16:
            rmat[p - 8, p] = 1.0
        if d < 16:
            invf[p, 0] = np.float32(500000.0) ** (-np.float32(2 * (d % 8)) / np.float32(16))
    k = np.arange(128)[:, None]
    r = np.arange(128)[None, :]
    tri = (k <= r).astype(np.float32)
    cm_diff = np.zeros((128, 8, 512), np.float32)
    for z in range(8):
        for blk in range(4):
            gi = 2 * blk + j
            if z < gi:
                cm_diff[:, z, blk * 128:(blk + 1) * 128] = 1.0
            elif z == gi:
                cm_diff[:, z, blk * 128:(blk + 1) * 128] = tri
    cm_idx = np.zeros((128, 256), np.float32)
    for cb in range(2):
        if cb == j:
            cm_idx[:, cb * 128:(cb + 1) * 128] = np.where(tri.T > 0, 0.0, -1e30)
        elif cb > j:
            cm_idx[:, cb * 128:(cb + 1) * 128] = -1e30
    return ident, rmat, invf, cm_diff, cm_idx


def _own_idx(j):
    return np.concatenate([np.arange(128) + (2 * m + j) * 128 for m in range(32)])


def make_in_maps(x, positions, mem, g_mix, w_in, b_gate, lambda_q1, lambda_k1, lambda_q2, lambda_k2,
                 g_diff_sub, g_mem, w_mem_kv, w_br_diff, w_br_dsa, w_br_mem, w_out, g_ffn,
                 w_route_group, b_route_group, w_route_expert, b_route_expert, w_exp_in, w_exp_out,
                 g_final, cores=range(8)):
    f = lambda a: np.ascontiguousarray(np.asarray(a))
    x = np.asarray(x); positions = np.asarray(positions); mem = np.asarray(mem)
    col8 = lambda g: f(np.asarray(g).reshape(8, 128).T)
    shared = {
        "w_in": f(np.asarray(w_in)[0]),
        "b_gate24": f(np.asarray(b_gate)[0].reshape(24, 128).T),
        "lam_in": f(np.concatenate([np.asarray(a)[0] for a in (lambda_q1, lambda_k1, lambda_q2, lambda_k2)])[None, :]),
        "gsub_in": f(np.asarray(g_diff_sub)[0][:, None]),
        "gmix8": col8(np.asarray(g_mix)[0]),
        "gmem8": col8(np.asarray(g_mem)[0]),
        "gffn8": col8(np.asarray(g_ffn)[0]),
        "w_mem_kv": f(np.asarray(w_mem_kv)[0]),
        "w_br": f(np.stack([np.asarray(w_br_diff)[0], np.asarray(w_br_dsa)[0], np.asarray(w_br_mem)[0]])),
        "w_out": f(np.asarray(w_out)[0]),
        "w_r": f(np.concatenate([np.asarray(w_route_group)[0], np.asarray(w_route_expert)[0]], axis=1)),
        "b_r": f(np.concatenate([np.asarray(b_route_group)[0], np.asarray(b_route_expert)[0]])[None, :]),
        "w_e_in": f(np.asarray(w_exp_in)[0]),
        "w_e_out": f(np.asarray(w_exp_out)[0]),
        "gfin": f(np.asarray(g_final)[None, :]),
    }
    maps = []
    for c in cores:
        b, j = c // 2, c % 2
        own = _own_idx(j)
        ident, rmat, invf, cm_diff, cm_idx = _consts(j)
        m = dict(shared)
        m.update({
            "xT_full": f(x[b].T), "xT_own": f(x[b][own].T), "x_own": f(x[b][own]),
            "pos_full": f(positions[b][None, :].astype(np.int32)),
            "pos_own": f(positions[b][own][None, :].astype(np.int32)),
            "memT": f(mem[b].T),
            "ident_in": ident, "rmat_in": rmat, "invf_in": invf, "cm_diff_in": cm_diff, "cm_idx_in": cm_idx,
        })
        maps.append(m)
    return maps


_NC_CACHE = {}


def kernel(**inputs):
    if "nc" not in _NC_CACHE:
        _NC_CACHE["nc"] = build()
    nc = _NC_CACHE["nc"]
    maps = make_in_maps(**inputs)
    res = run_bass_kernel_spmd(nc, maps, core_ids=list(range(8)))
    out = np.zeros((4, 8192, 1024), np.float32)
    for c in range(8):
        b, j = c // 2, c % 2
        out[b][_own_idx(j)] = res.results[c]["out_own"]
    return out
```

```python
import numpy as np
from contextlib import ExitStack
import concourse.bass as bass
import concourse.mybir as mybir
from concourse.bass_utils import run_bass_kernel_spmd

F32 = mybir.dt.float32
BF16 = mybir.dt.bfloat16
I32 = mybir.dt.int32
ALU = mybir.AluOpType
AF = mybir.ActivationFunctionType
AX = mybir.AxisListType

SAME_ENGINE_SYNC = True


class Tok:
    __slots__ = ("w", "r", "psum")

    def __init__(self, psum=False):
        self.w = None
        self.r = {}
        self.psum = psum


def toks(n):
    return [Tok() for _ in range(n)]


class Sched:
    ENGS = ("pe", "act", "dve", "pool", "sp")
    CH = 16000
    NDMA = 48

    def __init__(self, nc, es, est_counts):
        self.nc = nc
        self.q = {e: [] for e in self.ENGS}
        self.cnt = {e: 0 for e in self.ENGS}
        self.waited = {e: {} for e in self.ENGS}
        self.sems = {}
        for e in ("pe", "act", "dve", "pool"):
            n = est_counts.get(e, self.CH) // self.CH + 2
            self.sems[e] = [es.enter_context(nc.semaphore(f"s_{e}{i}")) for i in range(n)]
        self.NSW = 16
        self.dsem = [es.enter_context(nc.semaphore(f"s_dma{i}")) for i in range(self.NDMA + self.NSW)]
        self.dk = 0
        self.dk_sw = 0
        self.dlast = {}
        self.nwait = 0
        self.pending_unsig = {e: False for e in self.ENGS}
        self.log = {e: [] for e in self.ENGS}
        self.semname = {}

    def _semval(self, tok):
        if tok[0] == "c":
            _, e, k = tok
            return self.sems[e][(k - 1) // self.CH], (k - 1) % self.CH + 1, ("c", e, (k - 1) // self.CH)
        _, i, v = tok
        return self.dsem[i], v, ("d", i)

    def wait(self, eng, tok):
        if tok is None:
            return
        if tok[0] == "c" and tok[1] == eng:
            if eng == "pe" or not SAME_ENGINE_SYNC:
                return
        sem, val, key = self._semval(tok)
        if self.waited[eng].get(key, 0) >= val:
            return
        self.waited[eng][key] = val
        self.nwait += 1
        self.q[eng].append(lambda E, sem=sem, val=val: E.wait_ge(sem, val))
        self.log[eng].append(('wait', key, val))

    def _deps(self, eng, reads, writes):
        for t in reads:
            self.wait(eng, t.w)
            if t.psum:
                for k, r in t.r.items():
                    if k != eng and k != "pe":
                        self.wait(eng, r)
        for t in writes:
            self.wait(eng, t.w)
            for r in t.r.values():
                self.wait(eng, r)

    def _mark(self, tok, reads, writes):
        for t in reads:
            t.r[tok[1] if tok[0] == "c" else ("d", tok[1])] = tok
        for t in writes:
            t.w = tok
            t.r = {}

    def op(self, eng, fn, reads=(), writes=(), signal=True):
        self._deps(eng, reads, writes)
        if signal:
            self.cnt[eng] += 1
            k = self.cnt[eng]
            tok = ("c", eng, k)
            sem = self.sems[eng][(k - 1) // self.CH]
            self.q[eng].append(lambda E, fn=fn, sem=sem: fn(E).then_inc(sem, 1))
            self.log[eng].append(('op', k))
        else:
            tok = ("c", eng, self.cnt[eng] + 1)
            self.q[eng].append(lambda E, fn=fn: fn(E))
            self.log[eng].append(('op-unsig', self.cnt[eng] + 1))
        self._mark(tok, reads, writes)
        return tok

    def dma(self, eng, out, in_, reads=(), writes=(), **kw):
        self._deps(eng, reads, writes)
        if eng == "pool":
            i = self.NDMA + self.dk_sw % self.NSW
            v = 16 * (self.dk_sw // self.NSW + 1)
            self.dk_sw += 1
        else:
            i = self.dk % self.NDMA
            v = 16 * (self.dk // self.NDMA + 1)
            self.dk += 1
        self.dlast[i] = v
        if v > 16:
            self.wait(eng, ("d", i, v - 16))
        sem = self.dsem[i]
        self.q[eng].append(lambda E, out=out, in_=in_, sem=sem, kw=kw:
                           E.dma_start(out=out, in_=in_, **kw).then_inc(sem, 16))
        tok = ("d", i, v)
        self.log[eng].append(('dma', i, v))
        self._mark(tok, reads, writes)
        return tok

    def finish(self, out_toks):
        for t in out_toks:
            self.wait("sp", t.w)
        for e in ("pe", "act", "dve", "pool"):
            if self.cnt[e]:
                self.wait("sp", ("c", e, self.cnt[e]))

    def emit(self):
        nc = self.nc
        with nc.Block() as block:
            @block.tensor
            def _(E):
                for f in self.q["pe"]:
                    f(E)

            @block.scalar
            def _(E):
                for f in self.q["act"]:
                    f(E)

            @block.vector
            def _(E):
                for f in self.q["dve"]:
                    f(E)

            @block.gpsimd
            def _(E):
                for f in self.q["pool"]:
                    f(E)

            @block.sync
            def _(E):
                for f in self.q["sp"]:
                    f(E)

import math

PI = math.pi
FP8 = mybir.dt.float8e5
NCH_LIMIT = None
ROPE_ADD_ENG = 'pool'
ROPE_LEVEL = 5
EXP = 0
FM_LIMIT = 99
SIG_ALL = False
SKIP_NORM = False
DVE_VAR = 0
EPS = 1e-6
TF = 8192
TO = 4096
CW1 = 6.28125
CW2 = 2.0 * math.pi - 6.28125
C_DQ, C_DK, C_DV, C_SQ, C_SK, C_SV, C_IQ, C_IK, C_IW, C_MQ, C_GL = (
    0, 512, 1024, 1536, 2048, 2560, 3072, 3584, 3648, 3656, 4168)


class Buf:
    __slots__ = ("t", "k")

    def __init__(self, t):
        self.t = t
        self.k = Tok()


def build(debug=False, stop_after=99, norope=False, notm=False, nofm=False, notab=False, tabonly=False):
    nc = bass.Bass("TRN2", target_bir_lowering=False)

    def din(name, shape, dt=F32):
        return nc.dram_tensor(name, list(shape), dt, kind="ExternalInput").ap()

    def dscr(name, shape, dt):
        return nc.dram_tensor(name, list(shape), dt,
                              kind=("ExternalOutput" if debug else "Internal")).ap()

    xT_full = din("xT_full", [1024, TF])
    xT_own = din("xT_own", [1024, TO])
    x_own = din("x_own", [TO, 1024])
    pos_full = din("pos_full", [1, TF], I32)
    pos_own = din("pos_own", [1, TO], I32)
    memT = din("memT", [1024, 256])
    w_in = din("w_in", [1024, 7240])
    b_gate24 = din("b_gate24", [128, 24])
    lam_in = din("lam_in", [1, 256])
    gsub_in = din("gsub_in", [128, 1])
    gmix8 = din("gmix8", [128, 8])
    gmem8 = din("gmem8", [128, 8])
    gffn8 = din("gffn8", [128, 8])
    w_mem_kv = din("w_mem_kv", [1024, 1024])
    w_br = din("w_br", [3, 512, 1024])
    w_out = din("w_out", [1024, 1024])
    w_r = din("w_r", [1024, 20])
    b_r = din("b_r", [1, 20])
    w_e_in = din("w_e_in", [16, 1024, 1024])
    w_e_out = din("w_e_out", [16, 512, 1024])
    gfin = din("gfin", [1, 1024])
    ident_in = din("ident_in", [128, 128])
    rmat_in = din("rmat_in", [128, 128])
    invf_in = din("invf_in", [128, 1])
    cm_diff_in = din("cm_diff_in", [128, 8, 512])
    cm_idx_in = din("cm_idx_in", [128, 256])
    out_own = nc.dram_tensor("out_own", [TO, 1024], F32, kind="ExternalOutput").ap()

    NTF = dscr("NTF", [8, 128, TF], BF16)
    NTO = dscr("NTO", [8, 128, TO], BF16)
    KTD = dscr("KTD", [4, 128, TF], BF16)
    IKT = dscr("IKT", [128, TF], BF16)
    VD = dscr("VD", [TF, 512], BF16)
    KTS = dscr("KTS", [4, 128, TF], BF16)
    VAUG = dscr("VAUG", [TF, 1024], BF16)
    QTD = dscr("QTD", [4, 128, TO], BF16)
    QTS = dscr("QTS", [4, 128, TO], BF16)
    IQT = dscr("IQT", [4, 128, TO], BF16)
    MQT = dscr("MQT", [4, 128, TO], BF16)
    IW = dscr("IW", [TO, 8], F32)
    YD = dscr("YD", [4, 128, TO], BF16)
    YS = dscr("YS", [4, 128, TO], BF16)
    XNT = dscr("XNT", [8, 128, TO], BF16)
    H2 = dscr("H2", [TO, 1024], F32)

    with ExitStack() as es:
        S = Sched(nc, es, {"pe": 120000, "act": 40000, "dve": 60000, "pool": 20000})

        def sb(st, name, shape, dt):
            return Buf(st.enter_context(nc.sbuf_tensor(name, list(shape), dt)))

        pb = [Buf(es.enter_context(nc.psum_tensor(f"pb{i}", [128, 512], F32))) for i in range(8)]
        for b_ in pb:
            b_.k.psum = True

        ident = sb(es, "ident", [128, 128], BF16)
        rmat = sb(es, "rmat", [128, 128], BF16)
        ones = sb(es, "ones", [128, 128], BF16)
        zeros = sb(es, "zeros", [128, 512], BF16)
        invf = sb(es, "invf", [128, 1], F32)
        S.dma("pool", ident.t[:], ident_in, writes=[ident.k])
        S.dma("pool", rmat.t[:], rmat_in, writes=[rmat.k])
        S.dma("sp", invf.t[:], invf_in, writes=[invf.k])
        S.op("pool", lambda E: E.memset(ones.t[:], 1.0), writes=[ones.k])
        S.op("pool", lambda E: E.memset(zeros.t[:], 0.0), writes=[zeros.k])

        def barrier():
            for e in S.ENGS:
                for p in ("pe", "act", "dve", "pool"):
                    if p != e and S.cnt[p]:
                        S.wait(e, ("c", p, S.cnt[p]))
                for i, v in S.dlast.items():
                    S.wait(e, ("d", i, v))

        def mmgroup(out_ap, pbuf, pairs, rtoks, start=True, stop=True, sig_last=True, **kw):
            n = len(pairs)
            for i, (l, r) in enumerate(pairs):
                S.op("pe", lambda E, l=l, r=r, i=i: E.matmul(
                    out_ap, l, r, start=(start and i == 0), stop=(stop and i == n - 1), **kw),
                    reads=rtoks, writes=[pbuf.k], signal=((sig_last and i == n - 1) or SIG_ALL))

        def wload(dst_ap, src_ap, tok):
            S.dma("pool", dst_ap, src_ap, writes=[tok])

        def norm_chunk(xs, sq, nt, sd, rs, g8, w):
            S.op("act", lambda E: E.activation(out=sq.t[:, :, :w], in_=xs.t[:, :, :w], func=AF.Square),
                 reads=[xs.k], writes=[sq.k])
            mmgroup(pb[7].t[:, :w], pb[7], [(ones.t[:], sq.t[:, kc, :w]) for kc in range(8)], [sq.k, ones.k])
            S.op("act", lambda E: E.activation(out=sd.t[:, :w], in_=pb[7].t[:, :w], func=AF.Sqrt,
                                               scale=1.0 / 1024, bias=EPS),
                 reads=[pb[7].k], writes=[sd.k])
            S.op("dve", lambda E: E.reciprocal(out=rs.t[:, :w], in_=sd.t[:, :w]), reads=[sd.k], writes=[rs.k])
            for kc in range(8):
                S.op("dve", lambda E, kc=kc: E.scalar_tensor_tensor(
                    out=nt.t[:, kc, :w], in0=xs.t[:, kc, :w], scalar=g8.t[:, kc:kc + 1], in1=rs.t[:, :w],
                    op0=ALU.mult, op1=ALU.mult), reads=[xs.k, rs.k, g8.k], writes=[nt.k])

        def phase_norm(xT, T, g_in, dest, tag):
            with ExitStack() as ph:
                g8 = sb(ph, "g8" + tag, [128, 8], F32)
                S.dma("sp", g8.t[:], g_in, writes=[g8.k])
                xs = [sb(ph, f"xs{tag}{i}", [128, 8, 512], F32) for i in range(2)]
                sq = [sb(ph, f"sq{tag}{i}", [128, 8, 512], BF16) for i in range(2)]
                nt = [sb(ph, f"nt{tag}{i}", [128, 8, 512], BF16) for i in range(2)]
                sd = sb(ph, "sd" + tag, [128, 512], F32)
                rs = sb(ph, "rs" + tag, [128, 512], F32)
                nch = T // 512

                def ld(c):
                    S.dma("sp", xs[c % 2].t[:], xT[:, c * 512:(c + 1) * 512].rearrange("(k p) t -> p k t", p=128),
                          writes=[xs[c % 2].k])
                ld(0)
                for c in range(nch):
                    if c + 1 < nch:
                        ld(c + 1)
                    i = c % 2
                    norm_chunk(xs[i], sq[i], nt[i], sd, rs, g8, 512)
                    S.dma("sp", dest[:, :, c * 512:(c + 1) * 512].rearrange("k p t -> p k t"), nt[i].t[:],
                          reads=[nt[i].k])
                barrier()

        if not SKIP_NORM:
            phase_norm(xT_full, TF, gmix8, NTF, "f")
        if stop_after >= 1 and not SKIP_NORM:
            phase_norm(xT_own, TO, gmix8, NTO, "o")

        def phase_proj(nT_dram, T, pos_dram, wcols, W, fm_specs, tm_specs, tag):
            with ExitStack() as ph:
                wt = sb(ph, "wt" + tag, [128, 8, W], BF16)
                for (dst0, src0, n) in wcols:
                    wload(wt.t[:, :, dst0:dst0 + n],
                          w_in[:, src0:src0 + n].rearrange("(k p) c -> p k c", p=128), wt.k)
                ntc = [sb(ph, f"ntc{tag}{i}", [128, 8, 512], BF16) for i in range(2)]
                posi = sb(ph, "posi" + tag, [128, T], I32)
                S.dma("sp", posi.t[:], pos_dram[0, :].partition_broadcast(128), writes=[posi.k])
                pf = sb(ph, "pf" + tag, [128, 512], F32)
                ang = sb(ph, "ang" + tag, [128, 512], F32)
                ki = sb(ph, "ki" + tag, [128, 512], I32)
                kf = sb(ph, "kf" + tag, [128, 512], F32)
                rr = sb(ph, "rr" + tag, [128, 512], F32)
                tt = sb(ph, "tt" + tag, [128, 512], F32)
                Ccs = [sb(ph, f"Cc{tag}{i}", [128, 512], F32) for i in range(2)]
                Scs = [sb(ph, f"Sc{tag}{i}", [128, 512], F32) for i in range(2)]
                qbb = [sb(ph, f"qbb{tag}{i}", [128, 512], BF16) for i in range(2)]
                t1b = [sb(ph, f"t1b{tag}{i}", [128, 512], F32) for i in range(2)]
                t2b = [sb(ph, f"t2b{tag}{i}", [128, 512], F32) for i in range(2)]
                otb = [sb(ph, f"otb{tag}{i}", [128, 512], BF16) for i in range(3)]
                vst = [sb(ph, f"vst{tag}{i}", [128, 512], BF16) for i in range(2)]
                vau = [sb(ph, f"vau{tag}{i}", [128, 8, 128], BF16) for i in range(2)]
                iwst = [sb(ph, f"iwst{tag}{i}", [128, 8], F32) for i in range(2)]
                for i in range(2):
                    S.op("pool", lambda E, i=i: E.memset(vau[i].t[:], 1.0), writes=[vau[i].k])
                nch = T // 512 if NCH_LIMIT is None else (NCH_LIMIT * 2 if T == TF else NCH_LIMIT)

                def ld(c):
                    S.dma("sp", ntc[c % 2].t[:], nT_dram[:, :, c * 512:(c + 1) * 512].rearrange("k p t -> p k t"),
                          writes=[ntc[c % 2].k])

                def tables_dve(c):
                    S.op("dve", lambda E, c=c: E.tensor_copy(out=pf.t[:], in_=posi.t[:, c * 512:(c + 1) * 512]),
                         reads=[posi.k], writes=[pf.k])
                    S.op("dve", lambda E: E.tensor_scalar(out=ang.t[:], in0=pf.t[:], scalar1=invf.t[:, 0:1],
                                                          scalar2=None, op0=ALU.mult),
                         reads=[pf.k, invf.k], writes=[ang.k])
                    S.op("dve", lambda E: E.tensor_scalar(out=ki.t[:], in0=ang.t[:], scalar1=1.0 / (2 * PI),
                                                          scalar2=None, op0=ALU.mult),
                         reads=[ang.k], writes=[ki.k])
                    S.op("dve", lambda E: E.tensor_copy(out=kf.t[:], in_=ki.t[:]), reads=[ki.k], writes=[kf.k])
                    S.op("dve", lambda E: E.scalar_tensor_tensor(out=rr.t[:], in0=kf.t[:], scalar=-CW1, in1=ang.t[:],
                                                                 op0=ALU.mult, op1=ALU.add),
                         reads=[kf.k, ang.k], writes=[rr.k])
                    S.op("dve", lambda E: E.scalar_tensor_tensor(out=rr.t[:], in0=kf.t[:], scalar=-CW2, in1=rr.t[:],
                                                                 op0=ALU.mult, op1=ALU.add),
                         reads=[kf.k, rr.k], writes=[rr.k])
                    S.op("dve", lambda E: E.tensor_scalar(out=tt.t[:], in0=rr.t[:], scalar1=PI, scalar2=-2 * PI,
                                                          op0=ALU.is_gt, op1=ALU.mult), reads=[rr.k], writes=[tt.k])
                    S.op("dve", lambda E: E.tensor_tensor(out=rr.t[:], in0=rr.t[:], in1=tt.t[:], op=ALU.add),
                         reads=[rr.k, tt.k], writes=[rr.k])
                    S.op("dve", lambda E: E.tensor_scalar(out=tt.t[:], in0=rr.t[:], scalar1=-PI, scalar2=2 * PI,
                                                          op0=ALU.is_lt, op1=ALU.mult), reads=[rr.k], writes=[tt.k])
                    S.op("dve", lambda E: E.tensor_tensor(out=rr.t[:], in0=rr.t[:], in1=tt.t[:], op=ALU.add),
                         reads=[rr.k, tt.k], writes=[rr.k])
                    S.op("dve", lambda E: E.tensor_scalar(out=rr.t[:], in0=rr.t[:], scalar1=-3.141592, scalar2=3.141592,
                                                          op0=ALU.max, op1=ALU.min), reads=[rr.k], writes=[rr.k])

                def tables_act(c):
                    Cc, Sc = Ccs[c % 2], Scs[c % 2]
                    S.op("act", lambda E: E.activation(out=Sc.t[:], in_=rr.t[:], func=AF.Sin),
                         reads=[rr.k], writes=[Sc.k])
                    S.op("act", lambda E: E.activation(out=tt.t[:], in_=rr.t[:], func=AF.Abs),
                         reads=[rr.k], writes=[tt.k])
                    S.op("act", lambda E: E.activation(out=Cc.t[:], in_=tt.t[:], func=AF.Sin, scale=-1.0, bias=PI / 2),
                         reads=[tt.k], writes=[Cc.k])

                ld(0)
                nst = 0
                for c in range(nch):
                    if c + 1 < nch:
                        ld(c + 1)
                    nb = ntc[c % 2]
                    use_tab = any(sp[2] for sp in fm_specs) and not norope and not notab
                    if use_tab and c == 0:
                        tables_dve(0)
                        tables_act(0)
                    if use_tab and c + 1 < nch:
                        tables_dve(c + 1)
                    Cc, Sc = Ccs[c % 2], Scs[c % 2]
                    pabanks = (pb[0], pb[1], pb[6])
                    pending = None

                    def rope_tail(p):
                        ti_, pa_, qb_, ot_, dst_, Cc_, Sc_ = p
                        pr = pb[2 + ti_ % 2]
                        t1, t2 = t1b[ti_ % 2], t2b[ti_ % 2]
                        mmgroup(pr.t[:, :], pr, [(rmat.t[:], qb_.t[:])], [rmat.k, qb_.k])
                        S.op("dve", lambda E: E.tensor_tensor(out=t1.t[:], in0=pa_.t[:], in1=Cc_.t[:], op=ALU.mult),
                             reads=[pa_.k, Cc_.k], writes=[t1.k])
                        S.op("dve", lambda E: E.tensor_tensor(out=t2.t[:], in0=pr.t[:], in1=Sc_.t[:], op=ALU.mult),
                             reads=[pr.k, Sc_.k], writes=[t2.k])
                        S.op(ROPE_ADD_ENG, lambda E: E.tensor_tensor(out=ot_.t[:], in0=t1.t[:], in1=t2.t[:],
                                                                     op=ALU.add),
                             reads=[t1.k, t2.k], writes=[ot_.k])
                        S.dma("sp", dst_, ot_.t[:], reads=[ot_.k])

                    for ti, (wcol, dest, rope) in enumerate([] if nofm else fm_specs[:FM_LIMIT]):
                        rope = rope and not norope and not tabonly
                        pa = pabanks[ti % 3]
                        mmgroup(pa.t[:, :], pa, [(wt.t[:, kc, wcol:wcol + 128], nb.t[:, kc, :]) for kc in range(8)],
                                [wt.k, nb.k])
                        ot = otb[nst % 3]
                        nst += 1
                        if rope:
                            qb = qbb[ti % 2]
                            S.op("act", lambda E, qb=qb, pa=pa: E.activation(out=qb.t[:], in_=pa.t[:], func=AF.Copy),
                                 reads=[pa.k], writes=[qb.k])
                        else:
                            S.op("act", lambda E, ot=ot, pa=pa: E.activation(out=ot.t[:], in_=pa.t[:], func=AF.Copy),
                                 reads=[pa.k], writes=[ot.k])
                            S.dma("sp", dest(c), ot.t[:], reads=[ot.k])
                        if pending is not None:
                            rope_tail(pending)
                        pending = (ti, pa, qb, ot, dest(c), Cc, Sc) if rope else None
                    if pending is not None:
                        rope_tail(pending)
                    if use_tab and c + 1 < nch:
                        tables_act(c + 1)
                    for (wcol, ncol, kind) in ([] if notm else tm_specs):
                        for tb in range(4):
                            pv = pb[4 + tb % 2]
                            mmgroup(pv.t[:, :ncol], pv,
                                    [(nb.t[:, kc, tb * 128:(tb + 1) * 128], wt.t[:, kc, wcol:wcol + ncol])
                                     for kc in range(8)], [wt.k, nb.k])
                            r0 = (c * 4 + tb) * 128
                            if kind == "vd":
                                st_ = vst[tb % 2]
                                S.op("act", lambda E, st_=st_, pv=pv: E.activation(out=st_.t[:], in_=pv.t[:],
                                                                                  func=AF.Copy),
                                     reads=[pv.k], writes=[st_.k])
                                S.dma("sp", VD[r0:r0 + 128, :], st_.t[:], reads=[st_.k])
                            elif kind == "vaug":
                                st_ = vau[tb % 2]
                                S.op("act", lambda E, st_=st_, pv=pv: E.activation(
                                    out=st_.t[:, :, 0:64], in_=pv.t[:, :].rearrange("p (h d) -> p h d", h=8),
                                    func=AF.Copy), reads=[pv.k], writes=[st_.k])
                                S.dma("sp", VAUG[r0:r0 + 128, :].rearrange("p (h d) -> p h d", h=8), st_.t[:],
                                      reads=[st_.k])
                            else:
                                st_ = iwst[tb % 2]
                                S.op("act", lambda E, st_=st_, pv=pv: E.activation(out=st_.t[:], in_=pv.t[:, 0:8],
                                                                                  func=AF.Copy),
                                     reads=[pv.k], writes=[st_.k])
                                S.dma("sp", IW[r0:r0 + 128, :], st_.t[:], reads=[st_.k])
                barrier()

        def fm_dest(T4, h):
            return lambda c: T4[h, :, c * 512:(c + 1) * 512]

        if stop_after >= 2:
          phase_proj(NTF, TF, pos_full,
                   [(0, C_DK, 512), (512, C_IK, 64), (576, C_IK, 64), (640, C_DV, 512)], 1152,
                   [(h * 128, fm_dest(KTD, h), True) for h in range(4)]
                   + [(512, (lambda c: IKT[:, c * 512:(c + 1) * 512]), True)],
                   [(640, 512, "vd")], "p1")
        if stop_after >= 3:
          phase_proj(NTF, TF, pos_full,
                   [(0, C_SK, 512), (512, C_SV, 512)], 1024,
                   [(h * 128, fm_dest(KTS, h), True) for h in range(4)],
                   [(512, 512, "vaug")], "p2")
        if stop_after >= 4:
          phase_proj(NTO, TO, pos_own,
                   [(0, C_DQ, 512), (512, C_SQ, 512), (1024, C_IQ, 512), (1536, C_MQ, 512), (2048, C_IW, 8)], 2056,
                   [(h * 128, fm_dest(QTD, h), True) for h in range(4)]
                   + [(512 + h * 128, fm_dest(QTS, h), True) for h in range(4)]
                   + [(1024 + h * 128, fm_dest(IQT, h), True) for h in range(4)]
                   + [(1536 + h * 128, fm_dest(MQT, h), False) for h in range(4)],
                   [(2048, 8, "iw")], "p3")
        def _phaseA():
            if stop_after >= 5:
                with ExitStack() as ph:
                    KT = sb(ph, "KTa", [128, 4, TF], BF16)
                    V = sb(ph, "Va", [128, 64, 512], BF16)
                    cmd = sb(ph, "cmd", [128, 8, 512], BF16)
                    lamb = sb(ph, "lamb", [128, 4, 64], F32)
                    lp = sb(ph, "lp", [128, 2, 64], F32)
                    ls = sb(ph, "ls", [128, 2], F32)
                    le = sb(ph, "le", [128, 2], F32)
                    neglam = sb(ph, "neglam", [128, 1], F32)
                    gsub = sb(ph, "gsub", [128, 1], F32)
                    gs08 = sb(ph, "gs08", [128, 1], F32)
                    for h in range(4):
                        S.dma("sp", KT.t[:, h, :], KTD[h], writes=[KT.k])
                    for q4 in range(4):
                        S.dma("sp", V.t[:, q4 * 16:(q4 + 1) * 16, :],
                              VD[q4 * 2048:(q4 + 1) * 2048, :].rearrange("(kb p) c -> p kb c", p=128), writes=[V.k])
                    wload(cmd.t[:], cm_diff_in, cmd.k)
                    S.dma("sp", lamb.t[:].rearrange("p a d -> p (a d)"), lam_in[0, :].partition_broadcast(128), writes=[lamb.k])
                    S.dma("sp", gsub.t[:], gsub_in, writes=[gsub.k])
                    for a in range(2):
                        S.op("dve", lambda E, a=a: E.tensor_tensor(out=lp.t[:, a, :], in0=lamb.t[:, 2 * a, :],
                                                                  in1=lamb.t[:, 2 * a + 1, :], op=ALU.mult),
                             reads=[lamb.k], writes=[lp.k])
                        S.op("dve", lambda E, a=a: E.reduce_sum(out=ls.t[:, a:a + 1], in_=lp.t[:, a, :], axis=AX.X),
                             reads=[lp.k], writes=[ls.k])
                    S.op("act", lambda E: E.activation(out=le.t[:], in_=ls.t[:], func=AF.Exp), reads=[ls.k], writes=[le.k])
                    S.op("dve", lambda E: E.tensor_tensor(out=neglam.t[:], in0=le.t[:, 1:2], in1=le.t[:, 0:1], op=ALU.subtract),
                         reads=[le.k], writes=[neglam.k])
                    S.op("dve", lambda E: E.tensor_scalar(out=neglam.t[:], in0=neglam.t[:], scalar1=-0.2, scalar2=None,
                                                          op0=ALU.add), reads=[neglam.k], writes=[neglam.k])
                    S.op("dve", lambda E: E.tensor_scalar(out=gs08.t[:], in0=gsub.t[:], scalar1=0.8, scalar2=None,
                                                          op0=ALU.mult), reads=[gsub.k], writes=[gs08.k])
                    QP = [[sb(ph, f"QPa{i}_{mp}", [128, 4, 512], BF16) for mp in range(2)] for i in range(2)]
                    for i in range(2):
                        for mp in range(2):
                            S.op("pool", lambda E, i=i, mp=mp: E.memset(QP[i][mp].t[:], 0.0), writes=[QP[i][mp].k])
                    pT = [sb(ph, f"pTa{i}", [128, 512], BF16) for i in range(4)]
                    rdb = sb(ph, "rdba", [128, 512], F32)
                    o1b = sb(ph, "o1b", [128, 512], F32)
                    tb_ = sb(ph, "tba", [128, 512], F32)
                    yb = sb(ph, "yba", [128, 512], F32)
                    ysq = sb(ph, "ysqa", [128, 512], BF16)
                    sdn = sb(ph, "sdna", [128, 512], F32)
                    rn = sb(ph, "rna", [128, 512], F32)
                    yob = [sb(ph, f"yoba{i}", [128, 512], BF16) for i in range(2)]
                    NG = 8 if NCH_LIMIT is None else NCH_LIMIT

                    def ldq(g):
                        for mp in range(2):
                            r0 = mp * 64
                            S.dma("sp", QP[g % 2][mp].t[r0:r0 + 64, :, :],
                                  QTD[:, r0:r0 + 64, g * 512:(g + 1) * 512].rearrange("h p t -> p h t"),
                                  writes=[QP[g % 2][mp].k])
                    ldq(0)
                    for g in range(NG):
                        if g + 1 < NG:
                            ldq(g + 1)
                        nkb = 8 * g + 8
                        for h in range(4):
                            for mp in range(2):
                                po, pd = pb[2 + mp], pb[4 + mp]
                                Q = QP[g % 2][mp]
                                lo_, hi_ = mp * 64, (mp + 1) * 64
                                psb = (pb[0], pb[1], pb[7])
                                for step in range(nkb + 2):
                                    if step < nkb:
                                        kb = step
                                        ps, pt = psb[kb % 3], pT[kb % 4]
                                        c0 = ((kb - 8 * g) // 2) * 128 if kb >= 8 * g else 0
                                        S.op("pe", lambda E, ps=ps, kb=kb, h=h, Q=Q, c0=c0: E.matmul(
                                            ps.t[:, c0:], KT.t[:, h, kb * 128:(kb + 1) * 128], Q.t[:, h, c0:],
                                            start=True, stop=True), reads=[KT.k, Q.k], writes=[ps.k])
                                        S.op("act", lambda E, ps=ps, pt=pt, c0=c0: E.activation(out=pt.t[:, c0:], in_=ps.t[:, c0:],
                                                                                               func=AF.Exp, scale=0.125),
                                             reads=[ps.k], writes=[pt.k])
                                        if kb >= 8 * g:
                                            z = kb - 8 * g
                                            S.op("dve", lambda E, pt=pt, z=z, c0=c0: E.tensor_tensor(
                                                out=pt.t[:, c0:], in0=pt.t[:, c0:], in1=cmd.t[:, z, c0:], op=ALU.mult),
                                                 reads=[pt.k, cmd.k], writes=[pt.k])
                                    if step >= 2:
                                        kb = step - 2
                                        pt = pT[kb % 4]
                                        c0 = ((kb - 8 * g) // 2) * 128 if kb >= 8 * g else 0
                                        S.op("pe", lambda E, po=po, pt=pt, kb=kb, h=h, nkb=nkb, c0=c0: E.matmul(
                                            po.t[:, c0:], V.t[:, kb, h * 128:(h + 1) * 128], pt.t[:, c0:],
                                            start=(kb == 0), stop=(kb == nkb - 1)), reads=[V.k, pt.k], writes=[po.k],
                                            signal=False)
                                        S.op("pe", lambda E, pd=pd, pt=pt, kb=kb, nkb=nkb, c0=c0: E.matmul(
                                            pd.t[:, c0:], ones.t[:], pt.t[:, c0:], start=(kb == 0), stop=(kb == nkb - 1)),
                                            reads=[ones.k, pt.k], writes=[pd.k])
                                S.op("dve", lambda E, pd=pd: E.reciprocal(out=rdb.t[:], in_=pd.t[:]), reads=[pd.k], writes=[rdb.k])
                                if mp == 0:
                                    S.op("dve", lambda E, po=po: E.tensor_tensor(out=o1b.t[:], in0=po.t[:], in1=rdb.t[:],
                                                                                op=ALU.mult),
                                         reads=[po.k, rdb.k], writes=[o1b.k])
                                else:
                                    S.op("dve", lambda E, po=po: E.tensor_tensor(out=tb_.t[:], in0=po.t[:], in1=rdb.t[:],
                                                                                op=ALU.mult),
                                         reads=[po.k, rdb.k], writes=[tb_.k])
                            yo = yob[h % 2]
                            S.op("dve", lambda E: E.scalar_tensor_tensor(out=yb.t[:], in0=tb_.t[:], scalar=neglam.t[:, 0:1],
                                                                         in1=o1b.t[:], op0=ALU.mult, op1=ALU.add),
                                 reads=[tb_.k, neglam.k, o1b.k], writes=[yb.k])
                            S.op("act", lambda E: E.activation(out=ysq.t[:], in_=yb.t[:], func=AF.Square),
                                 reads=[yb.k], writes=[ysq.k])
                            mmgroup(pb[6].t[:, :], pb[6], [(ones.t[:], ysq.t[:])], [ones.k, ysq.k])
                            S.op("act", lambda E: E.activation(out=sdn.t[:], in_=pb[6].t[:], func=AF.Sqrt, scale=1.0 / 128,
                                                               bias=EPS), reads=[pb[6].k], writes=[sdn.k])
                            S.op("dve", lambda E: E.reciprocal(out=rn.t[:], in_=sdn.t[:]), reads=[sdn.k], writes=[rn.k])
                            S.op("dve", lambda E, yo=yo: E.scalar_tensor_tensor(out=yo.t[:], in0=yb.t[:], scalar=gs08.t[:, 0:1],
                                                                                in1=rn.t[:], op0=ALU.mult, op1=ALU.mult),
                                 reads=[yb.k, gs08.k, rn.k], writes=[yo.k])
                            S.dma("sp", YD[h, :, g * 512:(g + 1) * 512], yo.t[:], reads=[yo.k])
                    barrier()

        _phaseA()
        def _phaseB():
            if stop_after >= 6:
                with ExitStack() as ph:
                    KT = sb(ph, "KTb", [128, 4, TF], BF16)
                    IK = sb(ph, "IKb", [128, TF], BF16)
                    scores = [sb(ph, f"score{i}", [128, TF], F32) for i in range(2)]
                    maskbs = [sb(ph, f"maskb{i}", [128, TF], FP8) for i in range(2)]
                    NV = 8
                    Vr = [sb(ph, f"Vr{i}", [128, 1024], BF16) for i in range(NV)]
                    cmi = sb(ph, "cmi", [128, 256], F32)
                    for h in range(4):
                        S.dma("sp", KT.t[:, h, :], KTS[h], writes=[KT.k])
                    S.dma("sp", IK.t[:], IKT, writes=[IK.k])
                    S.dma("sp", cmi.t[:], cm_idx_in, writes=[cmi.k])
                    QS = [[sb(ph, f"QSb{i}_{par}", [128, 4, 512], BF16) for par in range(2)] for i in range(1)]
                    QI = [[sb(ph, f"QIb{i}_{par}", [128, 4, 512], BF16) for par in range(2)] for i in range(1)]
                    for i in range(1):
                        for par in range(2):
                            S.op("pool", lambda E, i=i, par=par: E.memset(QS[i][par].t[:], 0.0), writes=[QS[i][par].k])
                            S.op("pool", lambda E, i=i, par=par: E.memset(QI[i][par].t[:], 0.0), writes=[QI[i][par].k])
                    IWs = [sb(ph, f"IWb{i}", [128, 4, 8], F32) for i in range(1)]
                    Dh = sb(ph, "Dh", [128, 8, 128], BF16)
                    Rb = [sb(ph, f"Rb{i}", [128, 512], BF16) for i in range(3)]
                    pT = [sb(ph, f"pTb{i}", [128, 512], BF16) for i in range(4)]
                    rmax = sb(ph, "rmax", [128, 1], F32)
                    rmin = sb(ph, "rmin", [128, 1], F32)
                    span = sb(ph, "span", [128, 1], F32)
                    lo = sb(ph, "lo", [128, 1], F32)
                    mid = sb(ph, "mid", [128, 1], F32)
                    cnt = sb(ph, "cnt", [128, 1], F32)
                    gs = sb(ph, "gs", [128, 1], F32)
                    rdb = sb(ph, "rdbb", [128, 128], F32)
                    ysb = [sb(ph, f"ysb{i}", [128, 4, 128], BF16) for i in range(2)]
                    NG = 8 if NCH_LIMIT is None else NCH_LIMIT
                    NIT = 14
                    CSC = (8.0 ** -0.5) * (64.0 ** -0.5)

                    def ldq_i(g):
                        for par in range(2):
                            r0 = par * 64
                            S.dma("sp", QI[0][par].t[r0:r0 + 64, :, :],
                                  IQT[:, r0:r0 + 64, g * 512:(g + 1) * 512].rearrange("h p t -> p h t"), writes=[QI[0][par].k])
                        S.dma("sp", IWs[0].t[:], IW[g * 512:(g + 1) * 512, :].rearrange("(b p) h -> p b h", p=128),
                              writes=[IWs[0].k])

                    def ldq_s(g):
                        for par in range(2):
                            r0 = par * 64
                            S.dma("sp", QS[0][par].t[r0:r0 + 64, :, :],
                                  QTS[:, r0:r0 + 64, g * 512:(g + 1) * 512].rearrange("h p t -> p h t"), writes=[QS[0][par].k])
                    vcount = 0
                    blocks = [(g, blk) for g in range(NG) for blk in range(4)]

                    def stage1a(t):
                        g, blk = blocks[t]
                        if blk == 0:
                            ldq_i(g)
                        Qi, Iw = QI[0], IWs[0]
                        score = scores[t % 2]
                        m = 4 * g + blk
                        nkb = 2 * m + 2
                        n = nkb * 128
                        qs = slice(blk * 128, (blk + 1) * 128)
                        for h in range(8):
                            S.op("dve", lambda E, h=h, Iw=Iw, blk=blk: E.tensor_scalar(
                                out=Dh.t[:, h, :], in0=ident.t[:], scalar1=Iw.t[:, blk, h:h + 1], scalar2=CSC,
                                op0=ALU.mult, op1=ALU.mult), reads=[ident.k, Iw.k], writes=[Dh.k])
                        yield
                        nch = (nkb + 3) // 4
                        psc = pb[2]
                        items = [(c, h) for c in range(nch) for h in range(8)]
                        for idx in range(len(items) + 1):
                            if idx < len(items):
                                c, h = items[idx]
                                w = min(512, n - c * 512)
                                pa, rb = pb[h % 2], Rb[h % 3]
                                Qih = Qi[h % 2]
                                S.op("pe", lambda E, pa=pa, h=h, c=c, w=w, Qih=Qih, qs=qs: E.matmul(
                                    pa.t[:, :w], Qih.t[:, h // 2, qs], IK.t[:, c * 512:c * 512 + w],
                                    start=True, stop=True), reads=[Qih.k, IK.k], writes=[pa.k])
                                S.op("act", lambda E, pa=pa, rb=rb, w=w: E.activation(out=rb.t[:, :w], in_=pa.t[:, :w],
                                                                                     func=AF.Relu),
                                     reads=[pa.k], writes=[rb.k])
                            if idx >= 1:
                                c, h = items[idx - 1]
                                w = min(512, n - c * 512)
                                rb = Rb[h % 3]
                                S.op("pe", lambda E, psc=psc, h=h, rb=rb, w=w: E.matmul(
                                    psc.t[:, :w], Dh.t[:, h, :], rb.t[:, :w], start=(h == 0), stop=(h == 7)),
                                    reads=[Dh.k, rb.k], writes=[psc.k], signal=(h == 7))
                                if h == 7:
                                    S.op("act", lambda E, psc=psc, c=c, w=w, score=score: E.activation(
                                        out=score.t[:, c * 512:c * 512 + w], in_=psc.t[:, :w], func=AF.Copy),
                                        reads=[psc.k], writes=[score.k])
                            if idx % 8 == 7:
                                yield

                    def stage1b(t):
                        g, blk = blocks[t]
                        score = scores[t % 2]
                        maskb = maskbs[t % 2]
                        m = 4 * g + blk
                        nkb = 2 * m + 2
                        n = nkb * 128
                        S.op("dve", lambda E, n=n: E.tensor_reduce(out=rmax.t[:], in_=score.t[:, :n], axis=AX.X, op=ALU.max),
                             reads=[score.k], writes=[rmax.k])
                        S.op("dve", lambda E, n=n: E.tensor_reduce(out=rmin.t[:], in_=score.t[:, :n], axis=AX.X, op=ALU.min),
                             reads=[score.k], writes=[rmin.k])
                        S.op("dve", lambda E, n=n: E.tensor_tensor(out=score.t[:, n - 256:n], in0=score.t[:, n - 256:n],
                                                                  in1=cmi.t[:], op=ALU.add),
                             reads=[score.k, cmi.k], writes=[score.k])
                        S.op("dve", lambda E: E.tensor_tensor(out=span.t[:], in0=rmax.t[:], in1=rmin.t[:], op=ALU.subtract),
                             reads=[rmax.k, rmin.k], writes=[span.k])
                        S.op("dve", lambda E: E.tensor_scalar(out=span.t[:], in0=span.t[:], scalar1=2e-4, scalar2=None,
                                                              op0=ALU.add), reads=[span.k], writes=[span.k])
                        S.op("dve", lambda E: E.tensor_scalar(out=lo.t[:], in0=rmin.t[:], scalar1=-1e-4, scalar2=None,
                                                              op0=ALU.add), reads=[rmin.k], writes=[lo.k])
                        yield
                        for it in range(NIT):
                            f = 2.0 ** -(it + 1)
                            S.op("dve", lambda E, f=f: E.scalar_tensor_tensor(out=mid.t[:], in0=span.t[:], scalar=f,
                                                                              in1=lo.t[:], op0=ALU.mult, op1=ALU.add),
                                 reads=[span.k, lo.k], writes=[mid.k])
                            S.op("dve", lambda E, n=n, maskb=maskb: E.tensor_scalar(
                                out=maskb.t[:, :n], in0=score.t[:, :n], scalar1=mid.t[:, 0:1], scalar2=0.0, op0=ALU.is_ge,
                                op1=ALU.add, accum_out=cnt.t[:], saturate=False), reads=[score.k, mid.k], writes=[maskb.k, cnt.k])
                            S.op("dve", lambda E, f=f: E.tensor_scalar(out=gs.t[:], in0=cnt.t[:], scalar1=255.5, scalar2=f,
                                                                      op0=ALU.is_ge, op1=ALU.mult),
                                 reads=[cnt.k], writes=[gs.k])
                            S.op("dve", lambda E: E.scalar_tensor_tensor(out=lo.t[:], in0=gs.t[:], scalar=span.t[:, 0:1],
                                                                         in1=lo.t[:], op0=ALU.mult, op1=ALU.add),
                                 reads=[gs.k, span.k, lo.k], writes=[lo.k])
                            yield
                        S.op("dve", lambda E, n=n, maskb=maskb: E.tensor_scalar(
                            out=maskb.t[:, :n], in0=score.t[:, :n], scalar1=lo.t[:, 0:1], scalar2=-30000.0, op0=ALU.is_lt,
                            op1=ALU.mult, saturate=False), reads=[score.k, lo.k], writes=[maskb.k])

                    def stage2(t):
                        nonlocal vcount
                        g, blk = blocks[t]
                        if blk == 0:
                            ldq_s(g)
                        Qs = QS[0]
                        maskb = maskbs[t % 2]
                        m = 4 * g + blk
                        nkb = 2 * m + 2
                        qs = slice(blk * 128, (blk + 1) * 128)
                        for bnk in (4, 5):
                            mmgroup(pb[bnk].t[:, :], pb[bnk], [(zeros.t[:, 0:128], zeros.t[:])], [zeros.k])
                        ngr = (nkb + 3) // 4
                        slots = {}

                        def ldv(kg):
                            nonlocal vcount
                            for kb in range(kg * 4, min(nkb, kg * 4 + 4)):
                                sl = vcount % NV
                                vcount += 1
                                slots[kb] = sl
                                S.dma("sp", Vr[sl].t[:], VAUG[kb * 128:(kb + 1) * 128, :], writes=[Vr[sl].k])
                        ldv(0)
                        yield
                        psb = (pb[6], pb[7], pb[3])
                        items = [(kg, h) for kg in range(ngr) for h in range(8)]
                        for idx in range(len(items) + 2):
                            if idx < len(items):
                                kg, h = items[idx]
                                if h == 2 and kg + 1 < ngr:
                                    ldv(kg + 1)
                                kbs = list(range(kg * 4, min(nkb, kg * 4 + 4)))
                                wg = len(kbs) * 128
                                ps, pt = psb[idx % 3], pT[idx % 4]
                                Qsh = Qs[h % 2]
                                for i, kb in enumerate(kbs):
                                    S.op("pe", lambda E, ps=ps, i=i, kb=kb, h=h, Qsh=Qsh, qs=qs: E.matmul(
                                        ps.t[:, i * 128:(i + 1) * 128], KT.t[:, h // 2, kb * 128:(kb + 1) * 128],
                                        Qsh.t[:, h // 2, qs], start=True, stop=False, skip_group_check=True),
                                        reads=[KT.k, Qsh.k], writes=[ps.k], signal=False)
                                    S.op("pe", lambda E, ps=ps, i=i, kb=kb, maskb=maskb: E.matmul(
                                        ps.t[:, i * 128:(i + 1) * 128], maskb.t[:, kb * 128:(kb + 1) * 128], ident.t[:],
                                        start=False, stop=True, skip_group_check=True),
                                        reads=[maskb.k, ident.k], writes=[ps.k], signal=(i == len(kbs) - 1))
                                S.op("act", lambda E, ps=ps, pt=pt, wg=wg: E.activation(
                                    out=pt.t[:, :wg], in_=ps.t[:, :wg], func=AF.Exp, scale=0.125),
                                    reads=[ps.k], writes=[pt.k])
                            if idx >= 2:
                                kg, h = items[idx - 2]
                                kbs = list(range(kg * 4, min(nkb, kg * 4 + 4)))
                                pt = pT[(idx - 2) % 4]
                                acc = pb[4 + h // 4]
                                a0 = (h % 4) * 128
                                for i, kb in enumerate(kbs):
                                    vr = Vr[slots[kb]]
                                    S.op("pe", lambda E, acc=acc, a0=a0, vr=vr, h=h, pt=pt, i=i: E.matmul(
                                        acc.t[:, a0:a0 + 128], vr.t[:, h * 128:(h + 1) * 128],
                                        pt.t[:, i * 128:(i + 1) * 128], start=False, stop=False, skip_group_check=True),
                                        reads=[vr.k, pt.k], writes=[acc.k], signal=(i == len(kbs) - 1))
                            if idx % 8 == 7:
                                yield
                        yt = ysb[m % 2]
                        for h in range(8):
                            acc = pb[4 + h // 4]
                            a0 = (h % 4) * 128
                            S.op("dve", lambda E, acc=acc, a0=a0: E.reciprocal(out=rdb.t[0:64, :],
                                                                              in_=acc.t[64:128, a0:a0 + 128]),
                                 reads=[acc.k], writes=[rdb.k])
                            p0 = (h % 2) * 64
                            S.op("dve", lambda E, acc=acc, a0=a0, p0=p0, h=h, yt=yt: E.tensor_tensor(
                                out=yt.t[p0:p0 + 64, h // 2, :], in0=acc.t[0:64, a0:a0 + 128], in1=rdb.t[0:64, :],
                                op=ALU.mult), reads=[acc.k, rdb.k], writes=[yt.k])
                        S.dma("sp", YS[:, :, m * 128:(m + 1) * 128].rearrange("t p q -> p t q"), yt.t[:], reads=[yt.k])

                    nb_ = len(blocks)

                    def run_all(gen):
                        for _ in gen:
                            pass

                    def interleave(fast, slow, ratio):
                        fast = [g for g in fast if g is not None]
                        k = 0
                        while fast or slow is not None:
                            for g in list(fast):
                                try:
                                    next(g)
                                except StopIteration:
                                    fast.remove(g)
                            k += 1
                            if slow is not None and (k % ratio == 0 or not fast):
                                try:
                                    next(slow)
                                except StopIteration:
                                    slow = None
                    run_all(stage1a(0))
                    if nb_ > 1:
                        run_all(stage1a(1))
                    run_all(stage1b(0))
                    for t in range(nb_):
                        if t + 2 < nb_:
                            run_all(stage1a(t + 2))
                        if t + 1 < nb_:
                            run_all(stage1b(t + 1))
                        run_all(stage2(t))
                    barrier()

        _phaseB()
        def _phaseC():
            if stop_after >= 7:
                with ExitStack() as ph:
                    wg = sb(ph, "wgC", [128, 8, 3072], BF16)
                    wb = sb(ph, "wbC", [128, 12, 1024], BF16)
                    wo = sb(ph, "woC", [128, 8, 1024], BF16)
                    bg = sb(ph, "bgC", [128, 24], F32)
                    gf8 = sb(ph, "gf8C", [128, 8], F32)
                    mkT = sb(ph, "mkT", [128, 4, 256], BF16)
                    mv = sb(ph, "mvC", [128, 2, 512], BF16)
                    wload(wg.t[:], w_in[:, C_GL:C_GL + 3072].rearrange("(k p) c -> p k c", p=128), wg.k)
                    for br in range(3):
                        wload(wb.t[:, br * 4:(br + 1) * 4, :], w_br[br].rearrange("(k p) c -> p k c", p=128), wb.k)
                    wload(wo.t[:], w_out.rearrange("(k p) c -> p k c", p=128), wo.k)
                    S.dma("sp", bg.t[:], b_gate24, writes=[bg.k])
                    S.dma("sp", gf8.t[:], gffn8, writes=[gf8.k])
                    xs = [sb(ph, f"xsC{i}", [128, 8, 512], F32) for i in range(1)]
                    sq = sb(ph, "sqC", [128, 8, 512], BF16)
                    ntc = [sb(ph, f"ntC{i}", [128, 8, 512], BF16) for i in range(1)]
                    sd = sb(ph, "sdC", [128, 512], F32)
                    rs = sb(ph, "rsC", [128, 512], F32)
                    with ExitStack() as ph2:
                        wm = sb(ph2, "wmC", [128, 8, 1024], BF16)
                        gm8 = sb(ph2, "gm8C", [128, 8], F32)
                        wload(wm.t[:], w_mem_kv.rearrange("(k p) c -> p k c", p=128), wm.k)
                        S.dma("sp", gm8.t[:], gmem8, writes=[gm8.k])
                        S.dma("sp", xs[0].t[:, :, 0:256], memT.rearrange("(k p) t -> p k t", p=128), writes=[xs[0].k])
                        norm_chunk(xs[0], sq, ntc[0], sd, rs, gm8, 256)
                        for hm in range(4):
                            mmgroup(pb[0].t[:, :256], pb[0],
                                    [(wm.t[:, kc, hm * 128:(hm + 1) * 128], ntc[0].t[:, kc, 0:256]) for kc in range(8)],
                                    [wm.k, ntc[0].k])
                            S.op("act", lambda E, hm=hm: E.activation(out=mkT.t[:, hm, :], in_=pb[0].t[:, :256], func=AF.Copy),
                                 reads=[pb[0].k], writes=[mkT.k])
                        for mb in range(2):
                            mmgroup(pb[1].t[:, :], pb[1],
                                    [(ntc[0].t[:, kc, mb * 128:(mb + 1) * 128], wm.t[:, kc, 512:1024]) for kc in range(8)],
                                    [wm.k, ntc[0].k])
                            S.op("act", lambda E, mb=mb: E.activation(out=mv.t[:, mb, :], in_=pb[1].t[:, :], func=AF.Copy),
                                 reads=[pb[1].k], writes=[mv.k])
                        barrier()
                    mq = [sb(ph, f"mqC{i}", [128, 4, 512], BF16) for i in range(1)]
                    yd = [sb(ph, f"ydC{i}", [128, 4, 512], BF16) for i in range(1)]
                    ys = [sb(ph, f"ysC{i}", [128, 4, 512], BF16) for i in range(1)]
                    ym = sb(ph, "ymC", [128, 4, 512], BF16)
                    xt = [sb(ph, f"xtC{i}", [128, 4, 1024], F32) for i in range(1)]
                    pT = [sb(ph, f"pTC{i}", [128, 512], BF16) for i in range(2)]
                    rdb = sb(ph, "rdbC", [128, 512], F32)
                    gate = [sb(ph, f"gateC{i}", [128, 512], F32) for i in range(2)]
                    macc = sb(ph, "maccC", [128, 512], F32)
                    mtmp = sb(ph, "mtmpC", [128, 512], F32)
                    mg = sb(ph, "mgC", [128, 8, 512], BF16)
                    xn = sb(ph, "xnC", [128, 8, 512], BF16)
                    NCC = 8 if NCH_LIMIT is None else NCH_LIMIT

                    def ld_mq(c):
                        S.dma("sp", mq[0].t[:], MQT[:, :, c * 512:(c + 1) * 512].rearrange("h p t -> p h t"), writes=[mq[0].k])

                    def ld_merge(c):
                        cs = slice(c * 512, (c + 1) * 512)
                        S.dma("sp", ntc[0].t[:], NTO[:, :, cs].rearrange("k p t -> p k t"), writes=[ntc[0].k])
                        S.dma("sp", yd[0].t[:], YD[:, :, cs].rearrange("h p t -> p h t"), writes=[yd[0].k])
                        S.dma("sp", ys[0].t[:], YS[:, :, cs].rearrange("h p t -> p h t"), writes=[ys[0].k])

                    def ld_xs(c):
                        S.dma("sp", xs[0].t[:], xT_own[:, c * 512:(c + 1) * 512].rearrange("(k p) t -> p k t", p=128),
                              writes=[xs[0].k])

                    def ld_xt(c):
                        S.dma("sp", xt[0].t[:], x_own[c * 512:(c + 1) * 512, :].rearrange("(b p) d -> p b d", p=128),
                              writes=[xt[0].k])
                    ld_mq(0)
                    ld_merge(0)
                    ld_xs(0)
                    ld_xt(0)
                    for c in range(NCC):
                        i = 0
                        nxt = c + 1 < NCC
                        nb, mqc, ydc, ysc, xsc, xtc = ntc[i], mq[i], yd[i], ys[i], xs[i], xt[i]
                        for hm in range(4):
                            for mb in range(2):
                                ps, pt = pb[mb], pT[mb]
                                mmgroup(ps.t[:, :], ps, [(mkT.t[:, hm, mb * 128:(mb + 1) * 128], mqc.t[:, hm, :])],
                                        [mkT.k, mqc.k])
                                S.op("act", lambda E, ps=ps, pt=pt: E.activation(out=pt.t[:], in_=ps.t[:], func=AF.Exp,
                                                                                scale=128.0 ** -0.5),
                                     reads=[ps.k], writes=[pt.k])
                            mmgroup(pb[2].t[:, :], pb[2],
                                    [(mv.t[:, mb, hm * 128:(hm + 1) * 128], pT[mb].t[:]) for mb in range(2)],
                                    [mv.k, pT[0].k, pT[1].k])
                            mmgroup(pb[3].t[:, :], pb[3], [(ones.t[:], pT[mb].t[:]) for mb in range(2)],
                                    [ones.k, pT[0].k, pT[1].k])
                            S.op("dve", lambda E: E.reciprocal(out=rdb.t[:], in_=pb[3].t[:]), reads=[pb[3].k], writes=[rdb.k])
                            S.op("dve", lambda E, hm=hm: E.tensor_tensor(out=ym.t[:, hm, :], in0=pb[2].t[:], in1=rdb.t[:],
                                                                        op=ALU.mult),
                                 reads=[pb[2].k, rdb.k], writes=[ym.k])
                        if nxt:
                            ld_mq(c + 1)
                        ysrc = (ydc, ysc, ym)
                        for ct in range(8):
                            for br in range(3):
                                pg, pbr, gt = pb[4 + br % 2], pb[6 + br % 2], gate[br % 2]
                                col = br * 1024 + ct * 128
                                mmgroup(pg.t[:, :], pg, [(wg.t[:, kc, col:col + 128], nb.t[:, kc, :]) for kc in range(8)],
                                        [wg.k, nb.k])
                                S.op("act", lambda E, pg=pg, gt=gt, br=br, ct=ct: E.activation(
                                    out=gt.t[:], in_=pg.t[:], func=AF.Sigmoid, bias=bg.t[:, br * 8 + ct:br * 8 + ct + 1]),
                                    reads=[pg.k, bg.k], writes=[gt.k])
                                ysb_ = ysrc[br]
                                mmgroup(pbr.t[:, :], pbr,
                                        [(wb.t[:, br * 4 + kc, ct * 128:(ct + 1) * 128], ysb_.t[:, kc, :]) for kc in range(4)],
                                        [wb.k, ysb_.k])
                                if br == 0:
                                    S.op("dve", lambda E, pbr=pbr, gt=gt: E.tensor_tensor(out=macc.t[:], in0=pbr.t[:],
                                                                                         in1=gt.t[:], op=ALU.mult),
                                         reads=[pbr.k, gt.k], writes=[macc.k])
                                else:
                                    S.op("dve", lambda E, pbr=pbr, gt=gt: E.tensor_tensor(out=mtmp.t[:], in0=pbr.t[:],
                                                                                         in1=gt.t[:], op=ALU.mult),
                                         reads=[pbr.k, gt.k], writes=[mtmp.k])
                                    if br == 1:
                                        S.op("pool", lambda E: E.tensor_tensor(out=macc.t[:], in0=macc.t[:], in1=mtmp.t[:],
                                                                               op=ALU.add),
                                             reads=[macc.k, mtmp.k], writes=[macc.k])
                                    else:
                                        S.op("pool", lambda E, ct=ct: E.tensor_tensor(out=mg.t[:, ct, :], in0=macc.t[:],
                                                                                      in1=mtmp.t[:], op=ALU.add),
                                             reads=[macc.k, mtmp.k], writes=[mg.k])
                        if nxt:
                            ld_merge(c + 1)
                        for ct in range(8):
                            po = pb[ct % 2]
                            mmgroup(po.t[:, :], po, [(wo.t[:, kc, ct * 128:(ct + 1) * 128], mg.t[:, kc, :]) for kc in range(8)],
                                    [wo.k, mg.k])
                            S.op("dve", lambda E, po=po, ct=ct, xsc=xsc: E.tensor_tensor(out=xsc.t[:, ct, :], in0=po.t[:],
                                                                                        in1=xsc.t[:, ct, :], op=ALU.add),
                                 reads=[po.k, xsc.k], writes=[xsc.k])
                        for tb in range(4):
                            for hf in range(2):
                                po = pb[2 + (tb * 2 + hf) % 2]
                                mmgroup(po.t[:, :], po,
                                        [(mg.t[:, kc, tb * 128:(tb + 1) * 128], wo.t[:, kc, hf * 512:(hf + 1) * 512])
                                         for kc in range(8)], [wo.k, mg.k])
                                S.op("dve", lambda E, po=po, tb=tb, hf=hf, xtc=xtc: E.tensor_tensor(
                                    out=xtc.t[:, tb, hf * 512:(hf + 1) * 512], in0=po.t[:],
                                    in1=xtc.t[:, tb, hf * 512:(hf + 1) * 512], op=ALU.add),
                                    reads=[po.k, xtc.k], writes=[xtc.k])
                        S.dma("sp", H2[c * 512:(c + 1) * 512, :].rearrange("(b p) d -> p b d", p=128), xtc.t[:], reads=[xtc.k])
                        if nxt:
                            ld_xt(c + 1)
                        norm_chunk(xsc, sq, xn, sd, rs, gf8, 512)
                        S.dma("sp", XNT[:, :, c * 512:(c + 1) * 512].rearrange("k p t -> p k t"), xn.t[:], reads=[xn.k])
                        if nxt:
                            ld_xs(c + 1)
                    barrier()

        _phaseC()
        def _phaseD():
            if stop_after >= 8:
                with ExitStack() as ph:
                    acc = sb(ph, "accD", [128, 16, 1024], F32)
                    xnh = sb(ph, "xnhD", [128, 8, 2048], BF16)
                    w1 = [sb(ph, f"w1D{i}", [128, 8, 1024], BF16) for i in range(2)]
                    w2 = [sb(ph, f"w2D{i}", [128, 4, 1024], BF16) for i in range(2)]
                    wr = sb(ph, "wrD", [128, 8, 20], BF16)
                    brb = sb(ph, "brD", [128, 20], F32)
                    gfb = sb(ph, "gfbD", [128, 1024], F32)
                    cw = sb(ph, "cwD", [128, 16, 16], F32)
                    hid = [sb(ph, f"hidD{i}", [128, 4, 512], BF16) for i in range(2)]
                    sg = [sb(ph, f"sgD{i}", [128, 512], F32) for i in range(2)]
                    lg = sb(ph, "lgD", [128, 20], F32)
                    elm = sb(ph, "elmD", [128, 16], F32)
                    elm2 = sb(ph, "elm2D", [128, 16], F32)
                    oh1 = sb(ph, "oh1D", [128, 16], F32)
                    oh2 = sb(ph, "oh2D", [128, 16], F32)
                    ohg = sb(ph, "ohgD", [128, 4], F32)
                    egj = sb(ph, "egjD", [128, 4], F32)
                    sm = {nm: sb(ph, nm + "D", [128, 1], F32) for nm in
                          ("gmax", "ngmax", "sumg", "pgrp", "m1", "m2", "dd", "ed", "den", "wa", "wb2", "c1", "c2",
                           "ssum", "sdv", "rsv")}
                    junk = sb(ph, "junkD", [128, 1024], BF16)
                    ot = [sb(ph, f"otD{i}", [128, 1024], F32) for i in range(2)]
                    wload(wr.t[:], w_r.rearrange("(k p) c -> p k c", p=128), wr.k)
                    S.dma("sp", brb.t[:], b_r[0, :].partition_broadcast(128), writes=[brb.k])
                    S.dma("sp", gfb.t[:], gfin[0, :].partition_broadcast(128), writes=[gfb.k])
                    NHALF = 2 if NCH_LIMIT is None else 1
                    NE = 16
                    BIGN = 1e30

                    def small(eng, fn, reads, writes):
                        S.op(eng, fn, reads=[r.k for r in reads], writes=[w.k for w in writes])

                    ecount = 0
                    for hf in range(NHALF):
                        t0 = hf * 2048
                        S.dma("sp", acc.t[:], H2[t0:t0 + 2048, :].rearrange("(b p) d -> p b d", p=128), writes=[acc.k])
                        S.dma("sp", xnh.t[:], XNT[:, :, t0:t0 + 2048].rearrange("k p t -> p k t"), writes=[xnh.k])
                        def router(tb):
                            mmgroup(pb[6].t[:, 0:20], pb[6],
                                    [(xnh.t[:, kc, tb * 128:(tb + 1) * 128], wr.t[:, kc, :]) for kc in range(8)], [xnh.k, wr.k])
                            small("dve", lambda E: E.tensor_tensor(out=lg.t[:], in0=pb[6].t[:, 0:20], in1=brb.t[:], op=ALU.add),
                                  [pb[6], brb], [lg])
                            small("dve", lambda E: E.tensor_reduce(out=sm["gmax"].t[:], in_=lg.t[:, 0:4], axis=AX.X, op=ALU.max),
                                  [lg], [sm["gmax"]])
                            small("dve", lambda E: E.tensor_scalar(out=ohg.t[:], in0=lg.t[:, 0:4], scalar1=sm["gmax"].t[:, 0:1],
                                                                   scalar2=BIGN, op0=ALU.is_lt, op1=ALU.mult),
                                  [lg, sm["gmax"]], [ohg])
                            small("dve", lambda E: E.tensor_scalar(out=sm["ngmax"].t[:], in0=sm["gmax"].t[:], scalar1=-1.0,
                                                                   scalar2=None, op0=ALU.mult), [sm["gmax"]], [sm["ngmax"]])
                            small("act", lambda E: E.activation(out=egj.t[:], in_=lg.t[:, 0:4], func=AF.Exp,
                                                                bias=sm["ngmax"].t[:, 0:1], accum_out=sm["sumg"].t[:]),
                                  [lg, sm["ngmax"]], [egj, sm["sumg"]])
                            small("dve", lambda E: E.reciprocal(out=sm["pgrp"].t[:], in_=sm["sumg"].t[:]), [sm["sumg"]],
                                  [sm["pgrp"]])
                            for gi in range(4):
                                small("dve", lambda E, gi=gi: E.tensor_scalar(
                                    out=elm.t[:, gi * 4:(gi + 1) * 4], in0=lg.t[:, 4 + gi * 4:8 + gi * 4],
                                    scalar1=ohg.t[:, gi:gi + 1], scalar2=None, op0=ALU.subtract), [lg, ohg], [elm])
                            small("dve", lambda E: E.tensor_reduce(out=sm["m1"].t[:], in_=elm.t[:], axis=AX.X, op=ALU.max),
                                  [elm], [sm["m1"]])
                            small("dve", lambda E: E.tensor_scalar(out=oh1.t[:], in0=elm.t[:], scalar1=sm["m1"].t[:, 0:1],
                                                                   scalar2=None, op0=ALU.is_ge), [elm, sm["m1"]], [oh1])
                            small("dve", lambda E: E.scalar_tensor_tensor(out=elm2.t[:], in0=oh1.t[:], scalar=-BIGN, in1=elm.t[:],
                                                                          op0=ALU.mult, op1=ALU.add), [oh1, elm], [elm2])
                            small("dve", lambda E: E.tensor_reduce(out=sm["m2"].t[:], in_=elm2.t[:], axis=AX.X, op=ALU.max),
                                  [elm2], [sm["m2"]])
                            small("dve", lambda E: E.tensor_scalar(out=oh2.t[:], in0=elm2.t[:], scalar1=sm["m2"].t[:, 0:1],
                                                                   scalar2=None, op0=ALU.is_ge), [elm2, sm["m2"]], [oh2])
                            small("dve", lambda E: E.tensor_tensor(out=sm["dd"].t[:], in0=sm["m2"].t[:], in1=sm["m1"].t[:],
                                                                   op=ALU.subtract), [sm["m1"], sm["m2"]], [sm["dd"]])
                            small("act", lambda E: E.activation(out=sm["ed"].t[:], in_=sm["dd"].t[:], func=AF.Exp),
                                  [sm["dd"]], [sm["ed"]])
                            small("dve", lambda E: E.tensor_scalar(out=sm["den"].t[:], in0=sm["ed"].t[:], scalar1=1.0,
                                                                   scalar2=None, op0=ALU.add), [sm["ed"]], [sm["den"]])
                            small("dve", lambda E: E.reciprocal(out=sm["wa"].t[:], in_=sm["den"].t[:]), [sm["den"]], [sm["wa"]])
                            small("dve", lambda E: E.tensor_tensor(out=sm["wb2"].t[:], in0=sm["ed"].t[:], in1=sm["wa"].t[:],
                                                                   op=ALU.mult), [sm["ed"], sm["wa"]], [sm["wb2"]])
                            small("dve", lambda E: E.tensor_tensor(out=sm["c1"].t[:], in0=sm["wa"].t[:], in1=sm["pgrp"].t[:],
                                                                   op=ALU.mult), [sm["wa"], sm["pgrp"]], [sm["c1"]])
                            small("dve", lambda E: E.tensor_tensor(out=sm["c2"].t[:], in0=sm["wb2"].t[:], in1=sm["pgrp"].t[:],
                                                                   op=ALU.mult), [sm["wb2"], sm["pgrp"]], [sm["c2"]])
                            small("dve", lambda E: E.tensor_scalar(out=oh1.t[:], in0=oh1.t[:], scalar1=sm["c1"].t[:, 0:1],
                                                                   scalar2=None, op0=ALU.mult), [oh1, sm["c1"]], [oh1])
                            small("dve", lambda E, tb=tb: E.scalar_tensor_tensor(out=cw.t[:, tb, :], in0=oh2.t[:],
                                                                                 scalar=sm["c2"].t[:, 0:1], in1=oh1.t[:],
                                                                                 op0=ALU.mult, op1=ALU.add),
                                  [oh2, sm["c2"], oh1], [cw])
                        for e in range(NE):
                            wi, wo_ = w1[ecount % 2], w2[ecount % 2]
                            ecount += 1
                            wload(wi.t[:], w_e_in[e].rearrange("(k p) c -> p k c", p=128), wi.k)
                            wload(wo_.t[:], w_e_out[e].rearrange("(k p) c -> p k c", p=128), wo_.k)
                            for ch in range(4):
                                hd = hid[ch % 2]
                                cs = slice(ch * 512, (ch + 1) * 512)
                                for ct in range(4):
                                    pg, pu, sgt = pb[ct % 2], pb[2 + ct % 2], sg[ct % 2]
                                    mmgroup(pg.t[:, :], pg,
                                            [(wi.t[:, kc, ct * 128:(ct + 1) * 128], xnh.t[:, kc, cs]) for kc in range(8)],
                                            [wi.k, xnh.k])
                                    mmgroup(pu.t[:, :], pu,
                                            [(wi.t[:, kc, 512 + ct * 128:512 + (ct + 1) * 128], xnh.t[:, kc, cs])
                                             for kc in range(8)], [wi.k, xnh.k])
                                    S.op("act", lambda E, pg=pg, sgt=sgt: E.activation(out=sgt.t[:], in_=pg.t[:], func=AF.Silu),
                                         reads=[pg.k], writes=[sgt.k])
                                    S.op("dve", lambda E, pu=pu, sgt=sgt, hd=hd, ct=ct: E.tensor_tensor(
                                        out=hd.t[:, ct, :], in0=pu.t[:], in1=sgt.t[:], op=ALU.mult),
                                        reads=[pu.k, sgt.k], writes=[hd.k])
                                for tbl in range(4):
                                    tb = ch * 4 + tbl
                                    if e == 0:
                                        router(tb)
                                    for dh in range(2):
                                        po = pb[4 + (tbl * 2 + dh) % 2]
                                        mmgroup(po.t[:, :], po,
                                                [(hd.t[:, kc, tbl * 128:(tbl + 1) * 128], wo_.t[:, kc, dh * 512:(dh + 1) * 512])
                                                 for kc in range(4)], [hd.k, wo_.k])
                                        S.op("dve", lambda E, po=po, tb=tb, dh=dh, e=e: E.scalar_tensor_tensor(
                                            out=acc.t[:, tb, dh * 512:(dh + 1) * 512], in0=po.t[:], scalar=cw.t[:, tb, e:e + 1],
                                            in1=acc.t[:, tb, dh * 512:(dh + 1) * 512], op0=ALU.mult, op1=ALU.add),
                                            reads=[po.k, cw.k, acc.k], writes=[acc.k])
                        for tb in range(16):
                            o = ot[tb % 2]
                            small("act", lambda E, tb=tb: E.activation(out=junk.t[:], in_=acc.t[:, tb, :], func=AF.Square,
                                                                       accum_out=sm["ssum"].t[:]), [acc], [junk, sm["ssum"]])
                            small("act", lambda E: E.activation(out=sm["sdv"].t[:], in_=sm["ssum"].t[:], func=AF.Sqrt,
                                                                scale=1.0 / 1024, bias=EPS), [sm["ssum"]], [sm["sdv"]])
                            small("dve", lambda E: E.reciprocal(out=sm["rsv"].t[:], in_=sm["sdv"].t[:]), [sm["sdv"]], [sm["rsv"]])
                            small("dve", lambda E, tb=tb, o=o: E.scalar_tensor_tensor(out=o.t[:], in0=acc.t[:, tb, :],
                                                                                      scalar=sm["rsv"].t[:, 0:1], in1=gfb.t[:],
                                                                                      op0=ALU.mult, op1=ALU.mult),
                                  [acc, sm["rsv"], gfb], [o])
                            r0 = t0 + tb * 128
                            S.dma("sp", out_own[r0:r0 + 128, :], o.t[:], reads=[o.k])
                        barrier()

        _phaseD()
        S.finish([])
        globals()['_LAST_S'] = S
        S.emit()
    return nc


def _consts(j):
    ident = np.eye(128, dtype=np.float32)
    rmat = np.zeros((128, 128), np.float32)
    invf = np.zeros((128, 1), np.float32)
    for p in range(128):
        d = p % 64
        if d < 8:
            rmat[p + 8, p] = -1.0
        elif d < 16:
            rmat[p - 8, p] = 1.0
        if d < 16:
            invf[p, 0] = np.float32(500000.0) ** (-np.float32(2 * (d % 8)) / np.float32(16))
    k = np.arange(128)[:, None]
    r = np.arange(128)[None, :]
    tri = (k <= r).astype(np.float32)
    cm_diff = np.zeros((128, 8, 512), np.float32)
    for z in range(8):
        for blk in range(4):
            gi = 2 * blk + j
            if z < gi:
                cm_diff[:, z, blk * 128:(blk + 1) * 128] = 1.0
            elif z == gi:
                cm_diff[:, z, blk * 128:(blk + 1) * 128] = tri
    cm_idx = np.zeros((128, 256), np.float32)
    for cb in range(2):
        if cb == j:
            cm_idx[:, cb * 128:(cb + 1) * 128] = np.where(tri.T > 0, 0.0, -1e30)
        elif cb > j:
            cm_idx[:, cb * 128:(cb + 1) * 128] = -1e30
    return ident, rmat, invf, cm_diff, cm_idx


def _own_idx(j):
    return np.concatenate([np.arange(128) + (2 * m + j) * 128 for m in range(32)])


def make_in_maps(x, positions, mem, g_mix, w_in, b_gate, lambda_q1, lambda_k1, lambda_q2, lambda_k2,
                 g_diff_sub, g_mem, w_mem_kv, w_br_diff, w_br_dsa, w_br_mem, w_out, g_ffn,
                 w_route_group, b_route_group, w_route_expert, b_route_expert, w_exp_in, w_exp_out,
                 g_final, cores=range(8)):
    f = lambda a: np.ascontiguousarray(np.asarray(a))
    x = np.asarray(x); positions = np.asarray(positions); mem = np.asarray(mem)
    col8 = lambda g: f(np.asarray(g).reshape(8, 128).T)
    shared = {
        "w_in": f(np.asarray(w_in)[0]),
        "b_gate24": f(np.asarray(b_gate)[0].reshape(24, 128).T),
        "lam_in": f(np.concatenate([np.asarray(a)[0] for a in (lambda_q1, lambda_k1, lambda_q2, lambda_k2)])[None, :]),
        "gsub_in": f(np.asarray(g_diff_sub)[0][:, None]),
        "gmix8": col8(np.asarray(g_mix)[0]),
        "gmem8": col8(np.asarray(g_mem)[0]),
        "gffn8": col8(np.asarray(g_ffn)[0]),
        "w_mem_kv": f(np.asarray(w_mem_kv)[0]),
        "w_br": f(np.stack([np.asarray(w_br_diff)[0], np.asarray(w_br_dsa)[0], np.asarray(w_br_mem)[0]])),
        "w_out": f(np.asarray(w_out)[0]),
        "w_r": f(np.concatenate([np.asarray(w_route_group)[0], np.asarray(w_route_expert)[0]], axis=1)),
        "b_r": f(np.concatenate([np.asarray(b_route_group)[0], np.asarray(b_route_expert)[0]])[None, :]),
        "w_e_in": f(np.asarray(w_exp_in)[0]),
        "w_e_out": f(np.asarray(w_exp_out)[0]),
        "gfin": f(np.asarray(g_final)[None, :]),
    }
    maps = []
    for c in cores:
        b, j = c // 2, c % 2
        own = _own_idx(j)
        ident, rmat, invf, cm_diff, cm_idx = _consts(j)
        m = dict(shared)
        m.update({
            "xT_full": f(x[b].T), "xT_own": f(x[b][own].T), "x_own": f(x[b][own]),
            "pos_full": f(positions[b][None, :].astype(np.int32)),
            "pos_own": f(positions[b][own][None, :].astype(np.int32)),
            "memT": f(mem[b].T),
            "ident_in": ident, "rmat_in": rmat, "invf_in": invf, "cm_diff_in": cm_diff, "cm_idx_in": cm_idx,
        })
        maps.append(m)
    return maps


_NC_CACHE = {}


def kernel(**inputs):
    if "nc" not in _NC_CACHE:
        _NC_CACHE["nc"] = build()
    nc = _NC_CACHE["nc"]
    maps = make_in_maps(**inputs)
    res = run_bass_kernel_spmd(nc, maps, core_ids=list(range(8)))
    out = np.zeros((4, 8192, 1024), np.float32)
    for c in range(8):
        b, j = c // 2, c % 2
        out[b][_own_idx(j)] = res.results[c]["out_own"]
    return out
```

```python
import numpy as np
from contextlib import ExitStack
import concourse.bass as bass
import concourse.mybir as mybir
from concourse.bass_utils import run_bass_kernel_spmd

F32 = mybir.dt.float32
BF16 = mybir.dt.bfloat16
I32 = mybir.dt.int32
ALU = mybir.AluOpType
AF = mybir.ActivationFunctionType
AX = mybir.AxisListType

SAME_ENGINE_SYNC = True


class Tok:
    __slots__ = ("w", "r", "psum")

    def __init__(self, psum=False):
        self.w = None
        self.r = {}
        self.psum = psum


def toks(n):
    return [Tok() for _ in range(n)]


class Sched:
    ENGS = ("pe", "act", "dve", "pool", "sp")
    CH = 16000
    NDMA = 48

    def __init__(self, nc, es, est_counts):
        self.nc = nc
        self.q = {e: [] for e in self.ENGS}
        self.cnt = {e: 0 for e in self.ENGS}
        self.waited = {e: {} for e in self.ENGS}
        self.sems = {}
        for e in ("pe", "act", "dve", "pool"):
            n = est_counts.get(e, self.CH) // self.CH + 2
            self.sems[e] = [es.enter_context(nc.semaphore(f"s_{e}{i}")) for i in range(n)]
        self.NSW = 16
        self.dsem = [es.enter_context(nc.semaphore(f"s_dma{i}")) for i in range(self.NDMA + self.NSW)]
        self.dk = 0
        self.dk_sw = 0
        self.dlast = {}
        self.nwait = 0
        self.pending_unsig = {e: False for e in self.ENGS}
        self.log = {e: [] for e in self.ENGS}
        self.semname = {}

    def _semval(self, tok):
        if tok[0] == "c":
            _, e, k = tok
            return self.sems[e][(k - 1) // self.CH], (k - 1) % self.CH + 1, ("c", e, (k - 1) // self.CH)
        _, i, v = tok
        return self.dsem[i], v, ("d", i)

    def wait(self, eng, tok):
        if tok is None:
            return
        if tok[0] == "c" and tok[1] == eng:
            if eng == "pe" or not SAME_ENGINE_SYNC:
                return
        sem, val, key = self._semval(tok)
        if self.waited[eng].get(key, 0) >= val:
            return
        self.waited[eng][key] = val
        self.nwait += 1
        self.q[eng].append(lambda E, sem=sem, val=val: E.wait_ge(sem, val))
        self.log[eng].append(('wait', key, val))

    def _deps(self, eng, reads, writes):
        for t in reads:
            self.wait(eng, t.w)
            if t.psum:
                for k, r in t.r.items():
                    if k != eng and k != "pe":
                        self.wait(eng, r)
        for t in writes:
            self.wait(eng, t.w)
            for r in t.r.values():
                self.wait(eng, r)

    def _mark(self, tok, reads, writes):
        for t in reads:
            t.r[tok[1] if tok[0] == "c" else ("d", tok[1])] = tok
        for t in writes:
            t.w = tok
            t.r = {}

    def op(self, eng, fn, reads=(), writes=(), signal=True):
        self._deps(eng, reads, writes)
        if signal:
            self.cnt[eng] += 1
            k = self.cnt[eng]
            tok = ("c", eng, k)
            sem = self.sems[eng][(k - 1) // self.CH]
            self.q[eng].append(lambda E, fn=fn, sem=sem: fn(E).then_inc(sem, 1))
            self.log[eng].append(('op', k))
        else:
            tok = ("c", eng, self.cnt[eng] + 1)
            self.q[eng].append(lambda E, fn=fn: fn(E))
            self.log[eng].append(('op-unsig', self.cnt[eng] + 1))
        self._mark(tok, reads, writes)
        return tok

    def dma(self, eng, out, in_, reads=(), writes=(), **kw):
        self._deps(eng, reads, writes)
        if eng == "pool":
            i = self.NDMA + self.dk_sw % self.NSW
            v = 16 * (self.dk_sw // self.NSW + 1)
            self.dk_sw += 1
        else:
            i = self.dk % self.NDMA
            v = 16 * (self.dk // self.NDMA + 1)
            self.dk += 1
        self.dlast[i] = v
        if v > 16:
            self.wait(eng, ("d", i, v - 16))
        sem = self.dsem[i]
        self.q[eng].append(lambda E, out=out, in_=in_, sem=sem, kw=kw:
                           E.dma_start(out=out, in_=in_, **kw).then_inc(sem, 16))
        tok = ("d", i, v)
        self.log[eng].append(('dma', i, v))
        self._mark(tok, reads, writes)
        return tok

    def finish(self, out_toks):
        for t in out_toks:
            self.wait("sp", t.w)
        for e in ("pe", "act", "dve", "pool"):
            if self.cnt[e]:
                self.wait("sp", ("c", e, self.cnt[e]))

    def emit(self):
        nc = self.nc
        with nc.Block() as block:
            @block.tensor
            def _(E):
                for f in self.q["pe"]:
                    f(E)

            @block.scalar
            def _(E):
                for f in self.q["act"]:
                    f(E)

            @block.vector
            def _(E):
                for f in self.q["dve"]:
                    f(E)

            @block.gpsimd
            def _(E):
                for f in self.q["pool"]:
                    f(E)

            @block.sync
            def _(E):
                for f in self.q["sp"]:
                    f(E)

import math

PI = math.pi
FP8 = mybir.dt.float8e5
NCH_LIMIT = None
ROPE_ADD_ENG = 'pool'
ROPE_LEVEL = 5
EXP = 0
FM_LIMIT = 99
SIG_ALL = False
SKIP_NORM = False
DVE_VAR = 0
EPS = 1e-6
TF = 8192
TO = 4096
CW1 = 6.28125
CW2 = 2.0 * math.pi - 6.28125
C_DQ, C_DK, C_DV, C_SQ, C_SK, C_SV, C_IQ, C_IK, C_IW, C_MQ, C_GL = (
    0, 512, 1024, 1536, 2048, 2560, 3072, 3584, 3648, 3656, 4168)


class Buf:
    __slots__ = ("t", "k")

    def __init__(self, t):
        self.t = t
        self.k = Tok()


def build(debug=False, stop_after=99, norope=False, notm=False, nofm=False, notab=False, tabonly=False):
    nc = bass.Bass("TRN2", target_bir_lowering=False)

    def din(name, shape, dt=F32):
        return nc.dram_tensor(name, list(shape), dt, kind="ExternalInput").ap()

    def dscr(name, shape, dt):
        return nc.dram_tensor(name, list(shape), dt,
                              kind=("ExternalOutput" if debug else "Internal")).ap()

    xT_full = din("xT_full", [1024, TF])
    xT_own = din("xT_own", [1024, TO])
    x_own = din("x_own", [TO, 1024])
    pos_full = din("pos_full", [1, TF], I32)
    pos_own = din("pos_own", [1, TO], I32)
    memT = din("memT", [1024, 256])
    w_in = din("w_in", [1024, 7240])
    b_gate24 = din("b_gate24", [128, 24])
    lam_in = din("lam_in", [1, 256])
    gsub_in = din("gsub_in", [128, 1])
    gmix8 = din("gmix8", [128, 8])
    gmem8 = din("gmem8", [128, 8])
    gffn8 = din("gffn8", [128, 8])
    w_mem_kv = din("w_mem_kv", [1024, 1024])
    w_br = din("w_br", [3, 512, 1024])
    w_out = din("w_out", [1024, 1024])
    w_r = din("w_r", [1024, 20])
    b_r = din("b_r", [1, 20])
    w_e_in = din("w_e_in", [16, 1024, 1024])
    w_e_out = din("w_e_out", [16, 512, 1024])
    gfin = din("gfin", [1, 1024])
    ident_in = din("ident_in", [128, 128])
    rmat_in = din("rmat_in", [128, 128])
    invf_in = din("invf_in", [128, 1])
    cm_diff_in = din("cm_diff_in", [128, 8, 512])
    cm_idx_in = din("cm_idx_in", [128, 256])
    out_own = nc.dram_tensor("out_own", [TO, 1024], F32, kind="ExternalOutput").ap()

    NTF = dscr("NTF", [8, 128, TF], BF16)
    NTO = dscr("NTO", [8, 128, TO], BF16)
    KTD = dscr("KTD", [4, 128, TF], BF16)
    IKT = dscr("IKT", [128, TF], BF16)
    VD = dscr("VD", [TF, 512], BF16)
    KTS = dscr("KTS", [4, 128, TF], BF16)
    VAUG = dscr("VAUG", [TF, 1024], BF16)
    QTD = dscr("QTD", [4, 128, TO], BF16)
    QTS = dscr("QTS", [4, 128, TO], BF16)
    IQT = dscr("IQT", [4, 128, TO], BF16)
    MQT = dscr("MQT", [4, 128, TO], BF16)
    IW = dscr("IW", [TO, 8], F32)
    YD = dscr("YD", [4, 128, TO], BF16)
    YS = dscr("YS", [4, 128, TO], BF16)
    XNT = dscr("XNT", [8, 128, TO], BF16)
    H2 = dscr("H2", [TO, 1024], F32)

    with ExitStack() as es:
        S = Sched(nc, es, {"pe": 120000, "act": 40000, "dve": 60000, "pool": 20000})

        def sb(st, name, shape, dt):
            return Buf(st.enter_context(nc.sbuf_tensor(name, list(shape), dt)))

        pb = [Buf(es.enter_context(nc.psum_tensor(f"pb{i}", [128, 512], F32))) for i in range(8)]
        for b_ in pb:
            b_.k.psum = True

        ident = sb(es, "ident", [128, 128], BF16)
        rmat = sb(es, "rmat", [128, 128], BF16)
        ones = sb(es, "ones", [128, 128], BF16)
        zeros = sb(es, "zeros", [128, 512], BF16)
        invf = sb(es, "invf", [128, 1], F32)
        S.dma("pool", ident.t[:], ident_in, writes=[ident.k])
        S.dma("pool", rmat.t[:], rmat_in, writes=[rmat.k])
        S.dma("sp", invf.t[:], invf_in, writes=[invf.k])
        S.op("pool", lambda E: E.memset(ones.t[:], 1.0), writes=[ones.k])
        S.op("pool", lambda E: E.memset(zeros.t[:], 0.0), writes=[zeros.k])

        def barrier():
            for e in S.ENGS:
                for p in ("pe", "act", "dve", "pool"):
                    if p != e and S.cnt[p]:
                        S.wait(e, ("c", p, S.cnt[p]))
                for i, v in S.dlast.items():
                    S.wait(e, ("d", i, v))

        def mmgroup(out_ap, pbuf, pairs, rtoks, start=True, stop=True, sig_last=True, **kw):
            n = len(pairs)
            for i, (l, r) in enumerate(pairs):
                S.op("pe", lambda E, l=l, r=r, i=i: E.matmul(
                    out_ap, l, r, start=(start and i == 0), stop=(stop and i == n - 1), **kw),
                    reads=rtoks, writes=[pbuf.k], signal=((sig_last and i == n - 1) or SIG_ALL))

        def wload(dst_ap, src_ap, tok):
            S.dma("pool", dst_ap, src_ap, writes=[tok])

        def norm_chunk(xs, sq, nt, sd, rs, g8, w):
            S.op("act", lambda E: E.activation(out=sq.t[:, :, :w], in_=xs.t[:, :, :w], func=AF.Square),
                 reads=[xs.k], writes=[sq.k])
            mmgroup(pb[7].t[:, :w], pb[7], [(ones.t[:], sq.t[:, kc, :w]) for kc in range(8)], [sq.k, ones.k])
            S.op("act", lambda E: E.activation(out=sd.t[:, :w], in_=pb[7].t[:, :w], func=AF.Sqrt,
                                               scale=1.0 / 1024, bias=EPS),
                 reads=[pb[7].k], writes=[sd.k])
            S.op("dve", lambda E: E.reciprocal(out=rs.t[:, :w], in_=sd.t[:, :w]), reads=[sd.k], writes=[rs.k])
            for kc in range(8):
                S.op("dve", lambda E, kc=kc: E.scalar_tensor_tensor(
                    out=nt.t[:, kc, :w], in0=xs.t[:, kc, :w], scalar=g8.t[:, kc:kc + 1], in1=rs.t[:, :w],
                    op0=ALU.mult, op1=ALU.mult), reads=[xs.k, rs.k, g8.k], writes=[nt.k])

        def phase_norm(xT, T, g_in, dest, tag):
            with ExitStack() as ph:
                g8 = sb(ph, "g8" + tag, [128, 8], F32)
                S.dma("sp", g8.t[:], g_in, writes=[g8.k])
                xs = [sb(ph, f"xs{tag}{i}", [128, 8, 512], F32) for i in range(2)]
                sq = [sb(ph, f"sq{tag}{i}", [128, 8, 512], BF16) for i in range(2)]
                nt = [sb(ph, f"nt{tag}{i}", [128, 8, 512], BF16) for i in range(2)]
                sd = sb(ph, "sd" + tag, [128, 512], F32)
                rs = sb(ph, "rs" + tag, [128, 512], F32)
                nch = T // 512

                def ld(c):
                    S.dma("sp", xs[c % 2].t[:], xT[:, c * 512:(c + 1) * 512].rearrange("(k p) t -> p k t", p=128),
                          writes=[xs[c % 2].k])
                ld(0)
                for c in range(nch):
                    if c + 1 < nch:
                        ld(c + 1)
                    i = c % 2
                    norm_chunk(xs[i], sq[i], nt[i], sd, rs, g8, 512)
                    S.dma("sp", dest[:, :, c * 512:(c + 1) * 512].rearrange("k p t -> p k t"), nt[i].t[:],
                          reads=[nt[i].k])
                barrier()

        if not SKIP_NORM:
            phase_norm(xT_full, TF, gmix8, NTF, "f")
        if stop_after >= 1 and not SKIP_NORM:
            phase_norm(xT_own, TO, gmix8, NTO, "o")

        def phase_proj(nT_dram, T, pos_dram, wcols, W, fm_specs, tm_specs, tag):
            with ExitStack() as ph:
                wt = sb(ph, "wt" + tag, [128, 8, W], BF16)
                for (dst0, src0, n) in wcols:
                    wload(wt.t[:, :, dst0:dst0 + n],
                          w_in[:, src0:src0 + n].rearrange("(k p) c -> p k c", p=128), wt.k)
                ntc = [sb(ph, f"ntc{tag}{i}", [128, 8, 512], BF16) for i in range(2)]
                posi = sb(ph, "posi" + tag, [128, T], I32)
                S.dma("sp", posi.t[:], pos_dram[0, :].partition_broadcast(128), writes=[posi.k])
                pf = sb(ph, "pf" + tag, [128, 512], F32)
                ang = sb(ph, "ang" + tag, [128, 512], F32)
                ki = sb(ph, "ki" + tag, [128, 512], I32)
                kf = sb(ph, "kf" + tag, [128, 512], F32)
                rr = sb(ph, "rr" + tag, [128, 512], F32)
                tt = sb(ph, "tt" + tag, [128, 512], F32)
                Ccs = [sb(ph, f"Cc{tag}{i}", [128, 512], F32) for i in range(2)]
                Scs = [sb(ph, f"Sc{tag}{i}", [128, 512], F32) for i in range(2)]
                qbb = [sb(ph, f"qbb{tag}{i}", [128, 512], BF16) for i in range(2)]
                t1b = [sb(ph, f"t1b{tag}{i}", [128, 512], F32) for i in range(2)]
                t2b = [sb(ph, f"t2b{tag}{i}", [128, 512], F32) for i in range(2)]
                otb = [sb(ph, f"otb{tag}{i}", [128, 512], BF16) for i in range(3)]
                vst = [sb(ph, f"vst{tag}{i}", [128, 512], BF16) for i in range(2)]
                vau = [sb(ph, f"vau{tag}{i}", [128, 8, 128], BF16) for i in range(2)]
                iwst = [sb(ph, f"iwst{tag}{i}", [128, 8], F32) for i in range(2)]
                for i in range(2):
                    S.op("pool", lambda E, i=i: E.memset(vau[i].t[:], 1.0), writes=[vau[i].k])
                nch = T // 512 if NCH_LIMIT is None else (NCH_LIMIT * 2 if T == TF else NCH_LIMIT)

                def ld(c):
                    S.dma("sp", ntc[c % 2].t[:], nT_dram[:, :, c * 512:(c + 1) * 512].rearrange("k p t -> p k t"),
                          writes=[ntc[c % 2].k])

                def tables_dve(c):
                    S.op("dve", lambda E, c=c: E.tensor_copy(out=pf.t[:], in_=posi.t[:, c * 512:(c + 1) * 512]),
                         reads=[posi.k], writes=[pf.k])
                    S.op("dve", lambda E: E.tensor_scalar(out=ang.t[:], in0=pf.t[:], scalar1=invf.t[:, 0:1],
                                                          scalar2=None, op0=ALU.mult),
                         reads=[pf.k, invf.k], writes=[ang.k])
                    S.op("dve", lambda E: E.tensor_scalar(out=ki.t[:], in0=ang.t[:], scalar1=1.0 / (2 * PI),
                                                          scalar2=None, op0=ALU.mult),
                         reads=[ang.k], writes=[ki.k])
                    S.op("dve", lambda E: E.tensor_copy(out=kf.t[:], in_=ki.t[:]), reads=[ki.k], writes=[kf.k])
                    S.op("dve", lambda E: E.scalar_tensor_tensor(out=rr.t[:], in0=kf.t[:], scalar=-CW1, in1=ang.t[:],
                                                                 op0=ALU.mult, op1=ALU.add),
                         reads=[kf.k, ang.k], writes=[rr.k])
                    S.op("dve", lambda E: E.scalar_tensor_tensor(out=rr.t[:], in0=kf.t[:], scalar=-CW2, in1=rr.t[:],
                                                                 op0=ALU.mult, op1=ALU.add),
                         reads=[kf.k, rr.k], writes=[rr.k])
                    S.op("dve", lambda E: E.tensor_scalar(out=tt.t[:], in0=rr.t[:], scalar1=PI, scalar2=-2 * PI,
                                                          op0=ALU.is_gt, op1=ALU.mult), reads=[rr.k], writes=[tt.k])
                    S.op("dve", lambda E: E.tensor_tensor(out=rr.t[:], in0=rr.t[:], in1=tt.t[:], op=ALU.add),
                         reads=[rr.k, tt.k], writes=[rr.k])
                    S.op("dve", lambda E: E.tensor_scalar(out=tt.t[:], in0=rr.t[:], scalar1=-PI, scalar2=2 * PI,
                                                          op0=ALU.is_lt, op1=ALU.mult), reads=[rr.k], writes=[tt.k])
                    S.op("dve", lambda E: E.tensor_tensor(out=rr.t[:], in0=rr.t[:], in1=tt.t[:], op=ALU.add),
                         reads=[rr.k, tt.k], writes=[rr.k])
                    S.op("dve", lambda E: E.tensor_scalar(out=rr.t[:], in0=rr.t[:], scalar1=-3.141592, scalar2=3.141592,
                                                          op0=ALU.max, op1=ALU.min), reads=[rr.k], writes=[rr.k])

                def tables_act(c):
                    Cc, Sc = Ccs[c % 2], Scs[c % 2]
                    S.op("act", lambda E: E.activation(out=Sc.t[:], in_=rr.t[:], func=AF.Sin),
                         reads=[rr.k], writes=[Sc.k])
                    S.op("act", lambda E: E.activation(out=tt.t[:], in_=rr.t[:], func=AF.Abs),
                         reads=[rr.k], writes=[tt.k])
                    S.op("act", lambda E: E.activation(out=Cc.t[:], in_=tt.t[:], func=AF.Sin, scale=-1.0, bias=PI / 2),
                         reads=[tt.k], writes=[Cc.k])

                ld(0)
                nst = 0
                for c in range(nch):
                    if c + 1 < nch:
                        ld(c + 1)
                    nb = ntc[c % 2]
                    use_tab = any(sp[2] for sp in fm_specs) and not norope and not notab
                    if use_tab and c == 0:
                        tables_dve(0)
                        tables_act(0)
                    if use_tab and c + 1 < nch:
                        tables_dve(c + 1)
                    Cc, Sc = Ccs[c % 2], Scs[c % 2]
                    pabanks = (pb[0], pb[1], pb[6])
                    pending = None

                    def rope_tail(p):
                        ti_, pa_, qb_, ot_, dst_, Cc_, Sc_ = p
                        pr = pb[2 + ti_ % 2]
                        t1, t2 = t1b[ti_ % 2], t2b[ti_ % 2]
                        mmgroup(pr.t[:, :], pr, [(rmat.t[:], qb_.t[:])], [rmat.k, qb_.k])
                        S.op("dve", lambda E: E.tensor_tensor(out=t1.t[:], in0=pa_.t[:], in1=Cc_.t[:], op=ALU.mult),
                             reads=[pa_.k, Cc_.k], writes=[t1.k])
                        S.op("dve", lambda E: E.tensor_tensor(out=t2.t[:], in0=pr.t[:], in1=Sc_.t[:], op=ALU.mult),
                             reads=[pr.k, Sc_.k], writes=[t2.k])
                        S.op(ROPE_ADD_ENG, lambda E: E.tensor_tensor(out=ot_.t[:], in0=t1.t[:], in1=t2.t[:],
                                                                     op=ALU.add),
                             reads=[t1.k, t2.k], writes=[ot_.k])
                        S.dma("sp", dst_, ot_.t[:], reads=[ot_.k])

                    for ti, (wcol, dest, rope) in enumerate([] if nofm else fm_specs[:FM_LIMIT]):
                        rope = rope and not norope and not tabonly
                        pa = pabanks[ti % 3]
                        mmgroup(pa.t[:, :], pa, [(wt.t[:, kc, wcol:wcol + 128], nb.t[:, kc, :]) for kc in range(8)],
                                [wt.k, nb.k])
                        ot = otb[nst % 3]
                        nst += 1
                        if rope:
                            qb = qbb[ti % 2]
                            S.op("act", lambda E, qb=qb, pa=pa: E.activation(out=qb.t[:], in_=pa.t[:], func=AF.Copy),
                                 reads=[pa.k], writes=[qb.k])
                        else:
                            S.op("act", lambda E, ot=ot, pa=pa: E.activation(out=ot.t[:], in_=pa.t[:], func=AF.Copy),
                                 reads=[pa.k], writes=[ot.k])
                            S.dma("sp", dest(c), ot.t[:], reads=[ot.k])
                        if pending is not None:
                            rope_tail(pending)
                        pending = (ti, pa, qb, ot, dest(c), Cc, Sc) if rope else None
                    if pending is not None:
                        rope_tail(pending)
                    if use_tab and c + 1 < nch:
                        tables_act(c + 1)
                    for (wcol, ncol, kind) in ([] if notm else tm_specs):
                        for tb in range(4):
                            pv = pb[4 + tb % 2]
                            mmgroup(pv.t[:, :ncol], pv,
                                    [(nb.t[:, kc, tb * 128:(tb + 1) * 128], wt.t[:, kc, wcol:wcol + ncol])
                                     for kc in range(8)], [wt.k, nb.k])
                            r0 = (c * 4 + tb) * 128
                            if kind == "vd":
                                st_ = vst[tb % 2]
                                S.op("act", lambda E, st_=st_, pv=pv: E.activation(out=st_.t[:], in_=pv.t[:],
                                                                                  func=AF.Copy),
                                     reads=[pv.k], writes=[st_.k])
                                S.dma("sp", VD[r0:r0 + 128, :], st_.t[:], reads=[st_.k])
                            elif kind == "vaug":
                                st_ = vau[tb % 2]
                                S.op("act", lambda E, st_=st_, pv=pv: E.activation(
                                    out=st_.t[:, :, 0:64], in_=pv.t[:, :].rearrange("p (h d) -> p h d", h=8),
                                    func=AF.Copy), reads=[pv.k], writes=[st_.k])
                                S.dma("sp", VAUG[r0:r0 + 128, :].rearrange("p (h d) -> p h d", h=8), st_.t[:],
                                      reads=[st_.k])
                            else:
                                st_ = iwst[tb % 2]
                                S.op("act", lambda E, st_=st_, pv=pv: E.activation(out=st_.t[:], in_=pv.t[:, 0:8],
                                                                                  func=AF.Copy),
                                     reads=[pv.k], writes=[st_.k])
                                S.dma("sp", IW[r0:r0 + 128, :], st_.t[:], reads=[st_.k])
                barrier()

        def fm_dest(T4, h):
            return lambda c: T4[h, :, c * 512:(c + 1) * 512]

        if stop_after >= 2:
          phase_proj(NTF, TF, pos_full,
                   [(0, C_DK, 512), (512, C_IK, 64), (576, C_IK, 64), (640, C_DV, 512)], 1152,
                   [(h * 128, fm_dest(KTD, h), True) for h in range(4)]
                   + [(512, (lambda c: IKT[:, c * 512:(c + 1) * 512]), True)],
                   [(640, 512, "vd")], "p1")
        if stop_after >= 3:
          phase_proj(NTF, TF, pos_full,
                   [(0, C_SK, 512), (512, C_SV, 512)], 1024,
                   [(h * 128, fm_dest(KTS, h), True) for h in range(4)],
                   [(512, 512, "vaug")], "p2")
        if stop_after >= 4:
          phase_proj(NTO, TO, pos_own,
                   [(0, C_DQ, 512), (512, C_SQ, 512), (1024, C_IQ, 512), (1536, C_MQ, 512), (2048, C_IW, 8)], 2056,
                   [(h * 128, fm_dest(QTD, h), True) for h in range(4)]
                   + [(512 + h * 128, fm_dest(QTS, h), True) for h in range(4)]
                   + [(1024 + h * 128, fm_dest(IQT, h), True) for h in range(4)]
                   + [(1536 + h * 128, fm_dest(MQT, h), False) for h in range(4)],
                   [(2048, 8, "iw")], "p3")
        def _phaseA():
            if stop_after >= 5:
                with ExitStack() as ph:
                    KT = sb(ph, "KTa", [128, 4, TF], BF16)
                    V = sb(ph, "Va", [128, 64, 512], BF16)
                    cmd = sb(ph, "cmd", [128, 8, 512], BF16)
                    lamb = sb(ph, "lamb", [128, 4, 64], F32)
                    lp = sb(ph, "lp", [128, 2, 64], F32)
                    ls = sb(ph, "ls", [128, 2], F32)
                    le = sb(ph, "le", [128, 2], F32)
                    neglam = sb(ph, "neglam", [128, 1], F32)
                    gsub = sb(ph, "gsub", [128, 1], F32)
                    gs08 = sb(ph, "gs08", [128, 1], F32)
                    for h in range(4):
                        S.dma("sp", KT.t[:, h, :], KTD[h], writes=[KT.k])
                    for q4 in range(4):
                        S.dma("sp", V.t[:, q4 * 16:(q4 + 1) * 16, :],
                              VD[q4 * 2048:(q4 + 1) * 2048, :].rearrange("(kb p) c -> p kb c", p=128), writes=[V.k])
                    wload(cmd.t[:], cm_diff_in, cmd.k)
                    S.dma("sp", lamb.t[:].rearrange("p a d -> p (a d)"), lam_in[0, :].partition_broadcast(128), writes=[lamb.k])
                    S.dma("sp", gsub.t[:], gsub_in, writes=[gsub.k])
                    for a in range(2):
                        S.op("dve", lambda E, a=a: E.tensor_tensor(out=lp.t[:, a, :], in0=lamb.t[:, 2 * a, :],
                                                                  in1=lamb.t[:, 2 * a + 1, :], op=ALU.mult),
                             reads=[lamb.k], writes=[lp.k])
                        S.op("dve", lambda E, a=a: E.reduce_sum(out=ls.t[:, a:a + 1], in_=lp.t[:, a, :], axis=AX.X),
                             reads=[lp.k], writes=[ls.k])
                    S.op("act", lambda E: E.activation(out=le.t[:], in_=ls.t[:], func=AF.Exp), reads=[ls.k], writes=[le.k])
                    S.op("dve", lambda E: E.tensor_tensor(out=neglam.t[:], in0=le.t[:, 1:2], in1=le.t[:, 0:1], op=ALU.subtract),
                         reads=[le.k], writes=[neglam.k])
                    S.op("dve", lambda E: E.tensor_scalar(out=neglam.t[:], in0=neglam.t[:], scalar1=-0.2, scalar2=None,
                                                          op0=ALU.add), reads=[neglam.k], writes=[neglam.k])
                    S.op("dve", lambda E: E.tensor_scalar(out=gs08.t[:], in0=gsub.t[:], scalar1=0.8, scalar2=None,
                                                          op0=ALU.mult), reads=[gsub.k], writes=[gs08.k])
                    QP = [[sb(ph, f"QPa{i}_{mp}", [128, 4, 512], BF16) for mp in range(2)] for i in range(2)]
                    for i in range(2):
                        for mp in range(2):
                            S.op("pool", lambda E, i=i, mp=mp: E.memset(QP[i][mp].t[:], 0.0), writes=[QP[i][mp].k])
                    pT = [sb(ph, f"pTa{i}", [128, 512], BF16) for i in range(4)]
                    rdb = sb(ph, "rdba", [128, 512], F32)
                    o1b = sb(ph, "o1b", [128, 512], F32)
                    tb_ = sb(ph, "tba", [128, 512], F32)
                    yb = sb(ph, "yba", [128, 512], F32)
                    ysq = sb(ph, "ysqa", [128, 512], BF16)
                    sdn = sb(ph, "sdna", [128, 512], F32)
                    rn = sb(ph, "rna", [128, 512], F32)
                    yob = [sb(ph, f"yoba{i}", [128, 512], BF16) for i in range(2)]
                    NG = 8 if NCH_LIMIT is None else NCH_LIMIT

                    def ldq(g):
                        for mp in range(2):
                            r0 = mp * 64
                            S.dma("sp", QP[g % 2][mp].t[r0:r0 + 64, :, :],
                                  QTD[:, r0:r0 + 64, g * 512:(g + 1) * 512].rearrange("h p t -> p h t"),
                                  writes=[QP[g % 2][mp].k])
                    ldq(0)
                    for g in range(NG):
                        if g + 1 < NG:
                            ldq(g + 1)
                        nkb = 8 * g + 8
                        for h in range(4):
                            for mp in range(2):
                                po, pd = pb[2 + mp], pb[4 + mp]
                                Q = QP[g % 2][mp]
                                lo_, hi_ = mp * 64, (mp + 1) * 64
                                psb = (pb[0], pb[1], pb[7])
                                for step in range(nkb + 2):
                                    if step < nkb:
                                        kb = step
                                        ps, pt = psb[kb % 3], pT[kb % 4]
                                        c0 = ((kb - 8 * g) // 2) * 128 if kb >= 8 * g else 0
                                        S.op("pe", lambda E, ps=ps, kb=kb, h=h, Q=Q, c0=c0: E.matmul(
                                            ps.t[:, c0:], KT.t[:, h, kb * 128:(kb + 1) * 128], Q.t[:, h, c0:],
                                            start=True, stop=True), reads=[KT.k, Q.k], writes=[ps.k])
                                        S.op("act", lambda E, ps=ps, pt=pt, c0=c0: E.activation(out=pt.t[:, c0:], in_=ps.t[:, c0:],
                                                                                               func=AF.Exp, scale=0.125),
                                             reads=[ps.k], writes=[pt.k])
                                        if kb >= 8 * g:
                                            z = kb - 8 * g
                                            S.op("dve", lambda E, pt=pt, z=z, c0=c0: E.tensor_tensor(
                                                out=pt.t[:, c0:], in0=pt.t[:, c0:], in1=cmd.t[:, z, c0:], op=ALU.mult),
                                                 reads=[pt.k, cmd.k], writes=[pt.k])
                                    if step >= 2:
                                        kb = step - 2
                                        pt = pT[kb % 4]
                                        c0 = ((kb - 8 * g) // 2) * 128 if kb >= 8 * g else 0
                                        S.op("pe", lambda E, po=po, pt=pt, kb=kb, h=h, nkb=nkb, c0=c0: E.matmul(
                                            po.t[:, c0:], V.t[:, kb, h * 128:(h + 1) * 128], pt.t[:, c0:],
                                            start=(kb == 0), stop=(kb == nkb - 1)), reads=[V.k, pt.k], writes=[po.k],
                                            signal=False)
                                        S.op("pe", lambda E, pd=pd, pt=pt, kb=kb, nkb=nkb, c0=c0: E.matmul(
                                            pd.t[:, c0:], ones.t[:], pt.t[:, c0:], start=(kb == 0), stop=(kb == nkb - 1)),
                                            reads=[ones.k, pt.k], writes=[pd.k])
                                S.op("dve", lambda E, pd=pd: E.reciprocal(out=rdb.t[:], in_=pd.t[:]), reads=[pd.k], writes=[rdb.k])
                                if mp == 0:
                                    S.op("dve", lambda E, po=po: E.tensor_tensor(out=o1b.t[:], in0=po.t[:], in1=rdb.t[:],
                                                                                op=ALU.mult),
                                         reads=[po.k, rdb.k], writes=[o1b.k])
                                else:
                                    S.op("dve", lambda E, po=po: E.tensor_tensor(out=tb_.t[:], in0=po.t[:], in1=rdb.t[:],
                                                                                op=ALU.mult),
                                         reads=[po.k, rdb.k], writes=[tb_.k])
                            yo = yob[h % 2]
                            S.op("dve", lambda E: E.scalar_tensor_tensor(out=yb.t[:], in0=tb_.t[:], scalar=neglam.t[:, 0:1],
                                                                         in1=o1b.t[:], op0=ALU.mult, op1=ALU.add),
                                 reads=[tb_.k, neglam.k, o1b.k], writes=[yb.k])
                            S.op("act", lambda E: E.activation(out=ysq.t[:], in_=yb.t[:], func=AF.Square),
                                 reads=[yb.k], writes=[ysq.k])
                            mmgroup(pb[6].t[:, :], pb[6], [(ones.t[:], ysq.t[:])], [ones.k, ysq.k])
                            S.op("act", lambda E: E.activation(out=sdn.t[:], in_=pb[6].t[:], func=AF.Sqrt, scale=1.0 / 128,
                                                               bias=EPS), reads=[pb[6].k], writes=[sdn.k])
                            S.op("dve", lambda E: E.reciprocal(out=rn.t[:], in_=sdn.t[:]), reads=[sdn.k], writes=[rn.k])
                            S.op("dve", lambda E, yo=yo: E.scalar_tensor_tensor(out=yo.t[:], in0=yb.t[:], scalar=gs08.t[:, 0:1],
                                                                                in1=rn.t[:], op0=ALU.mult, op1=ALU.mult),
                                 reads=[yb.k, gs08.k, rn.k], writes=[yo.k])
                            S.dma("sp", YD[h, :, g * 512:(g + 1) * 512], yo.t[:], reads=[yo.k])
                    barrier()

        _phaseA()
        def _phaseB():
            if stop_after >= 6:
                with ExitStack() as ph:
                    KT = sb(ph, "KTb", [128, 4, TF], BF16)
                    IK = sb(ph, "IKb", [128, TF], BF16)
                    scores = [sb(ph, f"score{i}", [128, TF], F32) for i in range(2)]
                    maskbs = [sb(ph, f"maskb{i}", [128, TF], FP8) for i in range(2)]
                    NV = 8
                    Vr = [sb(ph, f"Vr{i}", [128, 1024], BF16) for i in range(NV)]
                    cmi = sb(ph, "cmi", [128, 256], F32)
                    for h in range(4):
                        S.dma("sp", KT.t[:, h, :], KTS[h], writes=[KT.k])
                    S.dma("sp", IK.t[:], IKT, writes=[IK.k])
                    S.dma("sp", cmi.t[:], cm_idx_in, writes=[cmi.k])
                    QS = [[sb(ph, f"QSb{i}_{par}", [128, 4, 512], BF16) for par in range(2)] for i in range(1)]
                    QI = [[sb(ph, f"QIb{i}_{par}", [128, 4, 512], BF16) for par in range(2)] for i in range(1)]
                    for i in range(1):
                        for par in range(2):
                            S.op("pool", lambda E, i=i, par=par: E.memset(QS[i][par].t[:], 0.0), writes=[QS[i][par].k])
                            S.op("pool", lambda E, i=i, par=par: E.memset(QI[i][par].t[:], 0.0), writes=[QI[i][par].k])
                    IWs = [sb(ph, f"IWb{i}", [128, 4, 8], F32) for i in range(1)]
                    Dh = sb(ph, "Dh", [128, 8, 128], BF16)
                    Rb = [sb(ph, f"Rb{i}", [128, 512], BF16) for i in range(3)]
                    pT = [sb(ph, f"pTb{i}", [128, 512], BF16) for i in range(4)]
                    rmax = sb(ph, "rmax", [128, 1], F32)
                    rmin = sb(ph, "rmin", [128, 1], F32)
                    span = sb(ph, "span", [128, 1], F32)
                    lo = sb(ph, "lo", [128, 1], F32)
                    mid = sb(ph, "mid", [128, 1], F32)
                    cnt = sb(ph, "cnt", [128, 1], F32)
                    gs = sb(ph, "gs", [128, 1], F32)
                    rdb = sb(ph, "rdbb", [128, 128], F32)
                    ysb = [sb(ph, f"ysb{i}", [128, 4, 128], BF16) for i in range(2)]
                    NG = 8 if NCH_LIMIT is None else NCH_LIMIT
                    NIT = 14
                    CSC = (8.0 ** -0.5) * (64.0 ** -0.5)

                    def ldq_i(g):
                        for par in range(2):
                            r0 = par * 64
                            S.dma("sp", QI[0][par].t[r0:r0 + 64, :, :],
                                  IQT[:, r0:r0 + 64, g * 512:(g + 1) * 512].rearrange("h p t -> p h t"), writes=[QI[0][par].k])
                        S.dma("sp", IWs[0].t[:], IW[g * 512:(g + 1) * 512, :].rearrange("(b p) h -> p b h", p=128),
                              writes=[IWs[0].k])

                    def ldq_s(g):
                        for par in range(2):
                            r0 = par * 64
                            S.dma("sp", QS[0][par].t[r0:r0 + 64, :, :],
                                  QTS[:, r0:r0 + 64, g * 512:(g + 1) * 512].rearrange("h p t -> p h t"), writes=[QS[0][par].k])
                    vcount = 0
                    blocks = [(g, blk) for g in range(NG) for blk in range(4)]

                    def stage1a(t):
                        g, blk = blocks[t]
                        if blk == 0:
                            ldq_i(g)
                        Qi, Iw = QI[0], IWs[0]
                        score = scores[t % 2]
                        m = 4 * g + blk
                        nkb = 2 * m + 2
                        n = nkb * 128
                        qs = slice(blk * 128, (blk + 1) * 128)
                        for h in range(8):
                            S.op("dve", lambda E, h=h, Iw=Iw, blk=blk: E.tensor_scalar(
                                out=Dh.t[:, h, :], in0=ident.t[:], scalar1=Iw.t[:, blk, h:h + 1], scalar2=CSC,
                                op0=ALU.mult, op1=ALU.mult), reads=[ident.k, Iw.k], writes=[Dh.k])
                        yield
                        nch = (nkb + 3) // 4
                        psc = pb[2]
                        items = [(c, h) for c in range(nch) for h in range(8)]
                        for idx in range(len(items) + 1):
                            if idx < len(items):
                                c, h = items[idx]
                                w = min(512, n - c * 512)
                                pa, rb = pb[h % 2], Rb[h % 3]
                                Qih = Qi[h % 2]
                                S.op("pe", lambda E, pa=pa, h=h, c=c, w=w, Qih=Qih, qs=qs: E.matmul(
                                    pa.t[:, :w], Qih.t[:, h // 2, qs], IK.t[:, c * 512:c * 512 + w],
                                    start=True, stop=True), reads=[Qih.k, IK.k], writes=[pa.k])
                                S.op("act", lambda E, pa=pa, rb=rb, w=w: E.activation(out=rb.t[:, :w], in_=pa.t[:, :w],
                                                                                     func=AF.Relu),
                                     reads=[pa.k], writes=[rb.k])
                            if idx >= 1:
                                c, h = items[idx - 1]
                                w = min(512, n - c * 512)
                                rb = Rb[h % 3]
                                S.op("pe", lambda E, psc=psc, h=h, rb=rb, w=w: E.matmul(
                                    psc.t[:, :w], Dh.t[:, h, :], rb.t[:, :w], start=(h == 0), stop=(h == 7)),
                                    reads=[Dh.k, rb.k], writes=[psc.k], signal=(h == 7))
                                if h == 7:
                                    S.op("act", lambda E, psc=psc, c=c, w=w, score=score: E.activation(
                                        out=score.t[:, c * 512:c * 512 + w], in_=psc.t[:, :w], func=AF.Copy),
                                        reads=[psc.k], writes=[score.k])
                            if idx % 8 == 7:
                                yield

                    def stage1b(t):
                        g, blk = blocks[t]
                        score = scores[t % 2]
                        maskb = maskbs[t % 2]
                        m = 4 * g + blk
                        nkb = 2 * m + 2
                        n = nkb * 128
                        S.op("dve", lambda E, n=n: E.tensor_reduce(out=rmax.t[:], in_=score.t[:, :n], axis=AX.X, op=ALU.max),
                             reads=[score.k], writes=[rmax.k])
                        S.op("dve", lambda E, n=n: E.tensor_reduce(out=rmin.t[:], in_=score.t[:, :n], axis=AX.X, op=ALU.min),
                             reads=[score.k], writes=[rmin.k])
                        S.op("dve", lambda E, n=n: E.tensor_tensor(out=score.t[:, n - 256:n], in0=score.t[:, n - 256:n],
                                                                  in1=cmi.t[:], op=ALU.add),
                             reads=[score.k, cmi.k], writes=[score.k])
                        S.op("dve", lambda E: E.tensor_tensor(out=span.t[:], in0=rmax.t[:], in1=rmin.t[:], op=ALU.subtract),
                             reads=[rmax.k, rmin.k], writes=[span.k])
                        S.op("dve", lambda E: E.tensor_scalar(out=span.t[:], in0=span.t[:], scalar1=2e-4, scalar2=None,
                                                              op0=ALU.add), reads=[span.k], writes=[span.k])
                        S.op("dve", lambda E: E.tensor_scalar(out=lo.t[:], in0=rmin.t[:], scalar1=-1e-4, scalar2=None,
                                                              op0=ALU.add), reads=[rmin.k], writes=[lo.k])
                        yield
                        for it in range(NIT):
                            f = 2.0 ** -(it + 1)
                            S.op("dve", lambda E, f=f: E.scalar_tensor_tensor(out=mid.t[:], in0=span.t[:], scalar=f,
                                                                              in1=lo.t[:], op0=ALU.mult, op1=ALU.add),
                                 reads=[span.k, lo.k], writes=[mid.k])
                            S.op("dve", lambda E, n=n, maskb=maskb: E.tensor_scalar(
                                out=maskb.t[:, :n], in0=score.t[:, :n], scalar1=mid.t[:, 0:1], scalar2=0.0, op0=ALU.is_ge,
                                op1=ALU.add, accum_out=cnt.t[:], saturate=False), reads=[score.k, mid.k], writes=[maskb.k, cnt.k])
                            S.op("dve", lambda E, f=f: E.tensor_scalar(out=gs.t[:], in0=cnt.t[:], scalar1=255.5, scalar2=f,
                                                                      op0=ALU.is_ge, op1=ALU.mult),
                                 reads=[cnt.k], writes=[gs.k])
                            S.op("dve", lambda E: E.scalar_tensor_tensor(out=lo.t[:], in0=gs.t[:], scalar=span.t[:, 0:1],
                                                                         in1=lo.t[:], op0=ALU.mult, op1=ALU.add),
                                 reads=[gs.k, span.k, lo.k], writes=[lo.k])
                            yield
                        S.op("dve", lambda E, n=n, maskb=maskb: E.tensor_scalar(
                            out=maskb.t[:, :n], in0=score.t[:, :n], scalar1=lo.t[:, 0:1], scalar2=-30000.0, op0=ALU.is_lt,
                            op1=ALU.mult, saturate=False), reads=[score.k, lo.k], writes=[maskb.k])

                    def stage2(t):
                        nonlocal vcount
                        g, blk = blocks[t]
                        if blk == 0:
                            ldq_s(g)
                        Qs = QS[0]
                        maskb = maskbs[t % 2]
                        m = 4 * g + blk
                        nkb = 2 * m + 2
                        qs = slice(blk * 128, (blk + 1) * 128)
                        for bnk in (4, 5):
                            mmgroup(pb[bnk].t[:, :], pb[bnk], [(zeros.t[:, 0:128], zeros.t[:])], [zeros.k])
                        ngr = (nkb + 3) // 4
                        slots = {}

                        def ldv(kg):
                            nonlocal vcount
                            for kb in range(kg * 4, min(nkb, kg * 4 + 4)):
                                sl = vcount % NV
                                vcount += 1
                                slots[kb] = sl
                                S.dma("sp", Vr[sl].t[:], VAUG[kb * 128:(kb + 1) * 128, :], writes=[Vr[sl].k])
                        ldv(0)
                        yield
                        psb = (pb[6], pb[7], pb[3])
                        items = [(kg, h) for kg in range(ngr) for h in range(8)]
                        for idx in range(len(items) + 2):
                            if idx < len(items):
                                kg, h = items[idx]
                                if h == 2 and kg + 1 < ngr:
                                    ldv(kg + 1)
                                kbs = list(range(kg * 4, min(nkb, kg * 4 + 4)))
                                wg = len(kbs) * 128
                                ps, pt = psb[idx % 3], pT[idx % 4]
                                Qsh = Qs[h % 2]
                                for i, kb in enumerate(kbs):
                                    S.op("pe", lambda E, ps=ps, i=i, kb=kb, h=h, Qsh=Qsh, qs=qs: E.matmul(
                                        ps.t[:, i * 128:(i + 1) * 128], KT.t[:, h // 2, kb * 128:(kb + 1) * 128],
                                        Qsh.t[:, h // 2, qs], start=True, stop=False, skip_group_check=True),
                                        reads=[KT.k, Qsh.k], writes=[ps.k], signal=False)
                                    S.op("pe", lambda E, ps=ps, i=i, kb=kb, maskb=maskb: E.matmul(
                                        ps.t[:, i * 128:(i + 1) * 128], maskb.t[:, kb * 128:(kb + 1) * 128], ident.t[:],
                                        start=False, stop=True, skip_group_check=True),
                                        reads=[maskb.k, ident.k], writes=[ps.k], signal=(i == len(kbs) - 1))
                                S.op("act", lambda E, ps=ps, pt=pt, wg=wg: E.activation(
                                    out=pt.t[:, :wg], in_=ps.t[:, :wg], func=AF.Exp, scale=0.125),
                                    reads=[ps.k], writes=[pt.k])
                            if idx >= 2:
                                kg, h = items[idx - 2]
                                kbs = list(range(kg * 4, min(nkb, kg * 4 + 4)))
                                pt = pT[(idx - 2) % 4]
                                acc = pb[4 + h // 4]
                                a0 = (h % 4) * 128
                                for i, kb in enumerate(kbs):
                                    vr = Vr[slots[kb]]
                                    S.op("pe", lambda E, acc=acc, a0=a0, vr=vr, h=h, pt=pt, i=i: E.matmul(
                                        acc.t[:, a0:a0 + 128], vr.t[:, h * 128:(h + 1) * 128],
                                        pt.t[:, i * 128:(i + 1) * 128], start=False, stop=False, skip_group_check=True),
                                        reads=[vr.k, pt.k], writes=[acc.k], signal=(i == len(kbs) - 1))
                            if idx % 8 == 7:
                                yield
                        yt = ysb[m % 2]
                        for h in range(8):
                            acc = pb[4 + h // 4]
                            a0 = (h % 4) * 128
                            S.op("dve", lambda E, acc=acc, a0=a0: E.reciprocal(out=rdb.t[0:64, :],
                                                                              in_=acc.t[64:128, a0:a0 + 128]),
                                 reads=[acc.k], writes=[rdb.k])
                            p0 = (h % 2) * 64
                            S.op("dve", lambda E, acc=acc, a0=a0, p0=p0, h=h, yt=yt: E.tensor_tensor(
                                out=yt.t[p0:p0 + 64, h // 2, :], in0=acc.t[0:64, a0:a0 + 128], in1=rdb.t[0:64, :],
                                op=ALU.mult), reads=[acc.k, rdb.k], writes=[yt.k])
                        S.dma("sp", YS[:, :, m * 128:(m + 1) * 128].rearrange("t p q -> p t q"), yt.t[:], reads=[yt.k])

                    nb_ = len(blocks)

                    def run_all(gen):
                        for _ in gen:
                            pass

                    def interleave(fast, slow, ratio):
                        fast = [g for g in fast if g is not None]
                        k = 0
                        while fast or slow is not None:
                            for g in list(fast):
                                try:
                                    next(g)
                                except StopIteration:
                                    fast.remove(g)
                            k += 1
                            if slow is not None and (k % ratio == 0 or not fast):
                                try:
                                    next(slow)
                                except StopIteration:
                                    slow = None
                    run_all(stage1a(0))
                    if nb_ > 1:
                        run_all(stage1a(1))
                    run_all(stage1b(0))
                    for t in range(nb_):
                        if t + 2 < nb_:
                            run_all(stage1a(t + 2))
                        if t + 1 < nb_:
                            run_all(stage1b(t + 1))
                        run_all(stage2(t))
                    barrier()

        _phaseB()
        def _phaseC():
            if stop_after >= 7:
                with ExitStack() as ph:
                    wg = sb(ph, "wgC", [128, 8, 3072], BF16)
                    wb = sb(ph, "wbC", [128, 12, 1024], BF16)
                    wo = sb(ph, "woC", [128, 8, 1024], BF16)
                    bg = sb(ph, "bgC", [128, 24], F32)
                    gf8 = sb(ph, "gf8C", [128, 8], F32)
                    mkT = sb(ph, "mkT", [128, 4, 256], BF16)
                    mv = sb(ph, "mvC", [128, 2, 512], BF16)
                    wload(wg.t[:], w_in[:, C_GL:C_GL + 3072].rearrange("(k p) c -> p k c", p=128), wg.k)
                    for br in range(3):
                        wload(wb.t[:, br * 4:(br + 1) * 4, :], w_br[br].rearrange("(k p) c -> p k c", p=128), wb.k)
                    wload(wo.t[:], w_out.rearrange("(k p) c -> p k c", p=128), wo.k)
                    S.dma("sp", bg.t[:], b_gate24, writes=[bg.k])
                    S.dma("sp", gf8.t[:], gffn8, writes=[gf8.k])
                    xs = [sb(ph, f"xsC{i}", [128, 8, 512], F32) for i in range(1)]
                    sq = sb(ph, "sqC", [128, 8, 512], BF16)
                    ntc = [sb(ph, f"ntC{i}", [128, 8, 512], BF16) for i in range(1)]
                    sd = sb(ph, "sdC", [128, 512], F32)
                    rs = sb(ph, "rsC", [128, 512], F32)
                    with ExitStack() as ph2:
                        wm = sb(ph2, "wmC", [128, 8, 1024], BF16)
                        gm8 = sb(ph2, "gm8C", [128, 8], F32)
                        wload(wm.t[:], w_mem_kv.rearrange("(k p) c -> p k c", p=128), wm.k)
                        S.dma("sp", gm8.t[:], gmem8, writes=[gm8.k])
                        S.dma("sp", xs[0].t[:, :, 0:256], memT.rearrange("(k p) t -> p k t", p=128), writes=[xs[0].k])
                        norm_chunk(xs[0], sq, ntc[0], sd, rs, gm8, 256)
                        for hm in range(4):
                            mmgroup(pb[0].t[:, :256], pb[0],
                                    [(wm.t[:, kc, hm * 128:(hm + 1) * 128], ntc[0].t[:, kc, 0:256]) for kc in range(8)],
                                    [wm.k, ntc[0].k])
                            S.op("act", lambda E, hm=hm: E.activation(out=mkT.t[:, hm, :], in_=pb[0].t[:, :256], func=AF.Copy),
                                 reads=[pb[0].k], writes=[mkT.k])
                        for mb in range(2):
                            mmgroup(pb[1].t[:, :], pb[1],
                                    [(ntc[0].t[:, kc, mb * 128:(mb + 1) * 128], wm.t[:, kc, 512:1024]) for kc in range(8)],
                                    [wm.k, ntc[0].k])
                            S.op("act", lambda E, mb=mb: E.activation(out=mv.t[:, mb, :], in_=pb[1].t[:, :], func=AF.Copy),
                                 reads=[pb[1].k], writes=[mv.k])
                        barrier()
                    mq = [sb(ph, f"mqC{i}", [128, 4, 512], BF16) for i in range(1)]
                    yd = [sb(ph, f"ydC{i}", [128, 4, 512], BF16) for i in range(1)]
                    ys = [sb(ph, f"ysC{i}", [128, 4, 512], BF16) for i in range(1)]
                    ym = sb(ph, "ymC", [128, 4, 512], BF16)
                    xt = [sb(ph, f"xtC{i}", [128, 4, 1024], F32) for i in range(1)]
                    pT = [sb(ph, f"pTC{i}", [128, 512], BF16) for i in range(2)]
                    rdb = sb(ph, "rdbC", [128, 512], F32)
                    gate = [sb(ph, f"gateC{i}", [128, 512], F32) for i in range(2)]
                    macc = sb(ph, "maccC", [128, 512], F32)
                    mtmp = sb(ph, "mtmpC", [128, 512], F32)
                    mg = sb(ph, "mgC", [128, 8, 512], BF16)
                    xn = sb(ph, "xnC", [128, 8, 512], BF16)
                    NCC = 8 if NCH_LIMIT is None else NCH_LIMIT

                    def ld_mq(c):
                        S.dma("sp", mq[0].t[:], MQT[:, :, c * 512:(c + 1) * 512].rearrange("h p t -> p h t"), writes=[mq[0].k])

                    def ld_merge(c):
                        cs = slice(c * 512, (c + 1) * 512)
                        S.dma("sp", ntc[0].t[:], NTO[:, :, cs].rearrange("k p t -> p k t"), writes=[ntc[0].k])
                        S.dma("sp", yd[0].t[:], YD[:, :, cs].rearrange("h p t -> p h t"), writes=[yd[0].k])
                        S.dma("sp", ys[0].t[:], YS[:, :, cs].rearrange("h p t -> p h t"), writes=[ys[0].k])

                    def ld_xs(c):
                        S.dma("sp", xs[0].t[:], xT_own[:, c * 512:(c + 1) * 512].rearrange("(k p) t -> p k t", p=128),
                              writes=[xs[0].k])

                    def ld_xt(c):
                        S.dma("sp", xt[0].t[:], x_own[c * 512:(c + 1) * 512, :].rearrange("(b p) d -> p b d", p=128),
                              writes=[xt[0].k])
                    ld_mq(0)
                    ld_merge(0)
                    ld_xs(0)
                    ld_xt(0)
                    for c in range(NCC):
                        i = 0
                        nxt = c + 1 < NCC
                        nb, mqc, ydc, ysc, xsc, xtc = ntc[i], mq[i], yd[i], ys[i], xs[i], xt[i]
                        for hm in range(4):
                            for mb in range(2):
                                ps, pt = pb[mb], pT[mb]
                                mmgroup(ps.t[:, :], ps, [(mkT.t[:, hm, mb * 128:(mb + 1) * 128], mqc.t[:, hm, :])],
                                        [mkT.k, mqc.k])
                                S.op("act", lambda E, ps=ps, pt=pt: E.activation(out=pt.t[:], in_=ps.t[:], func=AF.Exp,
                                                                                scale=128.0 ** -0.5),
                                     reads=[ps.k], writes=[pt.k])
                            mmgroup(pb[2].t[:, :], pb[2],
                                    [(mv.t[:, mb, hm * 128:(hm + 1) * 128], pT[mb].t[:]) for mb in range(2)],
                                    [mv.k, pT[0].k, pT[1].k])
                            mmgroup(pb[3].t[:, :], pb[3], [(ones.t[:], pT[mb].t[:]) for mb in range(2)],
                                    [ones.k, pT[0].k, pT[1].k])
                            S.op("dve", lambda E: E.reciprocal(out=rdb.t[:], in_=pb[3].t[:]), reads=[pb[3].k], writes=[rdb.k])
                            S.op("dve", lambda E, hm=hm: E.tensor_tensor(out=ym.t[:, hm, :], in0=pb[2].t[:], in1=rdb.t[:],
                                                                        op=ALU.mult),
                                 reads=[pb[2].k, rdb.k], writes=[ym.k])
                        if nxt:
                            ld_mq(c + 1)
                        ysrc = (ydc, ysc, ym)
                        for ct in range(8):
                            for br in range(3):
                                pg, pbr, gt = pb[4 + br % 2], pb[6 + br % 2], gate[br % 2]
                                col = br * 1024 + ct * 128
                                mmgroup(pg.t[:, :], pg, [(wg.t[:, kc, col:col + 128], nb.t[:, kc, :]) for kc in range(8)],
                                        [wg.k, nb.k])
                                S.op("act", lambda E, pg=pg, gt=gt, br=br, ct=ct: E.activation(
                                    out=gt.t[:], in_=pg.t[:], func=AF.Sigmoid, bias=bg.t[:, br * 8 + ct:br * 8 + ct + 1]),
                                    reads=[pg.k, bg.k], writes=[gt.k])
                                ysb_ = ysrc[br]
                                mmgroup(pbr.t[:, :], pbr,
                                        [(wb.t[:, br * 4 + kc, ct * 128:(ct + 1) * 128], ysb_.t[:, kc, :]) for kc in range(4)],
                                        [wb.k, ysb_.k])
                                if br == 0:
                                    S.op("dve", lambda E, pbr=pbr, gt=gt: E.tensor_tensor(out=macc.t[:], in0=pbr.t[:],
                                                                                         in1=gt.t[:], op=ALU.mult),
                                         reads=[pbr.k, gt.k], writes=[macc.k])
                                else:
                                    S.op("dve", lambda E, pbr=pbr, gt=gt: E.tensor_tensor(out=mtmp.t[:], in0=pbr.t[:],
                                                                                         in1=gt.t[:], op=ALU.mult),
                                         reads=[pbr.k, gt.k], writes=[mtmp.k])
                                    if br == 1:
                                        S.op("pool", lambda E: E.tensor_tensor(out=macc.t[:], in0=macc.t[:], in1=mtmp.t[:],
                                                                               op=ALU.add),
                                             reads=[macc.k, mtmp.k], writes=[macc.k])
                                    else:
                                        S.op("pool", lambda E, ct=ct: E.tensor_tensor(out=mg.t[:, ct, :], in0=macc.t[:],
                                                                                      in1=mtmp.t[:], op=ALU.add),
                                             reads=[macc.k, mtmp.k], writes=[mg.k])
                        if nxt:
                            ld_merge(c + 1)
                        for ct in range(8):
                            po = pb[ct % 2]
                            mmgroup(po.t[:, :], po, [(wo.t[:, kc, ct * 128:(ct + 1) * 128], mg.t[:, kc, :]) for kc in range(8)],
                                    [wo.k, mg.k])
                            S.op("dve", lambda E, po=po, ct=ct, xsc=xsc: E.tensor_tensor(out=xsc.t[:, ct, :], in0=po.t[:],
                                                                                        in1=xsc.t[:, ct, :], op=ALU.add),
                                 reads=[po.k, xsc.k], writes=[xsc.k])
                        for tb in range(4):
                            for hf in range(2):
                                po = pb[2 + (tb * 2 + hf) % 2]
                                mmgroup(po.t[:, :], po,
                                        [(mg.t[:, kc, tb * 128:(tb + 1) * 128], wo.t[:, kc, hf * 512:(hf + 1) * 512])
                                         for kc in range(8)], [wo.k, mg.k])
                                S.op("dve", lambda E, po=po, tb=tb, hf=hf, xtc=xtc: E.tensor_tensor(
                                    out=xtc.t[:, tb, hf * 512:(hf + 1) * 512], in0=po.t[:],
                                    in1=xtc.t[:, tb, hf * 512:(hf + 1) * 512], op=ALU.add),
                                    reads=[po.k, xtc.k], writes=[xtc.k])
                        S.dma("sp", H2[c * 512:(c + 1) * 512, :].rearrange("(b p) d -> p b d", p=128), xtc.t[:], reads=[xtc.k])
                        if nxt:
                            ld_xt(c + 1)
                        norm_chunk(xsc, sq, xn, sd, rs, gf8, 512)
                        S.dma("sp", XNT[:, :, c * 512:(c + 1) * 512].rearrange("k p t -> p k t"), xn.t[:], reads=[xn.k])
                        if nxt:
                            ld_xs(c + 1)
                    barrier()

        _phaseC()
        def _phaseD():
            if stop_after >= 8:
                with ExitStack() as ph:
                    acc = sb(ph, "accD", [128, 16, 1024], F32)
                    xnh = sb(ph, "xnhD", [128, 8, 2048], BF16)
                    w1 = [sb(ph, f"w1D{i}", [128, 8, 1024], BF16) for i in range(2)]
                    w2 = [sb(ph, f"w2D{i}", [128, 4, 1024], BF16) for i in range(2)]
                    wr = sb(ph, "wrD", [128, 8, 20], BF16)
                    brb = sb(ph, "brD", [128, 20], F32)
                    gfb = sb(ph, "gfbD", [128, 1024], F32)
                    cw = sb(ph, "cwD", [128, 16, 16], F32)
                    hid = [sb(ph, f"hidD{i}", [128, 4, 512], BF16) for i in range(2)]
                    sg = [sb(ph, f"sgD{i}", [128, 512], F32) for i in range(2)]
                    lg = sb(ph, "lgD", [128, 20], F32)
                    elm = sb(ph, "elmD", [128, 16], F32)
                    elm2 = sb(ph, "elm2D", [128, 16], F32)
                    oh1 = sb(ph, "oh1D", [128, 16], F32)
                    oh2 = sb(ph, "oh2D", [128, 16], F32)
                    ohg = sb(ph, "ohgD", [128, 4], F32)
                    egj = sb(ph, "egjD", [128, 4], F32)
                    sm = {nm: sb(ph, nm + "D", [128, 1], F32) for nm in
                          ("gmax", "ngmax", "sumg", "pgrp", "m1", "m2", "dd", "ed", "den", "wa", "wb2", "c1", "c2",
                           "ssum", "sdv", "rsv")}
                    junk = sb(ph, "junkD", [128, 1024], BF16)
                    ot = [sb(ph, f"otD{i}", [128, 1024], F32) for i in range(2)]
                    wload(wr.t[:], w_r.rearrange("(k p) c -> p k c", p=128), wr.k)
                    S.dma("sp", brb.t[:], b_r[0, :].partition_broadcast(128), writes=[brb.k])
                    S.dma("sp", gfb.t[:], gfin[0, :].partition_broadcast(128), writes=[gfb.k])
                    NHALF = 2 if NCH_LIMIT is None else 1
                    NE = 16
                    BIGN = 1e30

                    def small(eng, fn, reads, writes):
                        S.op(eng, fn, reads=[r.k for r in reads], writes=[w.k for w in writes])

                    ecount = 0
                    for hf in range(NHALF):
                        t0 = hf * 2048
                        S.dma("sp", acc.t[:], H2[t0:t0 + 2048, :].rearrange("(b p) d -> p b d", p=128), writes=[acc.k])
                        S.dma("sp", xnh.t[:], XNT[:, :, t0:t0 + 2048].rearrange("k p t -> p k t"), writes=[xnh.k])
                        def router(tb):
                            mmgroup(pb[6].t[:, 0:20], pb[6],
                                    [(xnh.t[:, kc, tb * 128:(tb + 1) * 128], wr.t[:, kc, :]) for kc in range(8)], [xnh.k, wr.k])
                            small("dve", lambda E: E.tensor_tensor(out=lg.t[:], in0=pb[6].t[:, 0:20], in1=brb.t[:], op=ALU.add),
                                  [pb[6], brb], [lg])
                            small("dve", lambda E: E.tensor_reduce(out=sm["gmax"].t[:], in_=lg.t[:, 0:4], axis=AX.X, op=ALU.max),
                                  [lg], [sm["gmax"]])
                            small("dve", lambda E: E.tensor_scalar(out=ohg.t[:], in0=lg.t[:, 0:4], scalar1=sm["gmax"].t[:, 0:1],
                                                                   scalar2=BIGN, op0=ALU.is_lt, op1=ALU.mult),
                                  [lg, sm["gmax"]], [ohg])
                            small("dve", lambda E: E.tensor_scalar(out=sm["ngmax"].t[:], in0=sm["gmax"].t[:], scalar1=-1.0,
                                                                   scalar2=None, op0=ALU.mult), [sm["gmax"]], [sm["ngmax"]])
                            small("act", lambda E: E.activation(out=egj.t[:], in_=lg.t[:, 0:4], func=AF.Exp,
                                                                bias=sm["ngmax"].t[:, 0:1], accum_out=sm["sumg"].t[:]),
                                  [lg, sm["ngmax"]], [egj, sm["sumg"]])
                            small("dve", lambda E: E.reciprocal(out=sm["pgrp"].t[:], in_=sm["sumg"].t[:]), [sm["sumg"]],
                                  [sm["pgrp"]])
                            for gi in range(4):
                                small("dve", lambda E, gi=gi: E.tensor_scalar(
                                    out=elm.t[:, gi * 4:(gi + 1) * 4], in0=lg.t[:, 4 + gi * 4:8 + gi * 4],
                                    scalar1=ohg.t[:, gi:gi + 1], scalar2=None, op0=ALU.subtract), [lg, ohg], [elm])
                            small("dve", lambda E: E.tensor_reduce(out=sm["m1"].t[:], in_=elm.t[:], axis=AX.X, op=ALU.max),
                                  [elm], [sm["m1"]])
                            small("dve", lambda E: E.tensor_scalar(out=oh1.t[:], in0=elm.t[:], scalar1=sm["m1"].t[:, 0:1],
                                                                   scalar2=None, op0=ALU.is_ge), [elm, sm["m1"]], [oh1])
                            small("dve", lambda E: E.scalar_tensor_tensor(out=elm2.t[:], in0=oh1.t[:], scalar=-BIGN, in1=elm.t[:],
                                                                          op0=ALU.mult, op1=ALU.add), [oh1, elm], [elm2])
                            small("dve", lambda E: E.tensor_reduce(out=sm["m2"].t[:], in_=elm2.t[:], axis=AX.X, op=ALU.max),
                                  [elm2], [sm["m2"]])
                            small("dve", lambda E: E.tensor_scalar(out=oh2.t[:], in0=elm2.t[:], scalar1=sm["m2"].t[:, 0:1],
                                                                   scalar2=None, op0=ALU.is_ge), [elm2, sm["m2"]], [oh2])
                            small("dve", lambda E: E.tensor_tensor(out=sm["dd"].t[:], in0=sm["m2"].t[:], in1=sm["m1"].t[:],
                                                                   op=ALU.subtract), [sm["m1"], sm["m2"]], [sm["dd"]])
                            small("act", lambda E: E.activation(out=sm["ed"].t[:], in_=sm["dd"].t[:], func=AF.Exp),
                                  [sm["dd"]], [sm["ed"]])
                            small("dve", lambda E: E.tensor_scalar(out=sm["den"].t[:], in0=sm["ed"].t[:], scalar1=1.0,
                                                                   scalar2=None, op0=ALU.add), [sm["ed"]], [sm["den"]])
                            small("dve", lambda E: E.reciprocal(out=sm["wa"].t[:], in_=sm["den"].t[:]), [sm["den"]], [sm["wa"]])
                            small("dve", lambda E: E.tensor_tensor(out=sm["wb2"].t[:], in0=sm["ed"].t[:], in1=sm["wa"].t[:],
                                                                   op=ALU.mult), [sm["ed"], sm["wa"]], [sm["wb2"]])
                            small("dve", lambda E: E.tensor_tensor(out=sm["c1"].t[:], in0=sm["wa"].t[:], in1=sm["pgrp"].t[:],
                                                                   op=ALU.mult), [sm["wa"], sm["pgrp"]], [sm["c1"]])
                            small("dve", lambda E: E.tensor_tensor(out=sm["c2"].t[:], in0=sm["wb2"].t[:], in1=sm["pgrp"].t[:],
                                                                   op=ALU.mult), [sm["wb2"], sm["pgrp"]], [sm["c2"]])
                            small("dve", lambda E: E.tensor_scalar(out=oh1.t[:], in0=oh1.t[:], scalar1=sm["c1"].t[:, 0:1],
                                                                   scalar2=None, op0=ALU.mult), [oh1, sm["c1"]], [oh1])
                            small("dve", lambda E, tb=tb: E.scalar_tensor_tensor(out=cw.t[:, tb, :], in0=oh2.t[:],
                                                                                 scalar=sm["c2"].t[:, 0:1], in1=oh1.t[:],
                                                                                 op0=ALU.mult, op1=ALU.add),
                                  [oh2, sm["c2"], oh1], [cw])
                        for e in range(NE):
                            wi, wo_ = w1[ecount % 2], w2[ecount % 2]
                            ecount += 1
                            wload(wi.t[:], w_e_in[e].rearrange("(k p) c -> p k c", p=128), wi.k)
                            wload(wo_.t[:], w_e_out[e].rearrange("(k p) c -> p k c", p=128), wo_.k)
                            for ch in range(4):
                                hd = hid[ch % 2]
                                cs = slice(ch * 512, (ch + 1) * 512)
                                for ct in range(4):
                                    pg, pu, sgt = pb[ct % 2], pb[2 + ct % 2], sg[ct % 2]
                                    mmgroup(pg.t[:, :], pg,
                                            [(wi.t[:, kc, ct * 128:(ct + 1) * 128], xnh.t[:, kc, cs]) for kc in range(8)],
                                            [wi.k, xnh.k])
                                    mmgroup(pu.t[:, :], pu,
                                            [(wi.t[:, kc, 512 + ct * 128:512 + (ct + 1) * 128], xnh.t[:, kc, cs])
                                             for kc in range(8)], [wi.k, xnh.k])
                                    S.op("act", lambda E, pg=pg, sgt=sgt: E.activation(out=sgt.t[:], in_=pg.t[:], func=AF.Silu),
                                         reads=[pg.k], writes=[sgt.k])
                                    S.op("dve", lambda E, pu=pu, sgt=sgt, hd=hd, ct=ct: E.tensor_tensor(
                                        out=hd.t[:, ct, :], in0=pu.t[:], in1=sgt.t[:], op=ALU.mult),
                                        reads=[pu.k, sgt.k], writes=[hd.k])
                                for tbl in range(4):
                                    tb = ch * 4 + tbl
                                    if e == 0:
                                        router(tb)
                                    for dh in range(2):
                                        po = pb[4 + (tbl * 2 + dh) % 2]
                                        mmgroup(po.t[:, :], po,
                                                [(hd.t[:, kc, tbl * 128:(tbl + 1) * 128], wo_.t[:, kc, dh * 512:(dh + 1) * 512])
                                                 for kc in range(4)], [hd.k, wo_.k])
                                        S.op("dve", lambda E, po=po, tb=tb, dh=dh, e=e: E.scalar_tensor_tensor(
                                            out=acc.t[:, tb, dh * 512:(dh + 1) * 512], in0=po.t[:], scalar=cw.t[:, tb, e:e + 1],
                                            in1=acc.t[:, tb, dh * 512:(dh + 1) * 512], op0=ALU.mult, op1=ALU.add),
                                            reads=[po.k, cw.k, acc.k], writes=[acc.k])
                        for tb in range(16):
                            o = ot[tb % 2]
                            small("act", lambda E, tb=tb: E.activation(out=junk.t[:], in_=acc.t[:, tb, :], func=AF.Square,
                                                                       accum_out=sm["ssum"].t[:]), [acc], [junk, sm["ssum"]])
                            small("act", lambda E: E.activation(out=sm["sdv"].t[:], in_=sm["ssum"].t[:], func=AF.Sqrt,
                                                                scale=1.0 / 1024, bias=EPS), [sm["ssum"]], [sm["sdv"]])
                            small("dve", lambda E: E.reciprocal(out=sm["rsv"].t[:], in_=sm["sdv"].t[:]), [sm["sdv"]], [sm["rsv"]])
                            small("dve", lambda E, tb=tb, o=o: E.scalar_tensor_tensor(out=o.t[:], in0=acc.t[:, tb, :],
                                                                                      scalar=sm["rsv"].t[:, 0:1], in1=gfb.t[:],
                                                                                      op0=ALU.mult, op1=ALU.mult),
                                  [acc, sm["rsv"], gfb], [o])
                            r0 = t0 + tb * 128
                            S.dma("sp", out_own[r0:r0 + 128, :], o.t[:], reads=[o.k])
                    barrier()

        _phaseD()
        S.finish([])
        globals()['_LAST_S'] = S
        S.emit()
    return nc


def _consts(j):
    ident = np.eye(128, dtype=np.float32)
    rmat = np.zeros((128, 128), np.float32)
    invf = np.zeros((128, 1), np.float32)
    for p in range(128):
        d = p % 64
        if d < 8:
            rmat[p + 8, p] = -1.0
        elif d < 16:
            rmat[p - 8, p] = 1.0
        if d < 16:
            invf[p, 0] = np.float32(500000.0) ** (-np.float32(2 * (d % 8)) / np.float32(16))
    k = np.arange(128)[:, None]
    r = np.arange(128)[None, :]
    tri = (k <= r).astype(np.float32)
    cm_diff = np.zeros((128, 8, 512), np.float32)
    for z in range(8):
        for blk in range(4):
            gi = 2 * blk + j
            if z < gi:
                cm_diff[:, z, blk * 128:(blk + 1) * 128] = 1.0
            elif z == gi:
                cm_diff[:, z, blk * 128:(blk + 1) * 128] = tri
    cm_idx = np.zeros((128, 256), np.float32)
    for cb in range(2):
        if cb == j:
            cm_idx[:, cb * 128:(cb + 1) * 128] = np.where(tri.T > 0, 0.0, -1e30)
        elif cb > j:
            cm_idx[:, cb * 128:(cb + 1) * 128] = -1e30
    return ident, rmat, invf, cm_diff, cm_idx


def _own_idx(j):
    return np.concatenate([np.arange(128) + (2 * m + j) * 128 for m in range(32)])


def make_in_maps(x, positions, mem, g_mix, w_in, b_gate, lambda_q1, lambda_k1, lambda_q2, lambda_k2,
                 g_diff_sub, g_mem, w_mem_kv, w_br_diff, w_br_dsa, w_br_mem, w_out, g_ffn,
                 w_route_group, b_route_group, w_route_expert, b_route_expert, w_exp_in, w_exp_out,
                 g_final, cores=range(8)):
    f = lambda a: np.ascontiguousarray(np.asarray(a))
    x = np.asarray(x); positions = np.asarray(positions); mem = np.asarray(mem)
    col8 = lambda g: f(np.asarray(g).reshape(8, 128).T)
    shared = {
        "w_in": f(np.asarray(w_in)[0]),
        "b_gate24": f(np.asarray(b_gate)[0].reshape(24, 128).T),
        "lam_in": f(np.concatenate([np.asarray(a)[0] for a in (lambda_q1, lambda_k1, lambda_q2, lambda_k2)])[None, :]),
        "gsub_in": f(np.asarray(g_diff_sub)[0][:, None]),
        "gmix8": col8(np.asarray(g_mix)[0]),
        "gmem8": col8(np.asarray(g_mem)[0]),
        "gffn8": col8(np.asarray(g_ffn)[0]),
        "w_mem_kv": f(np.asarray(w_mem_kv)[0]),
        "w_br": f(np.stack([np.asarray(w_br_diff)[0], np.asarray(w_br_dsa)[0], np.asarray(w_br_mem)[0]])),
        "w_out": f(np.asarray(w_out)[0]),
        "w_r": f(np.concatenate([np.asarray(w_route_group)[0], np.asarray(w_route_expert)[0]], axis=1)),
        "b_r": f(np.concatenate([np.asarray(b_route_group)[0], np.asarray(b_route_expert)[0]])[None, :]),
        "w_e_in": f(np.asarray(w_exp_in)[0]),
        "w_e_out": f(np.asarray(w_exp_out)[0]),
        "gfin": f(np.asarray(g_final)[None, :]),
    }
    maps = []
    for c in cores:
        b, j = c // 2, c % 2
        own = _own_idx(j)
        ident, rmat, invf, cm_diff, cm_idx = _consts(j)
        m = dict(shared)
        m.update({
            "xT_full": f(x[b].T), "xT_own": f(x[b][own].T), "x_own": f(x[b][own]),
            "pos_full": f(positions[b][None, :].astype(np.int32)),
            "pos_own": f(positions[b][own][None, :].astype(np.int32)),
            "memT": f(mem[b].T),
            "ident_in": ident, "rmat_in": rmat, "invf_in": invf, "cm_diff_in": cm_diff, "cm_idx_in": cm_idx,
        })
        maps.append(m)
    return maps


_NC_CACHE = {}


def kernel(**inputs):
    if "nc" not in _NC_CACHE:
        _NC_CACHE["nc"] = build()
    nc = _NC_CACHE["nc"]
    maps = make_in_maps(**inputs)
    res = run_bass_kernel_spmd(nc, maps, core_ids=list(range(8)))
    out = np.zeros((4, 8192, 1024), np.float32)
    for c in range(8):
        b, j = c // 2, c % 2
        out[b][_own_idx(j)] = res.results[c]["out_own"]
    return out
```
